# Optimizing a Trainium2 kernel written in Bass

```python
import math
import numpy as np
import jax
import jax.numpy as jnp
from jax import lax

D_MODEL = 1024
BATCH = 8
SEQ = 2048
DEPTH = 1

GRID_W = 64
CTX_LEN = 256
MIX_WIDTH = D_MODEL
MOD_CHUNKS = 6
EPS = 1e-6

ATTN_HEADS = 8
ATTN_KV_HEADS = 2
ATTN_HEAD_DIM = (MIX_WIDTH // 2) // ATTN_HEADS
ROPE_BASE = 10000.0
Q_BLOCK = 128

DN_HEADS = 4
DN_HEAD_DIM = (MIX_WIDTH // 2) // DN_HEADS
DN_CONV_W = 5
DN_CHUNK = 64

N_EXPERTS = 16
EC_CAPACITY_FACTOR = 2
EXPERT_FF = 1024

ATTN_Q_W = ATTN_HEADS * ATTN_HEAD_DIM
ATTN_KV_W = ATTN_KV_HEADS * ATTN_HEAD_DIM
DN_W = DN_HEADS * DN_HEAD_DIM
IN_SPLITS = (ATTN_Q_W, ATTN_KV_W, ATTN_KV_W, DN_W, DN_W, DN_W, DN_W, 2 * DN_HEADS, 2 * DN_HEADS)
IN_WIDTH = sum(IN_SPLITS)
IN_OFFSETS = tuple(int(v) for v in np.cumsum(IN_SPLITS)[:-1])

kernel_name = "hybrid_dit_gqa_gdn_ec_moe"


def rmsnorm(x, w):
    xf = x.astype(jnp.float32)
    xf = xf * lax.rsqrt(jnp.mean(xf * xf, axis=-1, keepdims=True) + EPS)
    return xf.astype(x.dtype) * w


def l2norm(x):
    xf = x.astype(jnp.float32)
    return (xf * lax.rsqrt(jnp.sum(xf * xf, axis=-1, keepdims=True) + EPS)).astype(x.dtype)


def modulate(h, shift, scale):
    return h * (1 + scale) + shift


def split_heads(t, n_heads):
    b, l, w = t.shape
    return t.reshape(b, l, n_heads, w // n_heads).transpose(0, 2, 1, 3)


def merge_heads(t):
    b, n, l, d = t.shape
    return t.transpose(0, 2, 1, 3).reshape(b, l, n * d)


def rope_axis(x, pos):
    m = x.shape[-1] // 2
    freqs = ROPE_BASE ** (-jnp.arange(m, dtype=jnp.float32) / m)
    ang = pos.astype(jnp.float32)[:, None] * freqs[None, :]
    cos = jnp.cos(ang).astype(x.dtype)
    sin = jnp.sin(ang).astype(x.dtype)
    x1, x2 = x[..., :m], x[..., m:]
    return jnp.concatenate([x1 * cos - x2 * sin, x2 * cos + x1 * sin], axis=-1)


def rope2d(x, rows, cols):
    h = x.shape[-1] // 2
    return jnp.concatenate([rope_axis(x[..., :h], rows), rope_axis(x[..., h:], cols)], axis=-1)


def attend_blocked(q, k, v):
    b, hq, lq, dh = q.shape
    hkv = k.shape[1]
    grp = hq // hkv
    nb = lq // Q_BLOCK
    qb = q.reshape(b, hkv, grp, nb, Q_BLOCK, dh).transpose(3, 0, 1, 2, 4, 5)
    scale = dh ** -0.5

    def one_block(qblk):
        s = jnp.einsum('bkgqd,bksd->bkgqs', qblk, k).astype(jnp.float32) * scale
        p = jax.nn.softmax(s, axis=-1).astype(v.dtype)
        return jnp.einsum('bkgqs,bksd->bkgqd', p, v)

    ob = lax.map(one_block, qb)
    return ob.transpose(1, 2, 3, 0, 4, 5).reshape(b, hq, lq, dh)


def short_conv(x, w):
    pad = DN_CONV_W // 2
    return lax.conv_general_dilated(
        x, w[:, None, :], window_strides=(1,), padding=[(pad, pad)],
        dimension_numbers=('NWC', 'WIO', 'NWC'), feature_group_count=x.shape[-1])


def delta_rule_chunked(q, k, v, g, beta, s0, need_output):
    f32 = jnp.float32
    b, h, l, dk = q.shape
    dv = v.shape[-1]
    n = l // DN_CHUNK
    q = q.astype(f32).reshape(b, h, n, DN_CHUNK, dk)
    k = k.astype(f32).reshape(b, h, n, DN_CHUNK, dk)
    v = v.astype(f32).reshape(b, h, n, DN_CHUNK, dv)
    g = g.astype(f32).reshape(b, h, n, DN_CHUNK)
    beta = beta.astype(f32).reshape(b, h, n, DN_CHUNK)

    g_cum = jnp.cumsum(g, axis=-1)
    tril = jnp.tril(jnp.ones((DN_CHUNK, DN_CHUNK), dtype=bool))
    eye = jnp.eye(DN_CHUNK, dtype=bool)
    decay = jnp.exp(jnp.where(tril, g_cum[..., :, None] - g_cum[..., None, :], -jnp.inf))
    k_beta = k * beta[..., None]
    a_strict = jnp.where(tril & ~eye, jnp.einsum('bhnid,bhnjd->bhnij', k_beta, k) * decay, 0.0)
    unit_lower = a_strict + eye.astype(f32)
    rhs = jnp.concatenate([v * beta[..., None], k_beta * jnp.exp(g_cum)[..., None]], axis=-1)
    sol = lax.linalg.triangular_solve(unit_lower, rhs, left_side=True, lower=True, unit_diagonal=True)
    u, w = sol[..., :dv], sol[..., dv:]
    k_dec = k * jnp.exp(g_cum[..., -1:] - g_cum)[..., None]
    g_last = jnp.exp(g_cum[..., -1])

    xs = [u, w, k_dec, g_last]
    if need_output:
        qk = jnp.einsum('bhnid,bhnjd->bhnij', q, k) * decay
        q_dec = q * jnp.exp(g_cum)[..., None]
        xs += [qk, q_dec]
    xs = tuple(jnp.moveaxis(t, 2, 0) for t in xs)

    def step(s, inp):
        u_c, w_c, kd_c, gl_c = inp[:4]
        v_new = u_c - jnp.einsum('bhck,bhkv->bhcv', w_c, s)
        s_next = s * gl_c[..., None, None] + jnp.einsum('bhck,bhcv->bhkv', kd_c, v_new)
        if need_output:
            qk_c, qd_c = inp[4:]
            o = jnp.einsum('bhck,bhkv->bhcv', qd_c, s) + jnp.einsum('bhij,bhjv->bhiv', qk_c, v_new)
            return s_next, o
        return s_next, None

    s_final, o = lax.scan(step, s0.astype(f32), xs)
    if need_output:
        o = jnp.moveaxis(o, 0, 2).reshape(b, h, l, dv)
    return o, s_final


def dn_gates(alpha, beta_logits, a_log, dt_bias):
    b, l, _ = alpha.shape
    alpha = alpha.astype(jnp.float32).reshape(b, l, 2, DN_HEADS).transpose(2, 0, 3, 1)
    beta = jax.nn.sigmoid(beta_logits.astype(jnp.float32).reshape(b, l, 2, DN_HEADS).transpose(2, 0, 3, 1))
    a = jnp.exp(a_log.astype(jnp.float32))[:, None, :, None]
    g = -a * jax.nn.softplus(alpha + dt_bias.astype(jnp.float32)[:, None, :, None])
    return g, beta


def token_mixer(h_lat, h_ctx, rows, cols, w_in, q_norm_w, k_norm_w, conv_w, a_log, dt_bias,
                o_norm_w, w_out, update_ctx):
    p_lat = h_lat @ w_in
    p_ctx = h_ctx @ w_in
    aq_l, ak_l, av_l, dq_l, dk_l, dv_l, dz_l, db_l, da_l = jnp.split(p_lat, IN_OFFSETS, axis=-1)
    aq_c, ak_c, av_c, dq_c, dk_c, dv_c, dz_c, db_c, da_c = jnp.split(p_ctx, IN_OFFSETS, axis=-1)

    def attn_qkv(aq, ak, av):
        q = rmsnorm(split_heads(aq, ATTN_HEADS), q_norm_w)
        k = rmsnorm(split_heads(ak, ATTN_KV_HEADS), k_norm_w)
        return q, k, split_heads(av, ATTN_KV_HEADS)

    q_l, k_l, v_l = attn_qkv(aq_l, ak_l, av_l)
    q_l = rope2d(q_l, rows, cols)
    k_l = rope2d(k_l, rows, cols)
    q_c, k_c, v_c = attn_qkv(aq_c, ak_c, av_c)
    k_all = jnp.concatenate([k_c, k_l], axis=2)
    v_all = jnp.concatenate([v_c, v_l], axis=2)
    att_lat = merge_heads(attend_blocked(q_l, k_all, v_all))

    def dn_qkv(dq, dk, dv):
        qkv = jax.nn.silu(short_conv(jnp.concatenate([dq, dk, dv], axis=-1), conv_w))
        q, k, v = jnp.split(qkv, 3, axis=-1)
        q = l2norm(split_heads(q, DN_HEADS)) * (DN_HEAD_DIM ** -0.5)
        k = l2norm(split_heads(k, DN_HEADS))
        return q, k, split_heads(v, DN_HEADS)

    dq_lh, dk_lh, dv_lh = dn_qkv(dq_l, dk_l, dv_l)
    dq_ch, dk_ch, dv_ch = dn_qkv(dq_c, dk_c, dv_c)
    g_l, b_l = dn_gates(da_l, db_l, a_log, dt_bias)
    g_c, b_c = dn_gates(da_c, db_c, a_log, dt_bias)
    batch = h_lat.shape[0]

    def seq_flip(t):
        return jnp.flip(t, axis=2)

    outs_lat, outs_ctx = [], []
    for d in range(2):
        ctx_in = (dq_ch, dk_ch, dv_ch, g_c[d], b_c[d])
        lat_in = (dq_lh, dk_lh, dv_lh, g_l[d], b_l[d])
        if d == 1:
            ctx_in = tuple(seq_flip(t) for t in ctx_in)
            lat_in = tuple(seq_flip(t) for t in lat_in)
        s0 = jnp.zeros((batch, DN_HEADS, DN_HEAD_DIM, DN_HEAD_DIM), jnp.float32)
        o_c, s_c = delta_rule_chunked(*ctx_in, s0, need_output=update_ctx)
        o_l, _ = delta_rule_chunked(*lat_in, s_c, need_output=True)
        outs_lat.append(seq_flip(o_l) if d == 1 else o_l)
        if update_ctx:
            outs_ctx.append(seq_flip(o_c) if d == 1 else o_c)

    def dn_output(o, dz):
        o = o.astype(dz.dtype).transpose(0, 2, 1, 3)
        z = dz.reshape(dz.shape[0], dz.shape[1], DN_HEADS, DN_HEAD_DIM)
        y = rmsnorm(o, o_norm_w) * jax.nn.silu(z)
        return y.reshape(dz.shape[0], dz.shape[1], DN_W)

    dn_lat = dn_output(outs_lat[0] + outs_lat[1], dz_l)
    y_lat = jnp.concatenate([att_lat, dn_lat], axis=-1) @ w_out

    y_ctx = None
    if update_ctx:
        att_ctx = merge_heads(attend_blocked(q_c, k_c, v_c))
        dn_ctx = dn_output(outs_ctx[0] + outs_ctx[1], dz_c)
        y_ctx = jnp.concatenate([att_ctx, dn_ctx], axis=-1) @ w_out
    return y_lat, y_ctx


def expert_choice_ffn(h, router_w, w_gate, w_up, w_down):
    b, t, d = h.shape
    cap = EC_CAPACITY_FACTOR * t // N_EXPERTS
    affinity = jax.nn.softmax((h @ router_w).astype(jnp.float32), axis=-1)
    gates, idx = lax.top_k(jnp.swapaxes(affinity, 1, 2), cap)
    xg = jax.vmap(lambda hb, ib: hb[ib])(h, idx)
    hid = jax.nn.silu(jnp.einsum('becd,edf->becf', xg, w_gate)) * jnp.einsum('becd,edf->becf', xg, w_up)
    y = jnp.einsum('becf,efd->becd', hid, w_down) * gates[..., None].astype(h.dtype)
    return jax.vmap(lambda yb, ib: jnp.zeros((t, d), yb.dtype).at[ib.reshape(-1)].add(yb.reshape(-1, d)))(y, idx)


def setup_inputs(seed: int = 0) -> dict:
    key = jax.random.key(seed)
    ks = jax.random.split(key, 20)
    f32 = jnp.float32

    def nrm(k, shape, scale):
        return jax.random.normal(k, shape, f32) * scale

    dt = jnp.exp(jax.random.uniform(ks[14], (DEPTH, 2, DN_HEADS), f32, math.log(1e-3), math.log(1e-1)))
    return {
        "x": nrm(ks[0], (BATCH, SEQ, D_MODEL), 1.0),
        "c": nrm(ks[1], (BATCH, D_MODEL), 1.0),
        "ctx": nrm(ks[2], (BATCH, CTX_LEN, D_MODEL), 1.0),
        "c_ctx": nrm(ks[3], (D_MODEL,), 1.0),
        "w_mod": nrm(ks[4], (DEPTH, D_MODEL, MOD_CHUNKS * D_MODEL), 0.5 * D_MODEL ** -0.5),
        "b_mod": nrm(ks[5], (DEPTH, MOD_CHUNKS * D_MODEL), 0.02),
        "norm1_w": 1.0 + nrm(ks[6], (DEPTH, D_MODEL), 0.02),
        "norm2_w": 1.0 + nrm(ks[7], (DEPTH, D_MODEL), 0.02),
        "w_in": nrm(ks[8], (DEPTH, D_MODEL, IN_WIDTH), D_MODEL ** -0.5),
        "q_norm_w": 1.0 + nrm(ks[9], (DEPTH, ATTN_HEAD_DIM), 0.02),
        "k_norm_w": 1.0 + nrm(ks[10], (DEPTH, ATTN_HEAD_DIM), 0.02),
        "conv_w": nrm(ks[11], (DEPTH, DN_CONV_W, 3 * DN_W), DN_CONV_W ** -0.5),
        "a_log": jnp.log(jax.random.uniform(ks[12], (DEPTH, 2, DN_HEADS), f32, 1.0, 16.0)),
        "dt_bias": dt + jnp.log(-jnp.expm1(-dt)),
        "o_norm_w": 1.0 + nrm(ks[13], (DEPTH, DN_HEAD_DIM), 0.02),
        "w_out": nrm(ks[15], (DEPTH, MIX_WIDTH, D_MODEL), MIX_WIDTH ** -0.5),
        "router_w": nrm(ks[16], (DEPTH, D_MODEL, N_EXPERTS), D_MODEL ** -0.5),
        "w_gate": nrm(ks[17], (DEPTH, N_EXPERTS, D_MODEL, EXPERT_FF), D_MODEL ** -0.5),
        "w_up": nrm(ks[18], (DEPTH, N_EXPERTS, D_MODEL, EXPERT_FF), D_MODEL ** -0.5),
        "w_down": nrm(ks[19], (DEPTH, N_EXPERTS, EXPERT_FF, D_MODEL), EXPERT_FF ** -0.5),
    }


def reference(x, c, ctx, c_ctx, w_mod, b_mod, norm1_w, norm2_w, w_in, q_norm_w, k_norm_w, conv_w,
              a_log, dt_bias, o_norm_w, w_out, router_w, w_gate, w_up, w_down):
    seq_len = x.shape[1]
    n_rows = seq_len // GRID_W
    rows = jnp.repeat(jnp.arange(n_rows, dtype=jnp.int32), GRID_W)
    cols = jnp.tile(jnp.arange(GRID_W, dtype=jnp.int32), n_rows)
    silu_c = jax.nn.silu(c)
    silu_cc = jax.nn.silu(c_ctx)
    for l in range(DEPTH):
        update_ctx = l < DEPTH - 1
        mod_lat = silu_c @ w_mod[l] + b_mod[l]
        mod_ctx = silu_cc @ w_mod[l] + b_mod[l]
        sh1, sc1, g1, sh2, sc2, g2 = jnp.split(mod_lat[:, None, :], MOD_CHUNKS, axis=-1)
        csh1, csc1, cg1, csh2, csc2, cg2 = jnp.split(mod_ctx, MOD_CHUNKS, axis=-1)

        h_lat = modulate(rmsnorm(x, norm1_w[l]), sh1, sc1)
        h_ctx = modulate(rmsnorm(ctx, norm1_w[l]), csh1, csc1)
        y_lat, y_ctx = token_mixer(h_lat, h_ctx, rows, cols, w_in[l], q_norm_w[l], k_norm_w[l], conv_w[l],
                                   a_log[l], dt_bias[l], o_norm_w[l], w_out[l], update_ctx)
        x = x + g1 * y_lat
        h2 = modulate(rmsnorm(x, norm2_w[l]), sh2, sc2)
        x = x + g2 * expert_choice_ffn(h2, router_w[l], w_gate[l], w_up[l], w_down[l])

        if update_ctx:
            ctx = ctx + cg1 * y_ctx
            h2c = modulate(rmsnorm(ctx, norm2_w[l]), csh2, csc2)
            ctx = ctx + cg2 * expert_choice_ffn(h2c, router_w[l], w_gate[l], w_up[l], w_down[l])
    return x
```

```python
import os
from contextlib import ExitStack
import numpy as np
import concourse.bass as bass
import concourse.mybir as mybir
from concourse.bass_utils import run_bass_kernel_spmd

F32 = mybir.dt.float32
BF16 = mybir.dt.bfloat16
U8 = mybir.dt.uint8
AF = mybir.ActivationFunctionType
ALU = mybir.AluOpType
AX = mybir.AxisListType

NDMA = 24
D = 1024
SEQ = 2048
CTX = 256
T = SEQ + CTX
NT = T // 128
INW = 2832
EPS = 1e-6
NE = 16
CAP = 256


def _is_psum_key(k):
    n = k if isinstance(k, str) else k[0]
    return isinstance(n, str) and (n.startswith('pb') or n in ('pT', 'pm', 'acc', 'rowbank'))


class Sched:
    def __init__(self, nc, stack):
        self.nc = nc
        self.eng = {'pe': nc.tensor, 'act': nc.scalar, 'dve': nc.vector, 'pool': nc.gpsimd, 'sp': nc.sync}
        self.prog = {e: [] for e in self.eng}
        self.sem = {}
        self.cnt = {}
        for e in self.eng:
            self.sem[e] = stack.enter_context(nc.semaphore('s_' + e))
            self.cnt[e] = 0
        self.dsem = [stack.enter_context(nc.semaphore('d%d' % i)) for i in range(NDMA)]
        self.dcnt = [0] * NDMA
        self.dnext = 0
        self.seen = {e: {} for e in self.eng}
        self.lastw = {}
        self.readers = {}

    def _wait(self, e, tok):
        k, v = tok
        if self.seen[e].get(k, 0) >= v:
            return
        self.seen[e][k] = v
        self.prog[e].append(('wait', k, v))

    def _deps(self, e, reads, writes, pe_accum=False):
        best = {}

        def add(t):
            if best.get(t[0], 0) < t[1]:
                best[t[0]] = t[1]
        for r in reads:
            t = self.lastw.get(r)
            if t is not None:
                add(t)
            if _is_psum_key(r):
                for t in self.readers.get(r, ()):
                    if t[0] != e:
                        add(t)
        for w in writes:
            t = self.lastw.get(w)
            if t is not None and not (pe_accum and t[0] == 'pe'):
                add(t)
            for t in self.readers.get(w, ()):
                add(t)
        for k, v in best.items():
            self._wait(e, (k, v))

    def _commit(self, tok, reads, writes):
        for r in reads:
            lst = self.readers.setdefault(r, [])
            lst[:] = [t for t in lst if t[0] != tok[0]]
            lst.append(tok)
        for w in writes:
            self.lastw[w] = tok
            self.readers[w] = []

    def op(self, e, fn, reads=(), writes=(), pe_accum=False):
        self._deps(e, reads, writes, pe_accum)
        self.cnt[e] += 1
        self.prog[e].append(('op', fn, self.cnt[e]))
        tok = (e, self.cnt[e])
        self._commit(tok, reads, writes)
        return tok

    def dma(self, q, out, in_, reads=(), writes=(), **kw):
        self._deps(q, reads, writes)
        i = self.dnext
        self.dnext = (self.dnext + 1) % NDMA
        if self.dcnt[i] > 0:
            self._wait(q, (('d', i), self.dcnt[i]))
        self.dcnt[i] += 16
        so = self.dsem[i]
        eo = self.eng[q]
        self.prog[q].append(('dma', lambda: eo.dma_start(out=out, in_=in_, **kw).then_inc(so, 16)))
        tok = (('d', i), self.dcnt[i])
        self._commit(tok, reads, writes)
        return tok

    def barrier(self):
        for e in self.eng:
            for i in range(NDMA):
                if self.dcnt[i] > 0:
                    self._wait(e, (('d', i), self.dcnt[i]))
            for e2 in ('pe', 'act', 'dve', 'pool'):
                if self.cnt[e2] > 0:
                    self._wait(e, (e2, self.cnt[e2]))
        self.lastw = {}
        self.readers = {}

    def finish(self):
        nc = self.nc
        for i in range(NDMA):
            if self.dcnt[i] > 0:
                self._wait('sp', (('d', i), self.dcnt[i]))
        for e in ('pe', 'act', 'dve', 'pool'):
            if self.cnt[e] > 0:
                self._wait('sp', (e, self.cnt[e]))
        needed = {e: set() for e in self.eng}
        for e in self.eng:
            for it in self.prog[e]:
                if it[0] == 'wait' and isinstance(it[1], str):
                    needed[it[1]].add(it[2])
        remap = {e: {v: i + 1 for i, v in enumerate(sorted(needed[e]))} for e in self.eng}
        self.maxsem = {e: len(needed[e]) for e in self.eng}

        def emit(e):
            eo = self.eng[e]
            for it in self.prog[e]:
                if it[0] == 'wait':
                    k, v = it[1], it[2]
                    if isinstance(k, str):
                        eo.wait_ge(self.sem[k], remap[k][v])
                    else:
                        eo.wait_ge(self.dsem[k[1]], v)
                elif it[0] == 'op':
                    ins = it[1]()
                    if it[2] in remap[e]:
                        ins.then_inc(self.sem[e], 1)
                else:
                    it[1]()

        with nc.Block() as block:
            @block.sync
            def _(eng):
                emit('sp')

            @block.tensor
            def _(eng):
                emit('pe')

            @block.scalar
            def _(eng):
                emit('act')

            @block.vector
            def _(eng):
                emit('dve')

            @block.gpsimd
            def _(eng):
                emit('pool')


class Arena:
    def __init__(self, ar, nbytes):
        self.ar = ar
        self.n = nbytes
        self.off = 0

    def alloc(self, shape, dtype):
        es = 2 if dtype == BF16 else 4
        per = int(np.prod(shape[1:])) * es
        off = (self.off + 63) // 64 * 64
        assert off + per <= getattr(self, 'top', self.n), ("arena overflow", off, per, getattr(self, 'top', self.n))
        self.off = off + per
        v = self.ar[:, off:off + per].bitcast(dtype)
        if len(shape) == 3:
            v = v.rearrange("p (a b) -> p a b", b=shape[2])
        elif len(shape) == 4:
            v = v.rearrange("p (a b c) -> p a b c", b=shape[2], c=shape[3])
        if shape[0] < 128:
            v = v[0:shape[0]]
        return v

    def alloc_top(self, shape, dtype):
        es = 2 if dtype == BF16 else 4
        per = int(np.prod(shape[1:])) * es
        top = getattr(self, 'top', self.n)
        off = (top - per) // 64 * 64
        assert off >= self.off, ("arena top overflow", off, self.off)
        self.top = off
        v = self.ar[:, off:off + per].bitcast(dtype)
        if len(shape) == 3:
            v = v.rearrange("p (a b) -> p a b", b=shape[2])
        if shape[0] < 128:
            v = v[0:shape[0]]
        return v

    def release_top(self):
        self.top = self.n

    def mark(self):
        return self.off

    def release(self, m):
        self.off = m


def host_consts():
    c = {}
    c["ident"] = np.eye(128, dtype=np.float32)
    p = np.arange(128)
    t = np.arange(SEQ)
    d = p % 64
    is_row = d < 32
    fi = np.where(is_row, d % 16, (d - 32) % 16).astype(np.float32)
    freqs = (np.float32(10000.0) ** (-fi / np.float32(16.0))).astype(np.float32)
    pos = np.where(is_row[:, None], (t // 64)[None, :], (t % 64)[None, :]).astype(np.float32)
    ang = (pos * freqs[:, None]).astype(np.float32)
    c["cosT"] = np.cos(ang).astype(np.float32)
    c["sinT"] = np.sin(ang).astype(np.float32)
    rm = np.zeros((128, 128), np.float32)
    for q in range(128):
        if (q % 32) < 16:
            rm[q + 16, q] = -1.0
        else:
            rm[q - 16, q] = 1.0
    c["rotm"] = rm
    c["blk64"] = (p[:, None] // 64 == p[None, :] // 64).astype(np.float32)
    same = c["blk64"]
    r, cc = p[:, None], p[None, :]
    uinc = (r <= cc) * same
    lstr = (r > cc) * same
    linc = (r >= cc) * same
    ustr = (r < cc) * same
    msx = np.zeros((2, 128, 130), np.float32)
    msx[0, :, :128] = lstr
    msx[1, :, :128] = ustr
    msx[:, :, 128] = 1.0
    c["dnm"] = np.stack([uinc, lstr, linc, ustr]).astype(np.float32)
    c["msx"] = msx
    sel = np.zeros((2, 128, 128), np.float32)
    sel[0, 0, :] = 1.0
    sel[1, 64, :] = 1.0
    c["selrow"] = sel
    c["iota256"] = np.tile(np.arange(256, dtype=np.float32)[None, :], (128, 1))
    c["iotap"] = np.stack([p, p + 128], axis=1).astype(np.float32)
    oh = np.zeros((16, 16, 128), np.float32)
    for e in range(16):
        oh[e, e, :] = 1.0
    c["oh16"] = oh
    return c


CONST_SHAPES = {"ident": [128, 128], "cosT": [128, SEQ], "sinT": [128, SEQ], "rotm": [128, 128], "blk64": [128, 128],
                "dnm": [4, 128, 128], "msx": [2, 128, 130], "selrow": [2, 128, 128], "iota256": [128, 256],
                "iotap": [128, 2], "oh16": [16, 16, 128]}

IN_SHAPES = {"x": [SEQ, D], "ctx": [CTX, D], "cT": [128, 16], "w_mod": [D, 6 * D], "bmrow": [2, 6 * D], "n2row": [2, D], "n12T": [128, 32],
             "w_in": [D, INW], "qkw": [128, 2], "convT": [128, 60], "alog": [128, NT * 8], "dtb": [128, NT * 8],
             "onw": [128, 128], "w_out": [D, D], "router_w": [D, NE], "w_gate": [NE, D, D], "w_up": [NE, D, D],
             "w_down": [NE, D, D]}


def build_program(stop_after=99, dbg=None):
    nc = bass.Bass("TRN2", target_bir_lowering=False)
    dr = {}
    for k, s in list(IN_SHAPES.items()) + list(CONST_SHAPES.items()):
        dr[k] = nc.dram_tensor(k, s, F32, kind="ExternalInput").ap()
    out = nc.dram_tensor("out", [SEQ, D], F32, kind="ExternalOutput").ap()
    x1s = nc.dram_tensor("x1s", [SEQ, D], F32).ap()
    dbg_t = {}
    if dbg:
        for k, s in dbg.items():
            dbg_t[k] = nc.dram_tensor("dbg_" + k, s, F32, kind="ExternalOutput").ap()

    with ExitStack() as st:
        S = Sched(nc, st)
        V, A, P, TE = nc.vector, nc.scalar, nc.gpsimd, nc.tensor
        ARN = 206 * 1024
        ar = st.enter_context(nc.sbuf_tensor("arena", [128, ARN], U8))
        AR = Arena(ar, ARN)
        pb = [st.enter_context(nc.psum_tensor("pb%d" % i, [128, 512], F32))[:] for i in range(8)]
        pbt = [pb[6 + i].bitcast(BF16).rearrange("p (a b) -> p a b", b=128) for i in range(2)]

        def dve(fn, r, w):
            return S.op('dve', fn, r, w)

        def act(fn, r, w):
            return S.op('act', fn, r, w)

        def pool(fn, r, w):
            return S.op('pool', fn, r, w)

        def mm(o, l, r_, start, stop, rd, wr):
            return S.op('pe', lambda: TE.matmul(o, lhsT=l, rhs=r_, start=start, stop=stop), rd, wr, pe_accum=not start)

        def tr(o, i, idn, rd, wr):
            return S.op('pe', lambda: TE.transpose(out=o, in_=i, identity=idn), rd, wr, pe_accum=True)

        def dump(name, src_ap, rd):
            if name in dbg_t:
                S.dma('sp', dbg_t[name], src_ap, reads=rd, writes=['dbg_' + name])

        ident = AR.alloc([128, 128], F32)
        identb = AR.alloc([128, 128], BF16)
        onesf = AR.alloc([128, 128], F32)
        onesb = AR.alloc([128, 128], BF16)
        rotb = AR.alloc([128, 128], BF16)
        blk64b = AR.alloc([128, 128], BF16)
        blk64f = AR.alloc([128, 128], F32)
        dnm = AR.alloc([128, 4, 128], F32)
        msx = AR.alloc([128, 2, 130], F32)
        selrow = AR.alloc([128, 2, 128], F32)
        iota256 = AR.alloc([128, 256], F32)
        iotap = AR.alloc([128, 2], F32)
        oh16 = AR.alloc([16, 16, 128], BF16)
        mT = AR.mark()
        tmpc = AR.alloc([128, 2048], F32)
        S.dma('sp', ident, dr["ident"], writes=['ident'])
        S.dma('sp', blk64f, dr["blk64"], writes=['blk64f'])
        S.dma('sp', dnm, dr["dnm"].rearrange("a p n -> p a n"), writes=['dnm'])
        S.dma('sp', msx, dr["msx"].rearrange("a p n -> p a n"), writes=['msx'])
        S.dma('sp', selrow, dr["selrow"].rearrange("a p n -> p a n"), writes=['selrow'])
        S.dma('sp', iota256, dr["iota256"], writes=['iota256'])
        S.dma('sp', iotap, dr["iotap"], writes=['iotap'])
        S.dma('sp', tmpc[:, 0:128], dr["rotm"], writes=['tmpc'])
        dve(lambda: V.tensor_copy(out=rotb, in_=tmpc[:, 0:128]), ['tmpc'], ['rotb'])
        dve(lambda: V.tensor_copy(out=identb, in_=ident), ['ident'], ['identb'])
        dve(lambda: V.tensor_copy(out=blk64b, in_=blk64f), ['blk64f'], ['blk64b'])
        pool(lambda: P.memset(onesf, 1.0), [], ['onesf'])
        pool(lambda: P.memset(onesb, 1.0), [], ['onesb'])
        S.dma('sp', tmpc[0:16, :], dr["oh16"].rearrange("k e m -> k (e m)"), reads=['rotb'], writes=['tmpc'])
        dve(lambda: V.tensor_copy(out=oh16, in_=tmpc[0:16, :].rearrange("k (e m) -> k e m", m=128)), ['tmpc'], ['oh16'])

        S.barrier()
        AR.release(mT)
        modv = AR.alloc([128, 96], F32)
        s1 = AR.alloc([128, 16], F32)
        s2 = AR.alloc([128, 16], F32)
        n12 = AR.alloc([128, 32], F32)
        g1row = AR.alloc([128, 1024], F32)
        g2row = AR.alloc([128, 1024], F32)
        s2row = AR.alloc([128, 1024], F32)
        b2row = AR.alloc([128, 1024], F32)
        mA = AR.mark()
        if os.environ.get('ARDBG'): print('A persistent end', AR.off)
        cT = AR.alloc([128, 16], F32)
        sil = AR.alloc([128, 16], F32)
        wm = [AR.alloc([128, 8, 1024], F32) for _ in range(2)]
        bmr = AR.alloc([2, 6144], F32)
        modrow = AR.alloc([2, 6144], F32)
        n2r = AR.alloc([2, 1024], F32)
        s2r = AR.alloc([2, 1024], F32)
        sel01 = AR.alloc([2, 128], F32)
        S.dma('sp', cT, dr["cT"], writes=['cT'])
        S.dma('sp', n12, dr["n12T"], writes=['n12'])
        S.dma('sp', bmr, dr["bmrow"], writes=['bmr'])
        S.dma('sp', n2r, dr["n2row"], writes=['n2r'])
        pool(lambda: P.memset(sel01, 0.0), [], ['sel01'])
        pool(lambda: P.memset(sel01[0:1, :], 1.0), ['sel01'], ['sel01'])
        act(lambda: A.activation(out=sil, in_=cT, func=AF.Silu), ['cT'], ['sil'])
        wmv = dr["w_mod"].rearrange("(kc p) n -> p kc n", p=128)
        for j in range(6):
            buf = wm[j % 2]
            for hh in range(2):
                S.dma('sp' if hh == 0 else 'act', buf[:, 4 * hh:4 * hh + 4, :], wmv[:, 4 * hh:4 * hh + 4, j * 1024:(j + 1) * 1024],
                      writes=[('wm', j % 2, hh)])
            for half in range(2):
                bi = (2 * j + half) % 4
                bank = pb[bi]
                for kc in range(8):
                    mm(bank[0:2, :], sil[:, 2 * kc:2 * kc + 2], buf[:, kc, half * 512:(half + 1) * 512], kc == 0, kc == 7,
                       [('wm', j % 2, kc // 4), 'sil'], [('pbm', bi)])
                c0 = j * 1024 + half * 512
                dve(lambda bank=bank, c0=c0: V.tensor_tensor(out=modrow[:, c0:c0 + 512], in0=bank[0:2, :], in1=bmr[:, c0:c0 + 512], op=ALU.add),
                    [('pbm', bi), 'bmr'], [('modrow', j, half)])
        pm = pb[4]
        for idx in range(16):
            tr(pm[:, idx * 2:idx * 2 + 2], modrow[0:2, idx * 128:(idx + 1) * 128], ident[0:2, 0:2],
               [('modrow', idx // 8, (idx % 8) // 4), 'ident'], ['pm'])
        dve(lambda: V.tensor_copy(out=modv[:, 0:32], in_=pm[:, 0:32]), ['pm'], ['modv'])
        dve(lambda: V.scalar_tensor_tensor(out=s1, in0=modv[:, 16:32], scalar=1.0, in1=n12[:, 0:16], op0=ALU.add, op1=ALU.mult),
            ['modv', 'n12'], ['s1'])
        dve(lambda: V.scalar_tensor_tensor(out=s2r, in0=modrow[:, 4096:5120], scalar=1.0, in1=n2r, op0=ALU.add, op1=ALU.mult),
            [('modrow', 4, 0), ('modrow', 4, 1), 'n2r'], ['s2r'])
        rows = [(g1row, modrow, 2048, [('modrow', 2, 0), ('modrow', 2, 1)], 'g1row'), (g2row, modrow, 5120, [('modrow', 5, 0), ('modrow', 5, 1)], 'g2row'),
                (s2row, s2r, 0, ['s2r'], 's2row'), (b2row, modrow, 3072, [('modrow', 3, 0), ('modrow', 3, 1)], 'b2row')]
        ri = 0
        for (dst, src, base, skeys, key) in rows:
            for half in range(2):
                bank = pb[5 + ri % 2]
                mm(bank, sel01[0:2, :], src[0:2, base + half * 512:base + (half + 1) * 512], True, True, skeys + ['sel01'], [('rowbank', ri % 2)])
                act(lambda dst=dst, bank=bank, half=half: A.copy(out=dst[:, half * 512:(half + 1) * 512], in_=bank),
                    [('rowbank', ri % 2)], [key])
                ri += 1
        dump("modv", modv, ['modv'])
        dump("g1row", g1row, ['g1row'])
        S.barrier()
        AR.release(mA)
        mDN0 = AR.mark()
        if stop_after <= 0:
            S.finish()
            return nc

        hT = AR.alloc_top([128, 8, T], BF16)
        mB = AR.mark()
        xin = [AR.alloc([128, 1024], F32) for _ in range(3)]
        junk = AR.alloc([128, 1024], F32)
        xn = [AR.alloc([128, 1024], BF16) for _ in range(2)]
        ssq = AR.alloc([128, NT], F32)
        rstd = AR.alloc([128, NT], F32)

        def b_stats(ti):
            src = dr["ctx"][ti * 128:(ti + 1) * 128, :] if ti < 2 else dr["x"][(ti - 2) * 128:(ti - 1) * 128, :]
            xb, xnb = xin[ti % 3], xn[ti % 2]
            S.dma('sp', xb, src, writes=[('xin', ti % 3)])
            act(lambda xb=xb, ti=ti: A.activation(out=junk, in_=xb, func=AF.Square, accum_out=ssq[:, ti:ti + 1]),
                [('xin', ti % 3)], ['junk', ('ssq', ti)])
            dve(lambda ti=ti: V.tensor_scalar(out=rstd[:, ti:ti + 1], in0=ssq[:, ti:ti + 1], scalar1=1.0 / D, scalar2=EPS,
                                              op0=ALU.mult, op1=ALU.add), [('ssq', ti)], [('rstd', ti)])
            act(lambda ti=ti: A.activation(out=rstd[:, ti:ti + 1], in_=rstd[:, ti:ti + 1], func=AF.Sqrt), [('rstd', ti)], [('rstd', ti)])
            dve(lambda ti=ti: V.reciprocal(out=rstd[:, ti:ti + 1], in_=rstd[:, ti:ti + 1]), [('rstd', ti)], [('rstd', ti)])
            dve(lambda xb=xb, xnb=xnb, ti=ti: V.tensor_scalar(out=xnb, in0=xb, scalar1=rstd[:, ti:ti + 1], scalar2=None, op0=ALU.mult),
                [('xin', ti % 3), ('rstd', ti)], [('xn', ti % 2)])

        def b_transpose(ti):
            t = 1 if ti < 2 else 0
            xnb = xn[ti % 2]
            bv = pbt[ti % 2]
            for kc in range(8):
                tr(bv[:, kc, :], xnb[:, kc * 128:(kc + 1) * 128], identb, [('xn', ti % 2), 'identb'], [('pT', ti % 2)])
            for kc in range(8):
                o_ = hT[:, kc, ti * 128:(ti + 1) * 128]
                sc_ = s1[:, kc * 2 + t:kc * 2 + t + 1]
                bi_ = modv[:, kc * 2 + t:kc * 2 + t + 1]
                act(lambda o_=o_, kc=kc, bv=bv, sc_=sc_, bi_=bi_: A.activation(out=o_, in_=bv[:, kc, :], func=AF.Identity, scale=sc_, bias=bi_),
                    [('pT', ti % 2), 's1', 'modv'], [('hT', ti, kc)])

        b_stats(0)
        for ti in range(NT):
            if ti + 1 < NT:
                b_stats(ti + 1)
            b_transpose(ti)
        if "hT" in dbg_t:
            hTf = AR.alloc([128, 8, 256], F32)
            dve(lambda: V.tensor_copy(out=hTf, in_=hT[:, :, 128:384]), [('hT', a, b) for a in (1, 2) for b in range(8)], ['hTf'])
            dump("hT", hTf, ['hTf'])
        S.barrier()
        AR.release(mB)
        if stop_after <= 1:
            S.finish()
            return nc

        attT = AR.alloc([128, 4, SEQ], BF16)
        mQ = AR.mark()
        if os.environ.get('ARDBG'): print('attT end', AR.off)
        qT = AR.alloc([128, 4, SEQ], BF16)
        kTd = AR.alloc([128, 2, T], BF16)
        vext = AR.alloc([128, NT, 2, 66], BF16)
        mC1 = AR.mark()
        cosT = AR.alloc([128, SEQ], F32)
        sinT = AR.alloc([128, SEQ], F32)
        qkw = AR.alloc([128, 2], F32)
        stg = [AR.alloc([128, 8, 128], F32) for _ in range(2)]
        wpc = [AR.alloc([128, 8, 128], BF16) for _ in range(2)]
        sqb = [AR.alloc([128, 512], BF16) for _ in range(2)]
        rq = [AR.alloc([128, 512], F32) for _ in range(2)]
        qnb = [AR.alloc([128, 512], BF16) for _ in range(2)]
        t1 = [AR.alloc([128, 512], F32) for _ in range(2)]
        t2 = [AR.alloc([128, 512], F32) for _ in range(2)]
        S.dma('sp', cosT, dr["cosT"], writes=['cosT'])
        S.dma('act', sinT, dr["sinT"], writes=['sinT'])
        S.dma('sp', qkw, dr["qkw"], writes=['qkw'])
        pool(lambda: P.memset(vext, 0.0), [], ['vext'])
        pool(lambda: P.memset(vext[:, :, :, 64:65], 1.0), ['vext'], ['vext1'])
        winv = dr["w_in"].rearrange("(kc p) n -> p kc n", p=128)
        wctr = [0]

        def load_w(col0, ncols=128, dup=False):
            ci = wctr[0] % 2
            wctr[0] += 1
            sg, wp = stg[ci], wpc[ci]
            sk = ('stg', 0 if stg[0] is stg[1] else ci)
            S.dma('sp' if ci == 0 else 'act', sg[:, :, 0:ncols], winv[:, :, col0:col0 + ncols], writes=[sk])
            if dup:
                pool(lambda: P.tensor_copy(out=wp[:, :, 0:64], in_=sg[:, :, 0:64]), [sk], [('wpc', ci)])
                pool(lambda: P.tensor_copy(out=wp[:, :, 64:128], in_=sg[:, :, 0:64]), [sk, ('wpc', ci)], [('wpc', ci)])
            else:
                pool(lambda: P.tensor_copy(out=wp[:, :, 0:ncols], in_=sg[:, :, 0:ncols]), [sk], [('wpc', ci)])
            return wp, ('wpc', ci)

        ectr = [0]

        def qk_epilogue(ps, pskey, n, wcol, rope_t0, dst, dstkey):
            i = ectr[0] % 2
            ectr[0] += 1
            act(lambda: A.activation(out=sqb[i][:, 0:n], in_=ps[:, 0:n], func=AF.Square), [pskey], [('sqb', i)])
            mm(pb[2][:, 0:n], blk64b, sqb[i][:, 0:n], True, True, [('sqb', i), 'blk64b'], ['pb2'])
            act(lambda: A.activation(out=rq[i][:, 0:n], in_=pb[2][:, 0:n], func=AF.Sqrt, scale=1.0 / 64, bias=EPS), ['pb2'], [('rq', i)])
            dve(lambda: V.reciprocal(out=rq[i][:, 0:n], in_=rq[i][:, 0:n]), [('rq', i)], [('rq', i)])
            if rope_t0 is None:
                dve(lambda: V.scalar_tensor_tensor(out=dst, in0=ps[:, 0:n], scalar=qkw[:, wcol:wcol + 1], in1=rq[i][:, 0:n],
                                                   op0=ALU.mult, op1=ALU.mult), [pskey, 'qkw', ('rq', i)], [dstkey])
                return
            dve(lambda: V.scalar_tensor_tensor(out=qnb[i][:, 0:n], in0=ps[:, 0:n], scalar=qkw[:, wcol:wcol + 1], in1=rq[i][:, 0:n],
                                               op0=ALU.mult, op1=ALU.mult), [pskey, 'qkw', ('rq', i)], [('qnb', i)])
            mm(pb[3][:, 0:n], rotb, qnb[i][:, 0:n], True, True, [('qnb', i), 'rotb'], ['pb3'])
            pool(lambda: P.tensor_tensor(out=t1[i][:, 0:n], in0=qnb[i][:, 0:n], in1=cosT[:, rope_t0:rope_t0 + n], op=ALU.mult),
                 [('qnb', i), 'cosT'], [('t1', i)])
            dve(lambda: V.tensor_tensor(out=t2[i][:, 0:n], in0=pb[3][:, 0:n], in1=sinT[:, rope_t0:rope_t0 + n], op=ALU.mult),
                ['pb3', 'sinT'], [('t2', i)])
            dve(lambda: V.tensor_tensor(out=dst, in0=t1[i][:, 0:n], in1=t2[i][:, 0:n], op=ALU.add), [('t1', i), ('t2', i)], [dstkey])

        NQB = int(os.environ.get('NQB', 4))
        pctr = [0]
        for j in range(4):
            wp, wk = load_w(128 * j)
            for b in range(NQB):
                bi = pctr[0] % 2
                pctr[0] += 1
                t0 = CTX + 512 * b
                for kc in range(8):
                    mm(pb[bi], wp[:, kc, :], hT[:, kc, t0:t0 + 512], kc == 0, kc == 7, [wk] + [('hT', ti_, kc) for ti_ in range(t0 // 128, t0 // 128 + 4)],
                       [('pbq', bi)])
                qk_epilogue(pb[bi], ('pbq', bi), 512, 0, 512 * b, qT[:, j, 512 * b:512 * (b + 1)], ('qT', j, b))
        for g in range(2):
            wp, wk = load_w(512 + 64 * g, 64, dup=True)
            for b in range(-1, NQB):
                bi = pctr[0] % 2
                pctr[0] += 1
                t0, n = (0, 256) if b < 0 else (CTX + 512 * b, 512)
                for kc in range(8):
                    mm(pb[bi][:, 0:n], wp[:, kc, :], hT[:, kc, t0:t0 + n], kc == 0, kc == 7,
                       [wk] + [('hT', ti_, kc) for ti_ in range(t0 // 128, (t0 + n) // 128)], [('pbq', bi)])
                qk_epilogue(pb[bi], ('pbq', bi), n, 1, None if b < 0 else 512 * b, kTd[:, g, t0:t0 + n], ('kTd', g, b))
        wp, wk = load_w(640)
        for ti in range(NT):
            bi = 4 + ti % 2
            for kc in range(8):
                mm(pb[bi][:, 0:128], hT[:, kc, ti * 128:(ti + 1) * 128], wp[:, kc, :], kc == 0, kc == 7, [wk, ('hT', ti, kc)], [('pbv', bi)])
            act(lambda ti=ti, bi=bi: A.copy(out=vext[:, ti, :, 0:64], in_=pb[bi][:, 0:128].rearrange("p (g d) -> p g d", d=64)),
                [('pbv', bi), 'vext'], [('vx', ti)])
        if "qT" in dbg_t:
            dq = AR.alloc([128, 4, 512], F32)
            dve(lambda: V.tensor_copy(out=dq, in_=qT[:, :, 0:512]), [('qT', j_, 0) for j_ in range(4)], ['dq'])
            dump("qT", dq, ['dq'])
        if "kT" in dbg_t:
            dk = AR.alloc([128, 2, 512], F32)
            dve(lambda: V.tensor_copy(out=dk, in_=kTd[:, :, 0:512]), [('kTd', g_, b_) for g_ in range(2) for b_ in (-1, 0)], ['dk'])
            dump("kT", dk, ['dk'])
        if "vext" in dbg_t:
            dv = AR.alloc([128, 2, 132], F32)
            dve(lambda: V.tensor_copy(out=dv, in_=vext[:, 1:3, :, :].rearrange("p a g d -> p a (g d)")), [('vx', 1), ('vx', 2), 'vext1'], ['dv'])
            dump("vext", dv, ['dv'])
        S.barrier()
        AR.release(mC1)
        if stop_after <= 2:
            S.finish()
            return nc

        mD = AR.mark()
        NSB = 3
        pbuf = [AR.alloc([128, 512], BF16) for _ in range(NSB)]
        rsb = AR.alloc([128, 512], F32)
        bcs = AR.alloc([64, 512], F32)
        NKT = int(os.environ.get('NKT', NT))
        pairs = [(j, qb) for j in range(int(os.environ.get('NJ', 4))) for qb in range(NQB)]
        its = [(p, kt, half) for p in range(len(pairs)) for kt in range(NKT) for half in range(2)]
        sbank = [pb[0], pb[1], pb[3]]

        def acc_of(p, half):
            return 4 + 2 * (p % 2) + half

        def emit_score(n):
            p, kt, half = its[n]
            j, qb = pairs[p]
            hp = slice(0, 64) if half == 0 else slice(64, 128)
            si = n % 3
            mm(sbank[si], kTd[hp, j // 2, kt * 128:(kt + 1) * 128], qT[hp, j, qb * 512:(qb + 1) * 512], True, True, [], [('pbs', si)])

        def emit_exp(n):
            si = n % 3
            act(lambda si=si: A.activation(out=pbuf[si], in_=sbank[si], func=AF.Exp, scale=0.125), [('pbs', si)], [('pbuf', si)])

        def emit_pv(n):
            p, kt, half = its[n]
            j, qb = pairs[p]
            si = n % 3
            ai = acc_of(p, half)
            mm(pb[ai][0:65, :], vext[:, kt, j // 2, 0:65], pbuf[si], kt == 0, kt == NKT - 1, [('pbuf', si)], [('acc', ai)])

        def emit_final(p, half):
            j, qb = pairs[p]
            hp = slice(0, 64) if half == 0 else slice(64, 128)
            ai = acc_of(p, half)
            acc = pb[ai]
            dve(lambda: V.reciprocal(out=rsb[64:65, :], in_=acc[64:65, :]), [('acc', ai)], ['rsb'])
            mm(pb[2][0:64, :], onesf[64:65, 0:64], rsb[64:65, :], True, True, ['rsb'], ['pb2'])
            act(lambda: A.copy(out=bcs, in_=pb[2][0:64, :]), ['pb2'], ['bcs'])
            dve(lambda: V.tensor_tensor(out=attT[hp, j, qb * 512:(qb + 1) * 512], in0=acc[0:64, :], in1=bcs, op=ALU.mult),
                [('acc', ai), 'bcs'], [('attT', j, half, qb)])

        NIT = len(its)
        for n in range(min(2, NIT)):
            emit_score(n)
        pending = []
        for m in range(NIT // 2):
            n0, n1 = 2 * m, 2 * m + 1
            if n0 + 2 < NIT:
                emit_score(n0 + 2)
            emit_exp(n0)
            if n1 + 2 < NIT:
                emit_score(n1 + 2)
            emit_pv(n0)
            emit_exp(n1)
            emit_pv(n1)
            p, kt, _ = its[n0]
            if pending and kt == min(3, NKT - 1):
                for (pp, hh) in pending:
                    emit_final(pp, hh)
                pending = []
            if kt == NKT - 1:
                for (pp, hh) in pending:
                    emit_final(pp, hh)
                pending = [(p, 0), (p, 1)]
        for (pp, hh) in pending:
            emit_final(pp, hh)
        if "attT" in dbg_t:
            da = AR.alloc([128, 4, 512], F32)
            dve(lambda: V.tensor_copy(out=da, in_=attT[:, :, 0:512]), [('attT', j_, h_, 0) for j_ in range(4) for h_ in range(2)], ['da'])
            dump("attT", da, ['da'])
        S.barrier()
        AR.release(mQ)
        if stop_after <= 3:
            S.finish()
            return nc

        zt = AR.alloc([128, 16, 512], BF16)
        dnoT = AR.alloc([128, 4, SEQ], BF16)
        mDN = AR.mark()
        dnT = AR.alloc([128, 12, T], BF16)
        gb = AR.alloc([128, NT, 16], F32)
        nbeta = AR.alloc([128, NT, 8], F32)
        mC2 = AR.mark()
        if os.environ.get('ARDBG'): print('C2 persistent end', AR.off, 'top', getattr(AR, 'top', None))
        stg = [AR.alloc([128, 8, 128], F32)] * 2
        wpc = [AR.alloc([128, 8, 128], BF16) for _ in range(2)]
        convw = AR.alloc([128, 60], F32)
        rawp = [AR.alloc([128, 2312], BF16)] * 2
        cx = [AR.alloc([128, 512], F32) for _ in range(4)]
        sqd = [AR.alloc([128, 512], BF16)] * 2
        rq2 = [AR.alloc([128, 512], F32) for _ in range(2)]
        gbraw = AR.alloc([128, NT, 16], F32)
        alog = AR.alloc([128, NT, 8], F32)
        dtb = AR.alloc([128, NT, 8], F32)
        S.dma('sp', convw, dr["convT"], writes=['convw'])
        S.dma('sp', alog, dr["alog"].rearrange("p (a b) -> p a b", b=8), writes=['alog'])
        S.dma('sp', dtb, dr["dtb"].rearrange("p (a b) -> p a b", b=8), writes=['dtb'])
        pool(lambda: P.memset(rawp[0], 0.0), [], [('rawp', 0)])
        cctr = [0]
        for m in range(int(os.environ.get('NDN', 12))):
            wp, wk = load_w(768 + 128 * m)
            rp = rawp[0]
            for b in range(-1, 4):
                t0, n = (0, 256) if b < 0 else (CTX + 512 * b, 512)
                off = 2 if b < 0 else 262 + 512 * b
                bi = pctr[0] % 2
                pctr[0] += 1
                for kc in range(8):
                    mm(pb[bi][:, 0:n], wp[:, kc, :], hT[:, kc, t0:t0 + n], kc == 0, kc == 7, [wk], [('pbq', bi)])
                act(lambda rp=rp, off=off, n=n, bi=bi: A.copy(out=rp[:, off:off + n], in_=pb[bi][:, 0:n]), [('pbq', bi)], [('rawp', 0)])

            def conv_front(m, b, ci):
                t0, n = (0, 256) if b < 0 else (CTX + 512 * b, 512)
                off = 2 if b < 0 else 262 + 512 * b
                acc_ = cx[ci][:, 0:n]
                ck = ('cx', ci)
                for jt in range(5):
                    src_ = rp[:, off + jt - 2:off + jt - 2 + n]
                    wc_ = convw[:, m * 5 + jt:m * 5 + jt + 1]
                    if jt == 0:
                        dve(lambda acc_=acc_, src_=src_, wc_=wc_: V.tensor_scalar(out=acc_, in0=src_, scalar1=wc_, scalar2=None, op0=ALU.mult),
                            [('rawp', 0), 'convw'], [ck])
                    else:
                        dve(lambda acc_=acc_, src_=src_, wc_=wc_: V.scalar_tensor_tensor(out=acc_, in0=src_, scalar=wc_, in1=acc_, op0=ALU.mult, op1=ALU.add),
                            [('rawp', 0), 'convw', ck], [ck])
                dst = dnT[:, m, t0:t0 + n]
                if m >= 8:
                    act(lambda dst=dst, acc_=acc_: A.activation(out=dst, in_=acc_, func=AF.Silu), [ck], [('dnT', m, b)])
                    return
                ni = ci % 2
                act(lambda acc_=acc_: A.activation(out=acc_, in_=acc_, func=AF.Silu), [ck], [ck])
                pool(lambda acc_=acc_, n=n: P.tensor_tensor(out=sqd[0][:, 0:n], in0=acc_, in1=acc_, op=ALU.mult), [ck], [('sqd', 0)])
                mm(pb[4 + ni][:, 0:n], onesb, sqd[0][:, 0:n], True, True, [('sqd', 0), 'onesb'], [('pbn', ni)])
                act(lambda n=n, ni=ni: A.activation(out=rq2[ni][:, 0:n], in_=pb[4 + ni][:, 0:n], func=AF.Sqrt, bias=EPS), [('pbn', ni)], [('rq2', ni)])

            def conv_back(m, b, ci):
                if m >= 8:
                    return
                t0, n = (0, 256) if b < 0 else (CTX + 512 * b, 512)
                ni = ci % 2
                dst = dnT[:, m, t0:t0 + n]
                dve(lambda n=n, ni=ni: V.reciprocal(out=rq2[ni][:, 0:n], in_=rq2[ni][:, 0:n]), [('rq2', ni)], [('rq2', ni)])
                sc = 128 ** -0.5 if m < 4 else 1.0
                dve(lambda dst=dst, n=n, ci=ci, ni=ni, sc=sc: V.scalar_tensor_tensor(out=dst, in0=cx[ci][:, 0:n], scalar=sc, in1=rq2[ni][:, 0:n], op0=ALU.mult, op1=ALU.mult),
                    [('cx', ci), ('rq2', ni)], [('dnT', m, b)])

            blks = []
            for b in range(-1, 4):
                blks.append((b, cctr[0] % 4))
                cctr[0] += 1
            conv_front(m, blks[0][0], blks[0][1])
            for k in range(5):
                if k + 1 < 5:
                    conv_front(m, blks[k + 1][0], blks[k + 1][1])
                conv_back(m, blks[k][0], blks[k][1])
        for zc in range(4):
            wp, wk = load_w(2304 + 128 * zc)
            for tl in range(16):
                ti = tl + 2
                bi = pctr[0] % 2
                pctr[0] += 1
                for kc in range(8):
                    mm(pb[bi][:, 0:128], hT[:, kc, ti * 128:(ti + 1) * 128], wp[:, kc, :], kc == 0, kc == 7, [wk], [('pbq', bi)])
                act(lambda tl=tl, zc=zc, bi=bi: A.copy(out=zt[:, tl, zc * 128:(zc + 1) * 128], in_=pb[bi][:, 0:128]), [('pbq', bi)], [('zt', tl, zc)])
        wp, wk = load_w(2816, 16)
        for ti in range(NT):
            bi = pctr[0] % 2
            pctr[0] += 1
            for kc in range(8):
                mm(pb[bi][:, 0:16], hT[:, kc, ti * 128:(ti + 1) * 128], wp[:, kc, 0:16], kc == 0, kc == 7, [wk], [('pbq', bi)])
            dve(lambda ti=ti, bi=bi: V.tensor_copy(out=gbraw[:, ti, :], in_=pb[bi][:, 0:16]), [('pbq', bi)], ['gbraw'])
        act(lambda: A.activation(out=gb[:, :, 0:8], in_=gbraw[:, :, 0:8], func=AF.Sigmoid), ['gbraw'], ['gb_b'])
        dve(lambda: V.tensor_scalar(out=nbeta, in0=gb[:, :, 0:8], scalar1=-1.0, scalar2=None, op0=ALU.mult), ['gb_b'], ['nbeta'])
        dve(lambda: V.tensor_tensor(out=gbraw[:, :, 8:16], in0=gbraw[:, :, 8:16], in1=dtb, op=ALU.add), ['gbraw', 'dtb'], ['gbraw2'])
        act(lambda: A.activation(out=gbraw[:, :, 8:16], in_=gbraw[:, :, 8:16], func=AF.Exp), ['gbraw2'], ['gbraw3'])
        act(lambda: A.activation(out=gbraw[:, :, 8:16], in_=gbraw[:, :, 8:16], func=AF.Ln, bias=1.0), ['gbraw3'], ['gbraw4'])
        act(lambda: A.activation(out=alog, in_=alog, func=AF.Exp), ['alog'], ['aexp'])
        dve(lambda: V.scalar_tensor_tensor(out=gb[:, :, 8:16], in0=gbraw[:, :, 8:16], scalar=-1.0, in1=alog, op0=ALU.mult, op1=ALU.mult),
            ['gbraw4', 'aexp'], ['gb_g'])
        if "dnT" in dbg_t:
            dd = AR.alloc([128, 12, 256], F32)
            dve(lambda: V.tensor_copy(out=dd, in_=dnT[:, :, 128:384]), [('dnT', m_, b_) for m_ in range(12) for b_ in (-1, 0)], ['dd'])
            dump("dnT", dd, ['dd'])
        dump("gb", gb, ['gb_b', 'gb_g'])
        S.barrier()
        AR.release(mC2)
        AR.release_top()
        if stop_after <= 4:
            S.finish()
            return nc

        osum = AR.alloc([128, 16, 512], F32)
        mE = AR.mark()
        NU = 4
        Sst = AR.alloc([128, 8, 128], F32)
        Sb = AR.alloc([128, 8, 128], BF16)
        ktok = [AR.alloc([128, 128], BF16) for _ in range(NU)]
        vtok = [AR.alloc([128, 128], BF16) for _ in range(NU)]
        KQ = [AR.alloc([128, 256], F32) for _ in range(NU)]
        gmx = [AR.alloc([128, 130], F32) for _ in range(NU)]
        Es = [AR.alloc([128, 128], F32) for _ in range(NU)]
        EmT = [AR.alloc([128, 128], F32) for _ in range(NU)]
        sc4 = [AR.alloc([128, 4], F32) for _ in range(NU)]
        Pm = [[AR.alloc([128, 128], F32) for _ in range(2)] for _ in range(NU)]
        PTm = [[AR.alloc([128, 128], F32) for _ in range(2)] for _ in range(NU)]
        TT = [AR.alloc([128, 128], F32) for _ in range(NU)]
        rv = [AR.alloc([128, 128], F32) for _ in range(NU)]
        rw = [AR.alloc([128, 128], F32) for _ in range(NU)]
        uu = [AR.alloc([128, 128], F32) for _ in range(NU)]
        wTb = [AR.alloc([128, 128], BF16) for _ in range(NU)]
        kdec = [AR.alloc([128, 128], BF16) for _ in range(NU)]
        dgb = [AR.alloc([128, 128], BF16) for _ in range(NU)]
        qdT = [AR.alloc([128, 128], BF16) for _ in range(NU)]
        qkT = [AR.alloc([128, 128], BF16) for _ in range(NU)]
        glb = [AR.alloc([128, 2], F32) for _ in range(NU)]
        vn = [AR.alloc([128, 128], BF16) for _ in range(NU)]
        dgf = Es
        pool(lambda: P.memset(Sst, 0.0), [], [('S', c_) for c_ in range(8)])
        pool(lambda: P.memset(Sb, 0.0), [], [('Sb', c_) for c_ in range(8)])
        pool(lambda: P.memset(osum, 0.0), [], [('osum', tl_, h_) for tl_ in range(16) for h_ in range(4)])

        def pe1(o, l, r_, rd, wr):
            return S.op('pe', lambda: TE.matmul(o, lhsT=l, rhs=r_, start=True, stop=True), rd, wr, pe_accum=True)

        NSTEP = int(os.environ.get('NSTEP', NT))
        fwd_order = list(range(NT))
        bwd_order = [1, 0] + list(range(NT - 1, 1, -1))

        def unit_gen(h, d, ti, i):
            B0, B2 = pb[2 * i], pb[2 * i + 1]
            PT = B2[:, 384:512].bitcast(BF16).rearrange("p (a b) -> p a b", b=128)
            k0, k2 = 'pbE0_%d' % i, 'pbE2_%d' % i
            kt_ = k2
            hi = i
            lat = ti >= 2
            tl = ti - 2
            tsl = slice(ti * 128, (ti + 1) * 128)
            c = d * 4 + h
            col = c
            Mtri = dnm[:, 0, :] if d == 0 else dnm[:, 2, :]
            maskA = dnm[:, 1, :] if d == 0 else dnm[:, 3, :]
            maskQK = Mtri
            gcol = gb[:, ti, 8 + col:9 + col]
            bcol = gb[:, ti, col:col + 1]
            nbcol = nbeta[:, ti, col:col + 1]
            kT_t = dnT[:, 4 + h, tsl]
            qT_t = dnT[:, h, tsl]
            vT_t = dnT[:, 8 + h, tsl]
            pk, pv = 0, 1
            tr(PT[:, pk, :], kT_t, identb, [], [kt_])
            tr(PT[:, pv, :], vT_t, identb, [], [kt_])
            pe1(B0[:, 0:128], kT_t, kT_t, [], [k0])
            if lat:
                pe1(B0[:, 128:256], kT_t, qT_t, [], [k0])
            dve(lambda: V.tensor_scalar(out=gmx[i], in0=msx[:, d, :], scalar1=gcol, scalar2=None, op0=ALU.mult), [], [('gmx', i)])
            yield
            act(lambda: A.copy(out=ktok[hi], in_=PT[:, pk, :]), [kt_], [('ktok', hi)])
            act(lambda: A.copy(out=vtok[hi], in_=PT[:, pv, :]), [kt_], [('vtok', hi)])
            act(lambda: A.copy(out=KQ[hi], in_=B0[:, 0:256]), [k0], [('KQ', hi)])
            pe1(B2[:, 0:130], Mtri, gmx[i], [('gmx', i)], [k2])
            pe1(B2[:, 130:132], blk64f, gmx[i][:, 128:130], [('gmx', i)], [k2])
            if lat:
                pe1(B2[:, 256:384], gmx[i][:, 0:128], Mtri, [('gmx', i)], [k2])
            yield
            act(lambda: A.activation(out=Es[i], in_=B2[:, 0:128], func=AF.Exp), [k2], [('Es', i)])
            act(lambda: A.activation(out=sc4[i][:, 0:1], in_=B2[:, 128:129], func=AF.Exp), [k2], [('egc', i)])
            act(lambda: A.copy(out=sc4[i][:, 1:2], in_=B2[:, 130:131]), [k2], [('tots', i)])
            act(lambda: A.activation(out=sc4[i][:, 2:3], in_=B2[:, 128:129], func=AF.Exp, scale=-1.0, bias=sc4[i][:, 1:2]),
                [k2, ('tots', i)], [('erest', i)])
            act(lambda: A.activation(out=sc4[i][:, 3:4], in_=sc4[i][:, 1:2], func=AF.Exp), [('tots', i)], [('etot', i)])
            if lat:
                act(lambda: A.activation(out=EmT[i], in_=B2[:, 256:384], func=AF.Exp), [k2], [('EmT', i)])
                pool(lambda: P.tensor_tensor(out=EmT[i], in0=EmT[i], in1=maskQK, op=ALU.mult), [('EmT', i)], [('EmT', i)])
            pool(lambda: P.tensor_tensor(out=Es[i], in0=Es[i], in1=maskA, op=ALU.mult), [('Es', i)], [('Es', i)])
            yield
            dve(lambda: V.scalar_tensor_tensor(out=Pm[i][0], in0=KQ[hi][:, 0:128], scalar=nbcol, in1=Es[i], op0=ALU.mult, op1=ALU.mult),
                [('KQ', hi), ('Es', i)], [('P', i, 0)])
            tr(B2[:, 0:128], Pm[i][0], ident, [('P', i, 0)], [k2])
            yield
            dve(lambda: V.tensor_copy(out=PTm[i][0], in_=B2[:, 0:128]), [k2], [('PT', i, 0)])
            dve(lambda: V.tensor_tensor(out=TT[i], in0=B2[:, 0:128], in1=ident, op=ALU.add), [k2], [('TT', i)])
            for k in range(5):
                cu, nx = k % 2, 1 - k % 2
                pe1(B0[:, 0:128], PTm[i][cu], Pm[i][cu], [('PT', i, cu), ('P', i, cu)], [k0])
                if k < 4:
                    pe1(B0[:, 128:256], Pm[i][cu], PTm[i][cu], [('PT', i, cu), ('P', i, cu)], [k0])
                yield
                if k < 4:
                    act(lambda nx=nx: A.copy(out=Pm[i][nx], in_=B0[:, 0:128]), [k0], [('P', i, nx)])
                    act(lambda nx=nx: A.copy(out=PTm[i][nx], in_=B0[:, 128:256]), [k0], [('PT', i, nx)])
                else:
                    act(lambda nx=nx: A.copy(out=Pm[i][nx], in_=B0[:, 0:128]), [k0], [('P', i, nx)])
                pe1(B2[:, 0:128], Pm[i][nx], TT[i], [('P', i, nx), ('TT', i)], [k2])
                yield
                dve(lambda: V.tensor_tensor(out=TT[i], in0=B2[:, 0:128], in1=TT[i], op=ALU.add), [k2, ('TT', i)], [('TT', i)])
            dve(lambda: V.tensor_scalar(out=rv[i], in0=vtok[hi], scalar1=bcol, scalar2=None, op0=ALU.mult), [('vtok', hi)], [('rv', i)])
            dve(lambda: V.tensor_scalar(out=rw[i], in0=ktok[hi], scalar1=bcol, scalar2=None, op0=ALU.mult), [('ktok', hi)], [('rw', i)])
            dve(lambda: V.tensor_scalar(out=rw[i], in0=rw[i], scalar1=sc4[i][:, 0:1], scalar2=None, op0=ALU.mult), [('rw', i), ('egc', i)], [('rw', i)])
            dve(lambda: V.tensor_scalar(out=kdec[i], in0=ktok[hi], scalar1=sc4[i][:, 2:3], scalar2=None, op0=ALU.mult),
                [('ktok', hi), ('erest', i)], [('kdec', i)])
            if lat:
                dve(lambda: V.tensor_scalar(out=dgb[i], in0=ident, scalar1=sc4[i][:, 0:1], scalar2=None, op0=ALU.mult), [('egc', i)], [('dgb', i)])
            pe1(B0[:, 0:128], TT[i], rv[i], [('TT', i), ('rv', i)], [k0])
            pe1(B0[:, 128:256], rw[i], TT[i], [('TT', i), ('rw', i)], [k0])
            pe1(B0[:, 256:257], selrow[:, 0, :], sc4[i][:, 3:4], [('etot', i)], [k0])
            pe1(B0[:, 257:258], selrow[:, 1, :], sc4[i][:, 3:4], [('etot', i)], [k0])
            if lat:
                pe1(B0[:, 384:512], onesb, dgb[i], [('dgb', i)], [k0])
            yield
            act(lambda: A.copy(out=uu[i], in_=B0[:, 0:128]), [k0], [('uu', i)])
            act(lambda: A.copy(out=wTb[i], in_=B0[:, 128:256]), [k0], [('wTb', i)])
            act(lambda: A.copy(out=glb[i], in_=B0[:, 256:258]), [k0], [('glb', i)])
            if lat:
                act(lambda: A.copy(out=dgf[i], in_=B0[:, 384:512]), [k0], [('Es', i)])
                dve(lambda: V.tensor_tensor(out=qdT[i], in0=qT_t, in1=dgf[i], op=ALU.mult), [('Es', i)], [('qdT', i)])
                dve(lambda: V.tensor_tensor(out=qkT[i], in0=KQ[hi][:, 128:256], in1=EmT[i], op=ALU.mult), [('KQ', hi), ('EmT', i)], [('qkT', i)])
            yield
            for pi_ in ((0, 1) if d == 0 else (1, 0)):
                R = slice(pi_ * 64, pi_ * 64 + 64)
                pe1(B2[:, 0:128], wTb[i], Sb[:, c, :], [('wTb', i), ('Sb', c)], [k2])
                if lat:
                    S.op('pe', lambda: TE.matmul(B2[:, 128:256], lhsT=qdT[i], rhs=Sb[:, c, :], start=True, stop=False),
                         [('qdT', i), ('Sb', c)], [k2], pe_accum=True)
                yield
                dve(lambda R=R: V.tensor_tensor(out=vn[i][R, :], in0=uu[i][R, :], in1=B2[R, 0:128], op=ALU.subtract), [('uu', i), k2], [('vn', i)])
                if lat:
                    S.op('pe', lambda R=R: TE.matmul(B2[:, 128:256], lhsT=qkT[i][R, :], rhs=vn[i][R, :], start=False, stop=True),
                         [('qkT', i), ('vn', i)], [k2], pe_accum=True)
                pe1(B2[:, 256:384], kdec[i][R, :], vn[i][R, :], [('kdec', i), ('vn', i)], [k2])
                yield
                if lat:
                    dve(lambda R=R: V.tensor_tensor(out=osum[R, tl, h * 128:(h + 1) * 128], in0=osum[R, tl, h * 128:(h + 1) * 128],
                                                    in1=B2[R, 128:256], op=ALU.add), [k2, ('osum', tl, h)], [('osum', tl, h)])
                dve(lambda pi_=pi_: V.scalar_tensor_tensor(out=Sst[:, c, :], in0=Sst[:, c, :], scalar=glb[i][:, pi_:pi_ + 1], in1=B2[:, 256:384],
                                                           op0=ALU.mult, op1=ALU.add), [k2, ('glb', i), ('S', c)], [('S', c)])
                act(lambda: A.copy(out=Sb[:, c, :], in_=Sst[:, c, :]), [('S', c)], [('Sb', c)])
                yield

        for step in range(NSTEP):
            chains = [(h, d) for h in range(4) for d in range(2)]
            for p0 in range(0, 8, NU):
                gens = []
                for slot, (h, d) in enumerate(chains[p0:p0 + NU]):
                    ti = fwd_order[step] if d == 0 else bwd_order[step]
                    gens.append(unit_gen(h, d, ti, slot))
                alive = list(gens)
                while alive:
                    for gobj in list(alive):
                        try:
                            next(gobj)
                        except StopIteration:
                            alive.remove(gobj)
        if "S0" in dbg_t:
            dump("S0", Sst[:, int(os.environ.get('DBGC', 0)), :], [('S', c_) for c_ in range(8)])
        if "osum" in dbg_t:
            dump("osum", osum[:, 0:4, :], [('osum', tl_, h_) for tl_ in range(4) for h_ in range(4)])
        S.barrier()
        AR.release(mE)
        if stop_after <= 5:
            S.finish()
            return nc

        onw = AR.alloc([128, 128], F32)
        sq2 = [AR.alloc([128, 512], F32) for _ in range(2)]
        sse = AR.alloc([128, 16, 4], F32)
        szl = [AR.alloc([128, 512], F32) for _ in range(2)]
        ydn = [AR.alloc([128, 512], BF16) for _ in range(2)]
        S.dma('sp', onw, dr["onw"], writes=['onw'])
        for tl in range(16):
            i = tl % 2
            okeys = [('osum', tl, h_) for h_ in range(4)]
            pool(lambda tl=tl, i=i: P.tensor_tensor(out=sq2[i], in0=osum[:, tl, :], in1=osum[:, tl, :], op=ALU.mult), okeys, [('sq2', i)])
            dve(lambda tl=tl, i=i: V.tensor_reduce(out=sse[:, tl, :], in_=sq2[i].rearrange("p (h d) -> p h d", d=128), axis=AX.X, op=ALU.add),
                [('sq2', i)], [('sse', tl)])
            dve(lambda tl=tl: V.tensor_scalar(out=sse[:, tl, :], in0=sse[:, tl, :], scalar1=1.0 / 128, scalar2=EPS, op0=ALU.mult, op1=ALU.add),
                [('sse', tl)], [('sse', tl)])
            act(lambda tl=tl: A.activation(out=sse[:, tl, :], in_=sse[:, tl, :], func=AF.Sqrt), [('sse', tl)], [('sse', tl)])
            dve(lambda tl=tl: V.reciprocal(out=sse[:, tl, :], in_=sse[:, tl, :]), [('sse', tl)], [('sse', tl)])
            act(lambda tl=tl, i=i: A.activation(out=szl[i], in_=zt[:, tl, :], func=AF.Silu), [], [('szl', i)])
            for h in range(4):
                dve(lambda tl=tl, i=i, h=h: V.scalar_tensor_tensor(out=sq2[i][:, h * 128:(h + 1) * 128], in0=osum[:, tl, h * 128:(h + 1) * 128],
                                                                 scalar=sse[:, tl, h:h + 1], in1=onw, op0=ALU.mult, op1=ALU.mult),
                    okeys + [('sse', tl), 'onw', ('sq2', i)], [('sq2', i)])
            dve(lambda i=i: V.tensor_tensor(out=ydn[i], in0=sq2[i], in1=szl[i], op=ALU.mult), [('sq2', i), ('szl', i)], [('ydn', i)])
            for h in range(4):
                tr(pbt[1][:, h, :], ydn[i][:, h * 128:(h + 1) * 128], identb, [('ydn', i)], ['pbt1'])
            act(lambda tl=tl: A.copy(out=dnoT[:, :, tl * 128:(tl + 1) * 128], in_=pbt[1][:, 0:4, :]), ['pbt1'], [('dnoT', tl)])
        if "dnoT" in dbg_t:
            ddn = AR.alloc([128, 4, 512], F32)
            dve(lambda: V.tensor_copy(out=ddn, in_=dnoT[:, :, 0:512]), [('dnoT', tl_) for tl_ in range(4)], ['ddn'])
            dump("dnoT", ddn, ['ddn'])
        S.barrier()
        AR.release(mDN)
        if stop_after <= 6:
            S.finish()
            return nc

        h2 = AR.alloc_top([128, 16, 1024], BF16)
        aff = AR.alloc_top([128, 16, 16], F32)
        posm = AR.alloc_top([128, 16, 16], F32)
        gmb = AR.alloc_top([128, 16, 32], BF16)
        posTb = AR.alloc_top([16, 2048], BF16)
        mF = AR.mark()
        woutb = AR.alloc([128, 8, 1024], BF16)
        rws = AR.alloc([128, 8, 16], F32)
        rwb = AR.alloc([128, 8, 16], BF16)
        xt2 = [AR.alloc([128, 1024], F32) for _ in range(2)]
        x1t = [AR.alloc([128, 1024], F32) for _ in range(2)]
        tmp2 = AR.alloc([128, 1024], F32)
        junk2 = AR.alloc([128, 1024], F32)
        ss2 = AR.alloc([128, 16], F32)
        h2Tt = [AR.alloc([128, 8, 128], BF16) for _ in range(2)]
        smx = AR.alloc([128, 16, 4], F32)
        ee = AR.alloc([128, 16], F32)
        woutv = dr["w_out"].rearrange("(kc p) n -> p kc n", p=128)
        for q in range(4):
            S.dma('pool', woutb[:, 2 * q:2 * q + 2, :], woutv[:, 2 * q:2 * q + 2, :], writes=[('woutb', q)])
        S.dma('sp', rws, dr["router_w"].rearrange("(kc p) e -> p kc e", p=128), writes=['rws'])
        dve(lambda: V.tensor_copy(out=rwb, in_=rws), ['rws'], ['rwb'])
        for tl in range(16):
            i = tl % 2
            tsl = slice(tl * 128, (tl + 1) * 128)
            S.dma('sp', xt2[i], dr["x"][tsl, :], writes=[('xt2', i)])
            for half in range(2):
                hs = slice(half * 512, (half + 1) * 512)
                for kc in range(8):
                    lhs = attT[:, kc, tsl] if kc < 4 else dnoT[:, kc - 4, tsl]
                    mm(pb[half], lhs, woutb[:, kc, hs], kc == 0, kc == 7, [('woutb', kc // 2)], [('pby', half)])
                dve(lambda i=i, half=half, hs=hs: V.tensor_tensor(out=x1t[i][:, hs], in0=pb[half], in1=g1row[:, hs], op=ALU.mult),
                    [('pby', half)], [('x1t', i, half)])
                dve(lambda i=i, hs=hs: V.tensor_tensor(out=x1t[i][:, hs], in0=x1t[i][:, hs], in1=xt2[i][:, hs], op=ALU.add),
                    [('x1t', i, half), ('xt2', i)], [('x1t', i, half)])
            S.dma('pool', x1s[tsl, :], x1t[i], reads=[('x1t', i, 0), ('x1t', i, 1)], writes=[('x1s', tl)])
            xk = [('x1t', i, 0), ('x1t', i, 1)]
            act(lambda i=i, tl=tl: A.activation(out=junk2, in_=x1t[i], func=AF.Square, accum_out=ss2[:, tl:tl + 1]), xk, ['junk2', ('ss2', tl)])
            dve(lambda tl=tl: V.tensor_scalar(out=ss2[:, tl:tl + 1], in0=ss2[:, tl:tl + 1], scalar1=1.0 / D, scalar2=EPS, op0=ALU.mult, op1=ALU.add),
                [('ss2', tl)], [('ss2', tl)])
            act(lambda tl=tl: A.activation(out=ss2[:, tl:tl + 1], in_=ss2[:, tl:tl + 1], func=AF.Sqrt), [('ss2', tl)], [('ss2', tl)])
            dve(lambda tl=tl: V.reciprocal(out=ss2[:, tl:tl + 1], in_=ss2[:, tl:tl + 1]), [('ss2', tl)], [('ss2', tl)])
            dve(lambda i=i, tl=tl: V.scalar_tensor_tensor(out=tmp2, in0=x1t[i], scalar=ss2[:, tl:tl + 1], in1=s2row, op0=ALU.mult, op1=ALU.mult),
                xk + [('ss2', tl)], ['tmp2'])
            dve(lambda tl=tl: V.tensor_tensor(out=h2[:, tl, :], in0=tmp2, in1=b2row, op=ALU.add), ['tmp2'], [('h2', tl)])
            for kc in range(8):
                tr(pbt[i][:, kc, :], h2[:, tl, kc * 128:(kc + 1) * 128], identb, [('h2', tl)], [('pbt', i)])
            act(lambda i=i: A.copy(out=h2Tt[i], in_=pbt[i]), [('pbt', i)], [('h2Tt', i)])
            for kc in range(8):
                mm(pb[2 + i][:, 0:16], h2Tt[i][:, kc, :], rwb[:, kc, :], kc == 0, kc == 7, [('h2Tt', i), 'rwb'], [('pbr', i)])
            dve(lambda i=i, tl=tl: V.tensor_reduce(out=smx[:, tl, 0:1], in_=pb[2 + i][:, 0:16], axis=AX.X, op=ALU.max), [('pbr', i)], [('smx', tl)])
            dve(lambda tl=tl: V.tensor_scalar(out=smx[:, tl, 1:2], in0=smx[:, tl, 0:1], scalar1=-1.0, scalar2=None, op0=ALU.mult), [('smx', tl)], [('smx', tl)])
            act(lambda i=i, tl=tl: A.activation(out=ee, in_=pb[2 + i][:, 0:16], func=AF.Exp, bias=smx[:, tl, 1:2], accum_out=smx[:, tl, 2:3]),
                [('pbr', i), ('smx', tl)], ['ee', ('smx', tl)])
            dve(lambda tl=tl: V.reciprocal(out=smx[:, tl, 3:4], in_=smx[:, tl, 2:3]), [('smx', tl)], [('smx', tl)])
            dve(lambda tl=tl: V.tensor_scalar(out=aff[:, tl, :], in0=ee, scalar1=smx[:, tl, 3:4], scalar2=None, op0=ALU.mult), ['ee', ('smx', tl)], [('aff', tl)])
        if "x1" in dbg_t:
            dump("x1", x1t[0], [('x1t', 0, 0), ('x1t', 0, 1)])
        dump("aff", aff, [('aff', tl_) for tl_ in range(16)])
        S.barrier()
        AR.release(mF)
        if stop_after <= 7:
            S.finish()
            return nc

        mF2 = AR.mark()
        affT = AR.alloc([16, 2048], F32)
        work = AR.alloc([16, 2048], F32)
        maskT = AR.alloc([16, 2048], F32)
        cum = AR.alloc([16, 2048], F32)
        ones16 = AR.alloc([16, 2048], F32)
        posTf = AR.alloc([16, 2048], F32)
        m8 = AR.alloc([16, 8], F32)
        mtok = AR.alloc([128, 256], F32)
        gm = AR.alloc([128, 256], F32)
        gml = AR.alloc([128, 256], F32)
        for tl in range(16):
            q = tl // 4
            tr(pb[q][0:16, (tl % 4) * 128:(tl % 4 + 1) * 128], aff[:, tl, :], ident, [], [('pbA', q)])
        for q in range(4):
            act(lambda q=q: A.copy(out=affT[:, q * 512:(q + 1) * 512], in_=pb[q][0:16, :]), [('pbA', q)], ['affT'])
        pool(lambda: P.tensor_copy(out=work, in_=affT), ['affT'], ['work'])
        pool(lambda: P.memset(ones16, 1.0), [], ['ones16'])
        for it_ in range(CAP // 8):
            dve(lambda: V.max(out=m8, in_=work), ['work'], ['m8'])
            dve(lambda: V.match_replace(out=work, in_to_replace=m8, in_values=work, imm_value=-1.0), ['m8', 'work'], ['work'])
        dve(lambda: V.tensor_scalar(out=maskT, in0=affT, scalar1=m8[:, 7:8], scalar2=None, op0=ALU.is_ge), ['affT', 'm8'], ['maskT'])
        dve(lambda: V.tensor_tensor_scan(out=cum, data0=ones16, data1=maskT, initial=0.0, op0=ALU.mult, op1=ALU.add), ['ones16', 'maskT'], ['cum'])
        dve(lambda: V.tensor_tensor(out=posTf, in0=cum, in1=maskT, op=ALU.mult), ['cum', 'maskT'], ['posTf'])
        dve(lambda: V.tensor_scalar(out=posTf, in0=posTf, scalar1=-1.0, scalar2=None, op0=ALU.add), ['posTf'], ['posTf'])
        act(lambda: A.copy(out=posTb, in_=posTf), ['posTf'], ['posTb'])
        for tl in range(16):
            tr(pb[4][:, tl * 16:(tl + 1) * 16], posTf[:, tl * 128:(tl + 1) * 128], ident[0:16, 0:16], ['posTf'], ['pb4'])
        posm_v = posm.rearrange("p t e -> p (t e)")
        aff_v = aff.rearrange("p t e -> p (t e)")
        gmb_v = gmb.rearrange("p t (e two) -> p (t e) two", two=2)
        act(lambda: A.copy(out=posm_v, in_=pb[4][:, 0:256]), ['pb4'], ['posm'])
        dve(lambda: V.tensor_scalar(out=mtok, in0=posm_v, scalar1=0.0, scalar2=None, op0=ALU.is_ge), ['posm'], ['mtok'])
        dve(lambda: V.tensor_tensor(out=gm, in0=aff_v, in1=mtok, op=ALU.mult), ['mtok'], ['gm'])
        dve(lambda: V.tensor_copy(out=gmb_v[:, :, 0], in_=gm), ['gm'], ['gmb0'])
        dve(lambda: V.tensor_tensor(out=gml, in0=gm, in1=gmb_v[:, :, 0], op=ALU.subtract), ['gm', 'gmb0'], ['gml'])
        dve(lambda: V.tensor_copy(out=gmb_v[:, :, 1], in_=gml), ['gml'], ['gmb1'])
        if "posT" in dbg_t:
            dump("posT", posTf, ['posTf'])
        S.barrier()
        AR.release(mDN0)
        if stop_after <= 8:
            S.finish()
            return nc

        yall = AR.alloc([128, NE, 2, 1024], BF16)
        mG = AR.mark()
        wgb = AR.alloc([128, 8, 1024], BF16)
        wub = AR.alloc([128, 8, 1024], BF16)
        wdb = AR.alloc([128, 8, 1024], BF16)
        Sel = AR.alloc([128, 16, 256], BF16)
        xgT = g1row.bitcast(BF16).rearrange("p (a b) -> p a b", b=256)
        hidT = s2row.bitcast(BF16).rearrange("p (a b) -> p a b", b=256)
        sgv = b2row.rearrange("p (a b) -> p a b", b=256)
        g4 = sgv[:, 2, 0:4]
        gs = sgv[:, 2, 8:10]
        def load_expert(e):
            for (wd, dst, key) in ((dr["w_gate"][e], wgb, 'wgb'), (dr["w_up"][e], wub, 'wub'), (dr["w_down"][e], wdb, 'wdb')):
                for kc in range(8):
                    S.dma('pool', dst[:, kc, :], wd[kc * 128:(kc + 1) * 128, :], writes=[(key, kc)])

        NEXP = int(os.environ.get('NEXP', NE))
        load_expert(0)
        for e in range(NEXP):
            for tl in range(16):
                dve(lambda tl=tl, e=e: V.tensor_scalar(out=Sel[:, tl, :], in0=iota256, scalar1=posm[:, tl, e:e + 1], scalar2=None, op0=ALU.is_equal),
                    [], [('Sel', tl)])
            for kc in range(8):
                bi = kc % 2
                for tl in range(16):
                    mm(pb[bi][:, 0:256], h2[:, tl, kc * 128:(kc + 1) * 128], Sel[:, tl, :], tl == 0, tl == 15, [('Sel', tl)], [('pbg', bi)])
                act(lambda kc=kc, bi=bi: A.copy(out=xgT[:, kc, :], in_=pb[bi][:, 0:256]), [('pbg', bi)], [('xgT', kc)])
            for cc in range(2):
                for tl in range(16):
                    mm(pb[0][:, 256 + cc * 2:258 + cc * 2], Sel[:, tl, cc * 128:(cc + 1) * 128], gmb[:, tl, 2 * e:2 * e + 2], tl == 0, tl == 15,
                       [('Sel', tl), 'gmb0', 'gmb1'], [('pbg', 0)])
            act(lambda: A.copy(out=g4, in_=pb[0][:, 256:260]), [('pbg', 0)], ['g4'])
            dve(lambda: V.tensor_tensor(out=gs, in0=g4.rearrange("p (c t) -> p c t", t=2)[:, :, 0], in1=g4.rearrange("p (c t) -> p c t", t=2)[:, :, 1], op=ALU.add),
                ['g4'], ['gs'])
            for ffc in range(8):
                pg, pu = (pb[2], pb[3]) if ffc % 2 == 0 else (pb[4], pb[5])
                kg, ku = ('pbf', 2 + (ffc % 2) * 2), ('pbf', 3 + (ffc % 2) * 2)
                for kc in range(8):
                    mm(pg[:, 0:256], wgb[:, kc, ffc * 128:(ffc + 1) * 128], xgT[:, kc, :], kc == 0, kc == 7, [('wgb', kc), ('xgT', kc)], [kg])
                for kc in range(8):
                    mm(pu[:, 0:256], wub[:, kc, ffc * 128:(ffc + 1) * 128], xgT[:, kc, :], kc == 0, kc == 7, [('wub', kc), ('xgT', kc)], [ku])
                si = ffc % 2
                act(lambda pg=pg, si=si: A.activation(out=sgv[:, si, :], in_=pg[:, 0:256], func=AF.Silu), [kg], [('sg', si)])
                dve(lambda pu=pu, si=si, ffc=ffc: V.tensor_tensor(out=hidT[:, ffc, :], in0=sgv[:, si, :], in1=pu[:, 0:256], op=ALU.mult),
                    [('sg', si), ku], [('hidT', ffc)])
            for cc in range(2):
                for half in range(2):
                    bi = (cc * 2 + half) % 2
                    for ffc in range(8):
                        mm(pb[bi], hidT[:, ffc, cc * 128:(cc + 1) * 128], wdb[:, ffc, half * 512:(half + 1) * 512], ffc == 0, ffc == 7,
                           [('hidT', ffc), ('wdb', ffc)], [('pbg', bi)])
                    act(lambda e=e, cc=cc, half=half, bi=bi: A.activation(out=yall[:, e, cc, half * 512:(half + 1) * 512], in_=pb[bi], func=AF.Identity, scale=gs[:, cc:cc + 1]),
                        [('pbg', bi), 'gs'], [('yall', e)])
            if e + 1 < NEXP:
                load_expert(e + 1)
        if "yall" in dbg_t:
            dy = AR.alloc([128, 2, 1024], F32)
            dve(lambda: V.tensor_copy(out=dy, in_=yall[:, 0, :, :]), [('yall', 0)], ['dy'])
            dump("yall", dy, ['dy'])
        S.barrier()
        AR.release(mG)
        if stop_after <= 9:
            S.finish()
            return nc

        SelT = [AR.alloc([128, 2, 256], BF16) for _ in range(2)]
        xo = [AR.alloc([128, 1024], F32) for _ in range(2)]
        ot = [AR.alloc([128, 1024], F32) for _ in range(2)]
        oc = 0
        for blk in range(8):
            for e in range(NE):
                si = e % 2
                pe1(pb[4 + si][:, 0:256], oh16[:, e, :], posTb[:, blk * 256:(blk + 1) * 256], [], [('pbp', si)])
                for cc in range(2):
                    dve(lambda si=si, cc=cc: V.tensor_scalar(out=SelT[si][:, cc, :], in0=pb[4 + si][:, 0:256], scalar1=iotap[:, cc:cc + 1], scalar2=None,
                                                          op0=ALU.is_equal), [('pbp', si)], [('SelT', si, cc)])
                for cc in range(2):
                    for tt in range(2):
                        for half in range(2):
                            first = (e == 0 and cc == 0)
                            last = (e == NE - 1 and cc == 1)
                            mm(pb[tt * 2 + half], SelT[si][:, cc, tt * 128:(tt + 1) * 128], yall[:, e, cc, half * 512:(half + 1) * 512], first, last,
                               [('SelT', si, cc)], [('pbo', tt * 2 + half)])
            for tt in range(2):
                tl = blk * 2 + tt
                i = oc % 2
                oc += 1
                S.dma('sp', xo[i], x1s[tl * 128:(tl + 1) * 128, :], reads=[('x1s', tl)], writes=[('xo', i)])
                for half in range(2):
                    hs = slice(half * 512, (half + 1) * 512)
                    dve(lambda i=i, tt=tt, half=half, hs=hs: V.tensor_tensor(out=ot[i][:, hs], in0=pb[tt * 2 + half], in1=g2row[:, hs], op=ALU.mult),
                        [('pbo', tt * 2 + half)], [('ot', i, half)])
                    dve(lambda i=i, hs=hs: V.tensor_tensor(out=ot[i][:, hs], in0=ot[i][:, hs], in1=xo[i][:, hs], op=ALU.add),
                        [('ot', i, half), ('xo', i)], [('ot', i, half)])
                S.dma('act', out[tl * 128:(tl + 1) * 128, :], ot[i], reads=[('ot', i, 0), ('ot', i, 1)], writes=[('out', tl)])
        S.finish()
    return nc


_CACHE = {}


def _layout_inputs(inp, b):
    f = np.float32
    m = {}
    m["x"] = np.ascontiguousarray(inp["x"][b], f)
    m["ctx"] = np.ascontiguousarray(inp["ctx"][b], f)
    cT = np.zeros((128, 8, 2), f)
    cT[:, :, 0] = inp["c"][b].reshape(8, 128).T
    cT[:, :, 1] = inp["c_ctx"].reshape(8, 128).T
    m["cT"] = cT.reshape(128, 16)
    m["w_mod"] = np.ascontiguousarray(inp["w_mod"][0], f)
    m["bmrow"] = np.ascontiguousarray(np.tile(inp["b_mod"][0][None, :], (2, 1)), f)
    m["n2row"] = np.ascontiguousarray(np.tile(inp["norm2_w"][0][None, :], (2, 1)), f)
    n1 = inp["norm1_w"][0].reshape(8, 128).T
    n2 = inp["norm2_w"][0].reshape(8, 128).T
    m["n12T"] = np.ascontiguousarray(np.concatenate([np.repeat(n1[:, :, None], 2, axis=2).reshape(128, 16),
                                                     np.repeat(n2[:, :, None], 2, axis=2).reshape(128, 16)], axis=1), f)
    m["w_in"] = np.ascontiguousarray(inp["w_in"][0], f)
    m["qkw"] = np.ascontiguousarray(np.stack([np.tile(inp["q_norm_w"][0], 2), np.tile(inp["k_norm_w"][0], 2)], axis=1), f)
    cw = inp["conv_w"][0]
    m["convT"] = np.ascontiguousarray(cw.reshape(5, 12, 128).transpose(2, 1, 0).reshape(128, 60), f)
    m["alog"] = np.ascontiguousarray(np.tile(inp["a_log"][0].reshape(1, 8), (128, NT)), f)
    m["dtb"] = np.ascontiguousarray(np.tile(inp["dt_bias"][0].reshape(1, 8), (128, NT)), f)
    m["onw"] = np.ascontiguousarray(np.tile(inp["o_norm_w"][0].reshape(1, 128), (128, 1)), f)
    m["w_out"] = np.ascontiguousarray(inp["w_out"][0], f)
    m["router_w"] = np.ascontiguousarray(inp["router_w"][0], f)
    m["w_gate"] = np.ascontiguousarray(inp["w_gate"][0], f)
    m["w_up"] = np.ascontiguousarray(inp["w_up"][0], f)
    m["w_down"] = np.ascontiguousarray(inp["w_down"][0], f)
    return m


def kernel(**inputs):
    inp = {k: np.asarray(v) for k, v in inputs.items()}
    if "nc" not in _CACHE:
        _CACHE["nc"] = build_program()
    nc = _CACHE["nc"]
    consts = host_consts()
    in_maps = []
    shared = None
    for b in range(8):
        m = _layout_inputs(inp, b)
        if shared is None:
            shared = {k: m[k] for k in m if k not in ("x", "ctx", "cT")}
        else:
            for k in shared:
                m[k] = shared[k]
        m.update(consts)
        in_maps.append(m)
    res = run_bass_kernel_spmd(nc, in_maps, core_ids=list(range(8)))
    return np.stack([np.asarray(r["out"], np.float32) for r in res.results], axis=0)
```

```python
import os
from contextlib import ExitStack
import numpy as np
import concourse.bass as bass
import concourse.mybir as mybir
from concourse.bass_utils import run_bass_kernel_spmd

F32 = mybir.dt.float32
BF16 = mybir.dt.bfloat16
U8 = mybir.dt.uint8
AF = mybir.ActivationFunctionType
ALU = mybir.AluOpType
AX = mybir.AxisListType

NDMA = 24
D = 1024
SEQ = 2048
CTX = 256
T = SEQ + CTX
NT = T // 128
INW = 2832
EPS = 1e-6
NE = 16
CAP = 256


def _is_psum_key(k):
    n = k if isinstance(k, str) else k[0]
    return isinstance(n, str) and (n.startswith('pb') or n in ('pT', 'pm', 'acc', 'rowbank'))


class Sched:
    def __init__(self, nc, stack):
        self.nc = nc
        self.eng = {'pe': nc.tensor, 'act': nc.scalar, 'dve': nc.vector, 'pool': nc.gpsimd, 'sp': nc.sync}
        self.prog = {e: [] for e in self.eng}
        self.sem = {}
        self.cnt = {}
        for e in self.eng:
            self.sem[e] = stack.enter_context(nc.semaphore('s_' + e))
            self.cnt[e] = 0
        self.dsem = [stack.enter_context(nc.semaphore('d%d' % i)) for i in range(NDMA)]
        self.dcnt = [0] * NDMA
        self.dnext = 0
        self.seen = {e: {} for e in self.eng}
        self.lastw = {}
        self.readers = {}

    def _wait(self, e, tok):
        k, v = tok
        if self.seen[e].get(k, 0) >= v:
            return
        self.seen[e][k] = v
        self.prog[e].append(('wait', k, v))

    def _deps(self, e, reads, writes, pe_accum=False):
        best = {}

        def add(t):
            if best.get(t[0], 0) < t[1]:
                best[t[0]] = t[1]
        for r in reads:
            t = self.lastw.get(r)
            if t is not None:
                add(t)
            if _is_psum_key(r):
                for t in self.readers.get(r, ()):
                    if t[0] != e:
                        add(t)
        for w in writes:
            t = self.lastw.get(w)
            if t is not None and not (pe_accum and t[0] == 'pe'):
                add(t)
            for t in self.readers.get(w, ()):
                add(t)
        for k, v in best.items():
            self._wait(e, (k, v))

    def _commit(self, tok, reads, writes):
        for r in reads:
            lst = self.readers.setdefault(r, [])
            lst[:] = [t for t in lst if t[0] != tok[0]]
            lst.append(tok)
        for w in writes:
            self.lastw[w] = tok
            self.readers[w] = []

    def op(self, e, fn, reads=(), writes=(), pe_accum=False):
        self._deps(e, reads, writes, pe_accum)
        self.cnt[e] += 1
        self.prog[e].append(('op', fn, self.cnt[e]))
        tok = (e, self.cnt[e])
        self._commit(tok, reads, writes)
        return tok

    def dma(self, q, out, in_, reads=(), writes=(), **kw):
        self._deps(q, reads, writes)
        i = self.dnext
        self.dnext = (self.dnext + 1) % NDMA
        if self.dcnt[i] > 0:
            self._wait(q, (('d', i), self.dcnt[i]))
        self.dcnt[i] += 16
        so = self.dsem[i]
        eo = self.eng[q]
        self.prog[q].append(('dma', lambda: eo.dma_start(out=out, in_=in_, **kw).then_inc(so, 16)))
        tok = (('d', i), self.dcnt[i])
        self._commit(tok, reads, writes)
        return tok

    def barrier(self):
        for e in self.eng:
            for i in range(NDMA):
                if self.dcnt[i] > 0:
                    self._wait(e, (('d', i), self.dcnt[i]))
            for e2 in ('pe', 'act', 'dve', 'pool'):
                if self.cnt[e2] > 0:
                    self._wait(e, (e2, self.cnt[e2]))
        self.lastw = {}
        self.readers = {}

    def finish(self):
        nc = self.nc
        for i in range(NDMA):
            if self.dcnt[i] > 0:
                self._wait('sp', (('d', i), self.dcnt[i]))
        for e in ('pe', 'act', 'dve', 'pool'):
            if self.cnt[e] > 0:
                self._wait('sp', (e, self.cnt[e]))
        needed = {e: set() for e in self.eng}
        for e in self.eng:
            for it in self.prog[e]:
                if it[0] == 'wait' and isinstance(it[1], str):
                    needed[it[1]].add(it[2])
        remap = {e: {v: i + 1 for i, v in enumerate(sorted(needed[e]))} for e in self.eng}
        self.maxsem = {e: len(needed[e]) for e in self.eng}

        def emit(e):
            eo = self.eng[e]
            for it in self.prog[e]:
                if it[0] == 'wait':
                    k, v = it[1], it[2]
                    if isinstance(k, str):
                        eo.wait_ge(self.sem[k], remap[k][v])
                    else:
                        eo.wait_ge(self.dsem[k[1]], v)
                elif it[0] == 'op':
                    ins = it[1]()
                    if it[2] in remap[e]:
                        ins.then_inc(self.sem[e], 1)
                else:
                    it[1]()

        with nc.Block() as block:
            @block.sync
            def _(eng):
                emit('sp')

            @block.tensor
            def _(eng):
                emit('pe')

            @block.scalar
            def _(eng):
                emit('act')

            @block.vector
            def _(eng):
                emit('dve')

            @block.gpsimd
            def _(eng):
                emit('pool')


class Arena:
    def __init__(self, ar, nbytes):
        self.ar = ar
        self.n = nbytes
        self.off = 0

    def alloc(self, shape, dtype):
        es = 2 if dtype == BF16 else 4
        per = int(np.prod(shape[1:])) * es
        off = (self.off + 63) // 64 * 64
        assert off + per <= getattr(self, 'top', self.n), ("arena overflow", off, per, getattr(self, 'top', self.n))
        self.off = off + per
        v = self.ar[:, off:off + per].bitcast(dtype)
        if len(shape) == 3:
            v = v.rearrange("p (a b) -> p a b", b=shape[2])
        elif len(shape) == 4:
            v = v.rearrange("p (a b c) -> p a b c", b=shape[2], c=shape[3])
        if shape[0] < 128:
            v = v[0:shape[0]]
        return v

    def alloc_top(self, shape, dtype):
        es = 2 if dtype == BF16 else 4
        per = int(np.prod(shape[1:])) * es
        top = getattr(self, 'top', self.n)
        off = (top - per) // 64 * 64
        assert off >= self.off, ("arena top overflow", off, self.off)
        self.top = off
        v = self.ar[:, off:off + per].bitcast(dtype)
        if len(shape) == 3:
            v = v.rearrange("p (a b) -> p a b", b=shape[2])
        if shape[0] < 128:
            v = v[0:shape[0]]
        return v

    def release_top(self):
        self.top = self.n

    def mark(self):
        return self.off

    def release(self, m):
        self.off = m


def host_consts():
    c = {}
    c["ident"] = np.eye(128, dtype=np.float32)
    p = np.arange(128)
    t = np.arange(SEQ)
    d = p % 64
    is_row = d < 32
    fi = np.where(is_row, d % 16, (d - 32) % 16).astype(np.float32)
    freqs = (np.float32(10000.0) ** (-fi / np.float32(16.0))).astype(np.float32)
    pos = np.where(is_row[:, None], (t // 64)[None, :], (t % 64)[None, :]).astype(np.float32)
    ang = (pos * freqs[:, None]).astype(np.float32)
    c["cosT"] = np.cos(ang).astype(np.float32)
    c["sinT"] = np.sin(ang).astype(np.float32)
    rm = np.zeros((128, 128), np.float32)
    for q in range(128):
        if (q % 32) < 16:
            rm[q + 16, q] = -1.0
        else:
            rm[q - 16, q] = 1.0
    c["rotm"] = rm
    c["blk64"] = (p[:, None] // 64 == p[None, :] // 64).astype(np.float32)
    same = c["blk64"]
    r, cc = p[:, None], p[None, :]
    uinc = (r <= cc) * same
    lstr = (r > cc) * same
    linc = (r >= cc) * same
    ustr = (r < cc) * same
    msx = np.zeros((2, 128, 130), np.float32)
    msx[0, :, :128] = lstr
    msx[1, :, :128] = ustr
    msx[:, :, 128] = 1.0
    c["dnm"] = np.stack([uinc, lstr, linc, ustr]).astype(np.float32)
    c["msx"] = msx
    sel = np.zeros((2, 128, 128), np.float32)
    sel[0, 0, :] = 1.0
    sel[1, 64, :] = 1.0
    c["selrow"] = sel
    c["iota256"] = np.tile(np.arange(256, dtype=np.float32)[None, :], (128, 1))
    c["iotap"] = np.stack([p, p + 128], axis=1).astype(np.float32)
    oh = np.zeros((16, 16, 128), np.float32)
    for e in range(16):
        oh[e, e, :] = 1.0
    c["oh16"] = oh
    return c


CONST_SHAPES = {"ident": [128, 128], "cosT": [128, SEQ], "sinT": [128, SEQ], "rotm": [128, 128], "blk64": [128, 128],
                "dnm": [4, 128, 128], "msx": [2, 128, 130], "selrow": [2, 128, 128], "iota256": [128, 256],
                "iotap": [128, 2], "oh16": [16, 16, 128]}

IN_SHAPES = {"x": [SEQ, D], "ctx": [CTX, D], "cT": [128, 16], "w_mod": [D, 6 * D], "bmrow": [2, 6 * D], "n2row": [2, D], "n12T": [128, 32],
             "w_in": [D, INW], "qkw": [128, 2], "convT": [128, 60], "alog": [128, NT * 8], "dtb": [128, NT * 8],
             "onw": [128, 128], "w_out": [D, D], "router_w": [D, NE], "w_gate": [NE, D, D], "w_up": [NE, D, D],
             "w_down": [NE, D, D]}


def build_program(stop_after=99, dbg=None):
    nc = bass.Bass("TRN2", target_bir_lowering=False)
    dr = {}
    for k, s in list(IN_SHAPES.items()) + list(CONST_SHAPES.items()):
        dr[k] = nc.dram_tensor(k, s, F32, kind="ExternalInput").ap()
    out = nc.dram_tensor("out", [SEQ, D], F32, kind="ExternalOutput").ap()
    x1s = nc.dram_tensor("x1s", [SEQ, D], F32).ap()
    dbg_t = {}
    if dbg:
        for k, s in dbg.items():
            dbg_t[k] = nc.dram_tensor("dbg_" + k, s, F32, kind="ExternalOutput").ap()

    with ExitStack() as st:
        S = Sched(nc, st)
        V, A, P, TE = nc.vector, nc.scalar, nc.gpsimd, nc.tensor
        ARN = 206 * 1024
        ar = st.enter_context(nc.sbuf_tensor("arena", [128, ARN], U8))
        AR = Arena(ar, ARN)
        pb = [st.enter_context(nc.psum_tensor("pb%d" % i, [128, 512], F32))[:] for i in range(8)]
        pbt = [pb[6 + i].bitcast(BF16).rearrange("p (a b) -> p a b", b=128) for i in range(2)]

        def dve(fn, r, w):
            return S.op('dve', fn, r, w)

        def act(fn, r, w):
            return S.op('act', fn, r, w)

        def pool(fn, r, w):
            return S.op('pool', fn, r, w)

        def mm(o, l, r_, start, stop, rd, wr):
            return S.op('pe', lambda: TE.matmul(o, lhsT=l, rhs=r_, start=start, stop=stop), rd, wr, pe_accum=not start)

        def tr(o, i, idn, rd, wr):
            return S.op('pe', lambda: TE.transpose(out=o, in_=i, identity=idn), rd, wr, pe_accum=True)

        def dump(name, src_ap, rd):
            if name in dbg_t:
                S.dma('sp', dbg_t[name], src_ap, reads=rd, writes=['dbg_' + name])

        ident = AR.alloc([128, 128], F32)
        identb = AR.alloc([128, 128], BF16)
        onesf = AR.alloc([128, 128], F32)
        onesb = AR.alloc([128, 128], BF16)
        rotb = AR.alloc([128, 128], BF16)
        blk64b = AR.alloc([128, 128], BF16)
        blk64f = AR.alloc([128, 128], F32)
        dnm = AR.alloc([128, 4, 128], F32)
        msx = AR.alloc([128, 2, 130], F32)
        selrow = AR.alloc([128, 2, 128], F32)
        iota256 = AR.alloc([128, 256], F32)
        iotap = AR.alloc([128, 2], F32)
        oh16 = AR.alloc([16, 16, 128], BF16)
        mT = AR.mark()
        tmpc = AR.alloc([128, 2048], F32)
        S.dma('sp', ident, dr["ident"], writes=['ident'])
        S.dma('sp', blk64f, dr["blk64"], writes=['blk64f'])
        S.dma('sp', dnm, dr["dnm"].rearrange("a p n -> p a n"), writes=['dnm'])
        S.dma('sp', msx, dr["msx"].rearrange("a p n -> p a n"), writes=['msx'])
        S.dma('sp', selrow, dr["selrow"].rearrange("a p n -> p a n"), writes=['selrow'])
        S.dma('sp', iota256, dr["iota256"], writes=['iota256'])
        S.dma('sp', iotap, dr["iotap"], writes=['iotap'])
        S.dma('sp', tmpc[:, 0:128], dr["rotm"], writes=['tmpc'])
        dve(lambda: V.tensor_copy(out=rotb, in_=tmpc[:, 0:128]), ['tmpc'], ['rotb'])
        dve(lambda: V.tensor_copy(out=identb, in_=ident), ['ident'], ['identb'])
        dve(lambda: V.tensor_copy(out=blk64b, in_=blk64f), ['blk64f'], ['blk64b'])
        pool(lambda: P.memset(onesf, 1.0), [], ['onesf'])
        pool(lambda: P.memset(onesb, 1.0), [], ['onesb'])
        S.dma('sp', tmpc[0:16, :], dr["oh16"].rearrange("k e m -> k (e m)"), reads=['rotb'], writes=['tmpc'])
        dve(lambda: V.tensor_copy(out=oh16, in_=tmpc[0:16, :].rearrange("k (e m) -> k e m", m=128)), ['tmpc'], ['oh16'])

        S.barrier()
        AR.release(mT)
        modv = AR.alloc([128, 96], F32)
        s1 = AR.alloc([128, 16], F32)
        s2 = AR.alloc([128, 16], F32)
        n12 = AR.alloc([128, 32], F32)
        g1row = AR.alloc([128, 1024], F32)
        g2row = AR.alloc([128, 1024], F32)
        s2row = AR.alloc([128, 1024], F32)
        b2row = AR.alloc([128, 1024], F32)
        mA = AR.mark()
        if os.environ.get('ARDBG'): print('A persistent end', AR.off)
        cT = AR.alloc([128, 16], F32)
        sil = AR.alloc([128, 16], F32)
        wm = [AR.alloc([128, 8, 1024], F32) for _ in range(2)]
        bmr = AR.alloc([2, 6144], F32)
        modrow = AR.alloc([2, 6144], F32)
        n2r = AR.alloc([2, 1024], F32)
        s2r = AR.alloc([2, 1024], F32)
        sel01 = AR.alloc([2, 128], F32)
        S.dma('sp', cT, dr["cT"], writes=['cT'])
        S.dma('sp', n12, dr["n12T"], writes=['n12'])
        S.dma('sp', bmr, dr["bmrow"], writes=['bmr'])
        S.dma('sp', n2r, dr["n2row"], writes=['n2r'])
        pool(lambda: P.memset(sel01, 0.0), [], ['sel01'])
        pool(lambda: P.memset(sel01[0:1, :], 1.0), ['sel01'], ['sel01'])
        act(lambda: A.activation(out=sil, in_=cT, func=AF.Silu), ['cT'], ['sil'])
        wmv = dr["w_mod"].rearrange("(kc p) n -> p kc n", p=128)
        for j in range(6):
            buf = wm[j % 2]
            for hh in range(2):
                S.dma('sp' if hh == 0 else 'act', buf[:, 4 * hh:4 * hh + 4, :], wmv[:, 4 * hh:4 * hh + 4, j * 1024:(j + 1) * 1024],
                      writes=[('wm', j % 2, hh)])
            for half in range(2):
                bi = (2 * j + half) % 4
                bank = pb[bi]
                for kc in range(8):
                    mm(bank[0:2, :], sil[:, 2 * kc:2 * kc + 2], buf[:, kc, half * 512:(half + 1) * 512], kc == 0, kc == 7,
                       [('wm', j % 2, kc // 4), 'sil'], [('pbm', bi)])
                c0 = j * 1024 + half * 512
                dve(lambda bank=bank, c0=c0: V.tensor_tensor(out=modrow[:, c0:c0 + 512], in0=bank[0:2, :], in1=bmr[:, c0:c0 + 512], op=ALU.add),
                    [('pbm', bi), 'bmr'], [('modrow', j, half)])
        pm = pb[4]
        for idx in range(16):
            tr(pm[:, idx * 2:idx * 2 + 2], modrow[0:2, idx * 128:(idx + 1) * 128], ident[0:2, 0:2],
               [('modrow', idx // 8, (idx % 8) // 4), 'ident'], ['pm'])
        dve(lambda: V.tensor_copy(out=modv[:, 0:32], in_=pm[:, 0:32]), ['pm'], ['modv'])
        dve(lambda: V.scalar_tensor_tensor(out=s1, in0=modv[:, 16:32], scalar=1.0, in1=n12[:, 0:16], op0=ALU.add, op1=ALU.mult),
            ['modv', 'n12'], ['s1'])
        dve(lambda: V.scalar_tensor_tensor(out=s2r, in0=modrow[:, 4096:5120], scalar=1.0, in1=n2r, op0=ALU.add, op1=ALU.mult),
            [('modrow', 4, 0), ('modrow', 4, 1), 'n2r'], ['s2r'])
        rows = [(g1row, modrow, 2048, [('modrow', 2, 0), ('modrow', 2, 1)], 'g1row'), (g2row, modrow, 5120, [('modrow', 5, 0), ('modrow', 5, 1)], 'g2row'),
                (s2row, s2r, 0, ['s2r'], 's2row'), (b2row, modrow, 3072, [('modrow', 3, 0), ('modrow', 3, 1)], 'b2row')]
        ri = 0
        for (dst, src, base, skeys, key) in rows:
            for half in range(2):
                bank = pb[5 + ri % 2]
                mm(bank, sel01[0:2, :], src[0:2, base + half * 512:base + (half + 1) * 512], True, True, skeys + ['sel01'], [('rowbank', ri % 2)])
                act(lambda dst=dst, bank=bank, half=half: A.copy(out=dst[:, half * 512:(half + 1) * 512], in_=bank),
                    [('rowbank', ri % 2)], [key])
                ri += 1
        dump("modv", modv, ['modv'])
        dump("g1row", g1row, ['g1row'])
        S.barrier()
        AR.release(mA)
        mDN0 = AR.mark()
        if stop_after <= 0:
            S.finish()
            return nc

        hT = AR.alloc_top([128, 8, T], BF16)
        mB = AR.mark()
        xin = [AR.alloc([128, 1024], F32) for _ in range(3)]
        junk = AR.alloc([128, 1024], F32)
        xn = [AR.alloc([128, 1024], BF16) for _ in range(2)]
        ssq = AR.alloc([128, NT], F32)
        rstd = AR.alloc([128, NT], F32)

        def b_stats(ti):
            src = dr["ctx"][ti * 128:(ti + 1) * 128, :] if ti < 2 else dr["x"][(ti - 2) * 128:(ti - 1) * 128, :]
            xb, xnb = xin[ti % 3], xn[ti % 2]
            S.dma('sp', xb, src, writes=[('xin', ti % 3)])
            act(lambda xb=xb, ti=ti: A.activation(out=junk, in_=xb, func=AF.Square, accum_out=ssq[:, ti:ti + 1]),
                [('xin', ti % 3)], ['junk', ('ssq', ti)])
            dve(lambda ti=ti: V.tensor_scalar(out=rstd[:, ti:ti + 1], in0=ssq[:, ti:ti + 1], scalar1=1.0 / D, scalar2=EPS,
                                              op0=ALU.mult, op1=ALU.add), [('ssq', ti)], [('rstd', ti)])
            act(lambda ti=ti: A.activation(out=rstd[:, ti:ti + 1], in_=rstd[:, ti:ti + 1], func=AF.Sqrt), [('rstd', ti)], [('rstd', ti)])
            dve(lambda ti=ti: V.reciprocal(out=rstd[:, ti:ti + 1], in_=rstd[:, ti:ti + 1]), [('rstd', ti)], [('rstd', ti)])
            dve(lambda xb=xb, xnb=xnb, ti=ti: V.tensor_scalar(out=xnb, in0=xb, scalar1=rstd[:, ti:ti + 1], scalar2=None, op0=ALU.mult),
                [('xin', ti % 3), ('rstd', ti)], [('xn', ti % 2)])

        def b_transpose(ti):
            t = 1 if ti < 2 else 0
            xnb = xn[ti % 2]
            bv = pbt[ti % 2]
            for kc in range(8):
                tr(bv[:, kc, :], xnb[:, kc * 128:(kc + 1) * 128], identb, [('xn', ti % 2), 'identb'], [('pT', ti % 2)])
            for kc in range(8):
                o_ = hT[:, kc, ti * 128:(ti + 1) * 128]
                sc_ = s1[:, kc * 2 + t:kc * 2 + t + 1]
                bi_ = modv[:, kc * 2 + t:kc * 2 + t + 1]
                act(lambda o_=o_, kc=kc, bv=bv, sc_=sc_, bi_=bi_: A.activation(out=o_, in_=bv[:, kc, :], func=AF.Identity, scale=sc_, bias=bi_),
                    [('pT', ti % 2), 's1', 'modv'], [('hT', ti, kc)])

        b_stats(0)
        for ti in range(NT):
            if ti + 1 < NT:
                b_stats(ti + 1)
            b_transpose(ti)
        if "hT" in dbg_t:
            hTf = AR.alloc([128, 8, 256], F32)
            dve(lambda: V.tensor_copy(out=hTf, in_=hT[:, :, 128:384]), [('hT', a, b) for a in (1, 2) for b in range(8)], ['hTf'])
            dump("hT", hTf, ['hTf'])
        S.barrier()
        AR.release(mB)
        if stop_after <= 1:
            S.finish()
            return nc

        attT = AR.alloc([128, 4, SEQ], BF16)
        mQ = AR.mark()
        if os.environ.get('ARDBG'): print('attT end', AR.off)
        qT = AR.alloc([128, 4, SEQ], BF16)
        kTd = AR.alloc([128, 2, T], BF16)
        vext = AR.alloc([128, NT, 2, 66], BF16)
        mC1 = AR.mark()
        cosT = AR.alloc([128, SEQ], F32)
        sinT = AR.alloc([128, SEQ], F32)
        qkw = AR.alloc([128, 2], F32)
        stg = [AR.alloc([128, 8, 128], F32) for _ in range(2)]
        wpc = [AR.alloc([128, 8, 128], BF16) for _ in range(2)]
        sqb = [AR.alloc([128, 512], BF16) for _ in range(2)]
        rq = [AR.alloc([128, 512], F32) for _ in range(2)]
        qnb = [AR.alloc([128, 512], BF16) for _ in range(2)]
        t1 = [AR.alloc([128, 512], F32) for _ in range(2)]
        t2 = [AR.alloc([128, 512], F32) for _ in range(2)]
        S.dma('sp', cosT, dr["cosT"], writes=['cosT'])
        S.dma('sp', sinT, dr["sinT"], writes=['sinT'])
        S.dma('sp', qkw, dr["qkw"], writes=['qkw'])
        pool(lambda: P.memset(vext, 0.0), [], ['vext'])
        pool(lambda: P.memset(vext[:, :, :, 64:65], 1.0), ['vext'], ['vext1'])
        winv = dr["w_in"].rearrange("(kc p) n -> p kc n", p=128)
        wctr = [0]

        def load_w(col0, ncols=128, dup=False):
            ci = wctr[0] % 2
            wctr[0] += 1
            wp = wpc[ci]
            if dup:
                S.dma('pool', wp[:, :, 0:64], winv[:, :, col0:col0 + 64], writes=[('wpc', ci)])
                S.dma('pool', wp[:, :, 64:128], winv[:, :, col0:col0 + 64], writes=[('wpc', ci)])
            else:
                S.dma('pool', wp[:, :, 0:ncols], winv[:, :, col0:col0 + ncols], writes=[('wpc', ci)])
            return wp, ('wpc', ci)

        ectr = [0]

        def qk_epilogue(ps, pskey, n, wcol, rope_t0, dst, dstkey):
            i = ectr[0] % 2
            ectr[0] += 1
            act(lambda: A.activation(out=sqb[i][:, 0:n], in_=ps[:, 0:n], func=AF.Square), [pskey], [('sqb', i)])
            mm(pb[2][:, 0:n], blk64b, sqb[i][:, 0:n], True, True, [('sqb', i), 'blk64b'], ['pb2'])
            act(lambda: A.activation(out=rq[i][:, 0:n], in_=pb[2][:, 0:n], func=AF.Sqrt, scale=1.0 / 64, bias=EPS), ['pb2'], [('rq', i)])
            dve(lambda: V.reciprocal(out=rq[i][:, 0:n], in_=rq[i][:, 0:n]), [('rq', i)], [('rq', i)])
            if rope_t0 is None:
                dve(lambda: V.scalar_tensor_tensor(out=dst, in0=ps[:, 0:n], scalar=qkw[:, wcol:wcol + 1], in1=rq[i][:, 0:n],
                                                   op0=ALU.mult, op1=ALU.mult), [pskey, 'qkw', ('rq', i)], [dstkey])
                return
            dve(lambda: V.scalar_tensor_tensor(out=qnb[i][:, 0:n], in0=ps[:, 0:n], scalar=qkw[:, wcol:wcol + 1], in1=rq[i][:, 0:n],
                                               op0=ALU.mult, op1=ALU.mult), [pskey, 'qkw', ('rq', i)], [('qnb', i)])
            mm(pb[3][:, 0:n], rotb, qnb[i][:, 0:n], True, True, [('qnb', i), 'rotb'], ['pb3'])
            pool(lambda: P.tensor_tensor(out=t1[i][:, 0:n], in0=qnb[i][:, 0:n], in1=cosT[:, rope_t0:rope_t0 + n], op=ALU.mult),
                 [('qnb', i), 'cosT'], [('t1', i)])
            dve(lambda: V.tensor_tensor(out=t2[i][:, 0:n], in0=pb[3][:, 0:n], in1=sinT[:, rope_t0:rope_t0 + n], op=ALU.mult),
                ['pb3', 'sinT'], [('t2', i)])
            dve(lambda: V.tensor_tensor(out=dst, in0=t1[i][:, 0:n], in1=t2[i][:, 0:n], op=ALU.add), [('t1', i), ('t2', i)], [dstkey])

        NQB = int(os.environ.get('NQB', 4))
        pctr = [0]
        for j in range(4):
            wp, wk = load_w(128 * j)
            for b in range(NQB):
                bi = pctr[0] % 2
                pctr[0] += 1
                t0 = CTX + 512 * b
                for kc in range(8):
                    mm(pb[bi], wp[:, kc, :], hT[:, kc, t0:t0 + 512], kc == 0, kc == 7, [wk] + [('hT', ti_, kc) for ti_ in range(t0 // 128, t0 // 128 + 4)],
                       [('pbq', bi)])
                qk_epilogue(pb[bi], ('pbq', bi), 512, 0, 512 * b, qT[:, j, 512 * b:512 * (b + 1)], ('qT', j, b))
        for g in range(2):
            wp, wk = load_w(512 + 64 * g, 64, dup=True)
            for b in range(-1, NQB):
                bi = pctr[0] % 2
                pctr[0] += 1
                t0, n = (0, 256) if b < 0 else (CTX + 512 * b, 512)
                for kc in range(8):
                    mm(pb[bi][:, 0:n], wp[:, kc, :], hT[:, kc, t0:t0 + n], kc == 0, kc == 7,
                       [wk] + [('hT', ti_, kc) for ti_ in range(t0 // 128, (t0 + n) // 128)], [('pbq', bi)])
                qk_epilogue(pb[bi], ('pbq', bi), n, 1, None if b < 0 else 512 * b, kTd[:, g, t0:t0 + n], ('kTd', g, b))
        wp, wk = load_w(640)
        for ti in range(NT):
            bi = 4 + ti % 2
            for kc in range(8):
                mm(pb[bi][:, 0:128], hT[:, kc, ti * 128:(ti + 1) * 128], wp[:, kc, :], kc == 0, kc == 7, [wk, ('hT', ti, kc)], [('pbv', bi)])
            act(lambda ti=ti, bi=bi: A.copy(out=vext[:, ti, :, 0:64], in_=pb[bi][:, 0:128].rearrange("p (g d) -> p g d", d=64)),
                [('pbv', bi), 'vext'], [('vx', ti)])
        if "qT" in dbg_t:
            dq = AR.alloc([128, 4, 512], F32)
            dve(lambda: V.tensor_copy(out=dq, in_=qT[:, :, 0:512]), [('qT', j_, 0) for j_ in range(4)], ['dq'])
            dump("qT", dq, ['dq'])
        if "kT" in dbg_t:
            dk = AR.alloc([128, 2, 512], F32)
            dve(lambda: V.tensor_copy(out=dk, in_=kTd[:, :, 0:512]), [('kTd', g_, b_) for g_ in range(2) for b_ in (-1, 0)], ['dk'])
            dump("kT", dk, ['dk'])
        if "vext" in dbg_t:
            dv = AR.alloc([128, 2, 132], F32)
            dve(lambda: V.tensor_copy(out=dv, in_=vext[:, 1:3, :, :].rearrange("p a g d -> p a (g d)")), [('vx', 1), ('vx', 2), 'vext1'], ['dv'])
            dump("vext", dv, ['dv'])
        S.barrier()
        AR.release(mC1)
        if stop_after <= 2:
            S.finish()
            return nc

        mD = AR.mark()
        NSB = 3
        pbuf = [AR.alloc([128, 512], BF16) for _ in range(NSB)]
        rsb = AR.alloc([128, 512], F32)
        bcs = AR.alloc([64, 512], F32)
        NKT = int(os.environ.get('NKT', NT))
        pairs = [(j, qb) for j in range(int(os.environ.get('NJ', 4))) for qb in range(NQB)]
        its = [(p, kt, half) for p in range(len(pairs)) for kt in range(NKT) for half in range(2)]
        sbank = [pb[0], pb[1], pb[3]]

        def acc_of(p, half):
            return 4 + 2 * (p % 2) + half

        def emit_score(n):
            p, kt, half = its[n]
            j, qb = pairs[p]
            hp = slice(0, 64) if half == 0 else slice(64, 128)
            si = n % 3
            mm(sbank[si], kTd[hp, j // 2, kt * 128:(kt + 1) * 128], qT[hp, j, qb * 512:(qb + 1) * 512], True, True, [], [('pbs', si)])

        def emit_exp(n):
            si = n % 3
            act(lambda si=si: A.activation(out=pbuf[si], in_=sbank[si], func=AF.Exp, scale=0.125), [('pbs', si)], [('pbuf', si)])

        def emit_pv(n):
            p, kt, half = its[n]
            j, qb = pairs[p]
            si = n % 3
            ai = acc_of(p, half)
            mm(pb[ai][0:65, :], vext[:, kt, j // 2, 0:65], pbuf[si], kt == 0, kt == NKT - 1, [('pbuf', si)], [('acc', ai)])

        def emit_final(p, half):
            j, qb = pairs[p]
            hp = slice(0, 64) if half == 0 else slice(64, 128)
            ai = acc_of(p, half)
            acc = pb[ai]
            dve(lambda: V.reciprocal(out=rsb[64:65, :], in_=acc[64:65, :]), [('acc', ai)], ['rsb'])
            mm(pb[2][0:64, :], onesf[64:65, 0:64], rsb[64:65, :], True, True, ['rsb'], ['pb2'])
            act(lambda: A.copy(out=bcs, in_=pb[2][0:64, :]), ['pb2'], ['bcs'])
            dve(lambda: V.tensor_tensor(out=attT[hp, j, qb * 512:(qb + 1) * 512], in0=acc[0:64, :], in1=bcs, op=ALU.mult),
                [('acc', ai), 'bcs'], [('attT', j, half, qb)])

        NIT = len(its)
        for n in range(min(2, NIT)):
            emit_score(n)
        pending = []
        for m in range(NIT // 2):
            n0, n1 = 2 * m, 2 * m + 1
            if n0 + 2 < NIT:
                emit_score(n0 + 2)
            emit_exp(n0)
            if n1 + 2 < NIT:
                emit_score(n1 + 2)
            emit_pv(n0)
            emit_exp(n1)
            emit_pv(n1)
            p, kt, _ = its[n0]
            if pending and kt == min(3, NKT - 1):
                for (pp, hh) in pending:
                    emit_final(pp, hh)
                pending = []
            if kt == NKT - 1:
                for (pp, hh) in pending:
                    emit_final(pp, hh)
                pending = [(p, 0), (p, 1)]
        for (pp, hh) in pending:
            emit_final(pp, hh)
        if "attT" in dbg_t:
            da = AR.alloc([128, 4, 512], F32)
            dve(lambda: V.tensor_copy(out=da, in_=attT[:, :, 0:512]), [('attT', j_, h_, 0) for j_ in range(4) for h_ in range(2)], ['da'])
            dump("attT", da, ['da'])
        S.barrier()
        AR.release(mQ)
        if stop_after <= 3:
            S.finish()
            return nc

        zt = AR.alloc([128, 16, 512], BF16)
        dnoT = AR.alloc([128, 4, SEQ], BF16)
        mDN = AR.mark()
        dnT = AR.alloc([128, 12, T], BF16)
        gb = AR.alloc([128, NT, 16], F32)
        nbeta = AR.alloc([128, NT, 8], F32)
        mC2 = AR.mark()
        if os.environ.get('ARDBG'): print('C2 persistent end', AR.off, 'top', getattr(AR, 'top', None))
        stg = [AR.alloc([128, 8, 128], F32)] * 2
        wpc = [AR.alloc([128, 8, 128], BF16) for _ in range(2)]
        convw = AR.alloc([128, 60], F32)
        diagc = [AR.alloc([128, 5, 128], BF16) for _ in range(2)]
        rawp = [AR.alloc([128, 2312], BF16)] * 2
        cx = [AR.alloc([128, 512], F32) for _ in range(2)]
        sqd = [AR.alloc([128, 512], BF16)] * 2
        rq2 = [AR.alloc([128, 512], F32)] * 2
        gbraw = AR.alloc([128, NT, 16], F32)
        alog = AR.alloc([128, NT, 8], F32)
        dtb = AR.alloc([128, NT, 8], F32)
        S.dma('sp', convw, dr["convT"], writes=['convw'])
        S.dma('sp', alog, dr["alog"].rearrange("p (a b) -> p a b", b=8), writes=['alog'])
        S.dma('sp', dtb, dr["dtb"].rearrange("p (a b) -> p a b", b=8), writes=['dtb'])
        pool(lambda: P.memset(rawp[0], 0.0), [], [('rawp', 0)])
        cctr = [0]
        for m in range(int(os.environ.get('NDN', 12))):
            wp, wk = load_w(768 + 128 * m)
            rp, dgc = rawp[0], diagc[m % 2]
            for jt in range(5):
                dve(lambda jt=jt, m=m, dgc=dgc: V.tensor_scalar(out=dgc[:, jt, :], in0=ident, scalar1=convw[:, m * 5 + jt:m * 5 + jt + 1], scalar2=None,
                                                            op0=ALU.mult), ['ident', 'convw'], [('diagc', m % 2)])
            for b in range(-1, 4):
                t0, n = (0, 256) if b < 0 else (CTX + 512 * b, 512)
                off = 2 if b < 0 else 262 + 512 * b
                bi = pctr[0] % 2
                pctr[0] += 1
                for kc in range(8):
                    mm(pb[bi][:, 0:n], wp[:, kc, :], hT[:, kc, t0:t0 + n], kc == 0, kc == 7, [wk], [('pbq', bi)])
                act(lambda rp=rp, off=off, n=n, bi=bi: A.copy(out=rp[:, off:off + n], in_=pb[bi][:, 0:n]), [('pbq', bi)], [('rawp', 0)])
            for b in range(-1, 4):
                t0, n = (0, 256) if b < 0 else (CTX + 512 * b, 512)
                off = 2 if b < 0 else 262 + 512 * b
                ci = cctr[0] % 2
                cctr[0] += 1
                bank = pb[2 + ci]
                for jt in range(5):
                    mm(bank[:, 0:n], dgc[:, jt, :], rp[:, off + jt - 2:off + jt - 2 + n], jt == 0, jt == 4, [('diagc', m % 2), ('rawp', 0)], [('pbc', ci)])
                dst = dnT[:, m, t0:t0 + n]
                if m >= 8:
                    act(lambda dst=dst, bank=bank, n=n: A.activation(out=dst, in_=bank[:, 0:n], func=AF.Silu), [('pbc', ci)], [('dnT', m, b)])
                else:
                    act(lambda bank=bank, n=n, ci=ci: A.activation(out=cx[ci][:, 0:n], in_=bank[:, 0:n], func=AF.Silu), [('pbc', ci)], [('cx', ci)])
                    pool(lambda n=n, ci=ci: P.tensor_tensor(out=sqd[ci][:, 0:n], in0=cx[ci][:, 0:n], in1=cx[ci][:, 0:n], op=ALU.mult), [('cx', ci)], [('sqd', 0)])
                    mm(pb[4 + ci][:, 0:n], onesb, sqd[ci][:, 0:n], True, True, [('sqd', 0), 'onesb'], [('pbn', ci)])
                    act(lambda n=n, ci=ci: A.activation(out=rq2[ci][:, 0:n], in_=pb[4 + ci][:, 0:n], func=AF.Sqrt, bias=EPS), [('pbn', ci)], [('rq2', 0)])
                    dve(lambda n=n, ci=ci: V.reciprocal(out=rq2[ci][:, 0:n], in_=rq2[ci][:, 0:n]), [('rq2', 0)], [('rq2', 0)])
                    sc = 128 ** -0.5 if m < 4 else 1.0
                    dve(lambda dst=dst, n=n, ci=ci, sc=sc: V.scalar_tensor_tensor(out=dst, in0=cx[ci][:, 0:n], scalar=sc, in1=rq2[ci][:, 0:n], op0=ALU.mult, op1=ALU.mult),
                        [('cx', ci), ('rq2', 0)], [('dnT', m, b)])
        for zc in range(4):
            wp, wk = load_w(2304 + 128 * zc)
            for tl in range(16):
                ti = tl + 2
                bi = pctr[0] % 2
                pctr[0] += 1
                for kc in range(8):
                    mm(pb[bi][:, 0:128], hT[:, kc, ti * 128:(ti + 1) * 128], wp[:, kc, :], kc == 0, kc == 7, [wk], [('pbq', bi)])
                act(lambda tl=tl, zc=zc, bi=bi: A.copy(out=zt[:, tl, zc * 128:(zc + 1) * 128], in_=pb[bi][:, 0:128]), [('pbq', bi)], [('zt', tl, zc)])
        wp, wk = load_w(2816, 16)
        for ti in range(NT):
            bi = pctr[0] % 2
            pctr[0] += 1
            for kc in range(8):
                mm(pb[bi][:, 0:16], hT[:, kc, ti * 128:(ti + 1) * 128], wp[:, kc, 0:16], kc == 0, kc == 7, [wk], [('pbq', bi)])
            dve(lambda ti=ti, bi=bi: V.tensor_copy(out=gbraw[:, ti, :], in_=pb[bi][:, 0:16]), [('pbq', bi)], ['gbraw'])
        act(lambda: A.activation(out=gb[:, :, 0:8], in_=gbraw[:, :, 0:8], func=AF.Sigmoid), ['gbraw'], ['gb_b'])
        dve(lambda: V.tensor_scalar(out=nbeta, in0=gb[:, :, 0:8], scalar1=-1.0, scalar2=None, op0=ALU.mult), ['gb_b'], ['nbeta'])
        dve(lambda: V.tensor_tensor(out=gbraw[:, :, 8:16], in0=gbraw[:, :, 8:16], in1=dtb, op=ALU.add), ['gbraw', 'dtb'], ['gbraw2'])
        act(lambda: A.activation(out=gbraw[:, :, 8:16], in_=gbraw[:, :, 8:16], func=AF.Exp), ['gbraw2'], ['gbraw3'])
        act(lambda: A.activation(out=gbraw[:, :, 8:16], in_=gbraw[:, :, 8:16], func=AF.Ln, bias=1.0), ['gbraw3'], ['gbraw4'])
        act(lambda: A.activation(out=alog, in_=alog, func=AF.Exp), ['alog'], ['aexp'])
        dve(lambda: V.scalar_tensor_tensor(out=gb[:, :, 8:16], in0=gbraw[:, :, 8:16], scalar=-1.0, in1=alog, op0=ALU.mult, op1=ALU.mult),
            ['gbraw4', 'aexp'], ['gb_g'])
        if "dnT" in dbg_t:
            dd = AR.alloc([128, 12, 256], F32)
            dve(lambda: V.tensor_copy(out=dd, in_=dnT[:, :, 128:384]), [('dnT', m_, b_) for m_ in range(12) for b_ in (-1, 0)], ['dd'])
            dump("dnT", dd, ['dd'])
        dump("gb", gb, ['gb_b', 'gb_g'])
        S.barrier()
        AR.release(mC2)
        AR.release_top()
        if stop_after <= 4:
            S.finish()
            return nc

        osum = AR.alloc([128, 16, 512], F32)
        mE = AR.mark()
        NU = 4
        Sst = AR.alloc([128, 8, 128], F32)
        Sb = AR.alloc([128, 8, 128], BF16)
        ktok = [AR.alloc([128, 128], BF16) for _ in range(NU)]
        vtok = [AR.alloc([128, 128], BF16) for _ in range(NU)]
        KQ = [AR.alloc([128, 256], F32) for _ in range(NU)]
        gmx = [AR.alloc([128, 130], F32) for _ in range(NU)]
        Es = [AR.alloc([128, 128], F32) for _ in range(NU)]
        EmT = [AR.alloc([128, 128], F32) for _ in range(NU)]
        sc4 = [AR.alloc([128, 4], F32) for _ in range(NU)]
        Pm = [[AR.alloc([128, 128], F32) for _ in range(2)] for _ in range(NU)]
        PTm = [[AR.alloc([128, 128], F32) for _ in range(2)] for _ in range(NU)]
        TT = [AR.alloc([128, 128], F32) for _ in range(NU)]
        rv = [AR.alloc([128, 128], F32) for _ in range(NU)]
        rw = [AR.alloc([128, 128], F32) for _ in range(NU)]
        uu = [AR.alloc([128, 128], F32) for _ in range(NU)]
        wTb = [AR.alloc([128, 128], BF16) for _ in range(NU)]
        kdec = [AR.alloc([128, 128], BF16) for _ in range(NU)]
        dgb = [AR.alloc([128, 128], BF16) for _ in range(NU)]
        qdT = [AR.alloc([128, 128], BF16) for _ in range(NU)]
        qkT = [AR.alloc([128, 128], BF16) for _ in range(NU)]
        glb = [AR.alloc([128, 2], F32) for _ in range(NU)]
        vn = [AR.alloc([128, 128], BF16) for _ in range(NU)]
        dgf = Es
        pool(lambda: P.memset(Sst, 0.0), [], [('S', c_) for c_ in range(8)])
        pool(lambda: P.memset(Sb, 0.0), [], [('Sb', c_) for c_ in range(8)])
        pool(lambda: P.memset(osum, 0.0), [], [('osum', tl_, h_) for tl_ in range(16) for h_ in range(4)])

        def pe1(o, l, r_, rd, wr):
            return S.op('pe', lambda: TE.matmul(o, lhsT=l, rhs=r_, start=True, stop=True), rd, wr, pe_accum=True)

        NSTEP = int(os.environ.get('NSTEP', NT))
        fwd_order = list(range(NT))
        bwd_order = [1, 0] + list(range(NT - 1, 1, -1))

        def unit_gen(h, d, ti, i):
            B0, B2 = pb[2 * i], pb[2 * i + 1]
            PT = B2[:, 384:512].bitcast(BF16).rearrange("p (a b) -> p a b", b=128)
            k0, k2 = 'pbE0_%d' % i, 'pbE2_%d' % i
            kt_ = k2
            hi = i
            lat = ti >= 2
            tl = ti - 2
            tsl = slice(ti * 128, (ti + 1) * 128)
            c = d * 4 + h
            col = c
            Mtri = dnm[:, 0, :] if d == 0 else dnm[:, 2, :]
            maskA = dnm[:, 1, :] if d == 0 else dnm[:, 3, :]
            maskQK = Mtri
            gcol = gb[:, ti, 8 + col:9 + col]
            bcol = gb[:, ti, col:col + 1]
            nbcol = nbeta[:, ti, col:col + 1]
            kT_t = dnT[:, 4 + h, tsl]
            qT_t = dnT[:, h, tsl]
            vT_t = dnT[:, 8 + h, tsl]
            pk, pv = 0, 1
            tr(PT[:, pk, :], kT_t, identb, [], [kt_])
            tr(PT[:, pv, :], vT_t, identb, [], [kt_])
            pe1(B0[:, 0:128], kT_t, kT_t, [], [k0])
            if lat:
                pe1(B0[:, 128:256], kT_t, qT_t, [], [k0])
            dve(lambda: V.tensor_scalar(out=gmx[i], in0=msx[:, d, :], scalar1=gcol, scalar2=None, op0=ALU.mult), [], [('gmx', i)])
            yield
            act(lambda: A.copy(out=ktok[hi], in_=PT[:, pk, :]), [kt_], [('ktok', hi)])
            act(lambda: A.copy(out=vtok[hi], in_=PT[:, pv, :]), [kt_], [('vtok', hi)])
            act(lambda: A.copy(out=KQ[hi], in_=B0[:, 0:256]), [k0], [('KQ', hi)])
            pe1(B2[:, 0:130], Mtri, gmx[i], [('gmx', i)], [k2])
            pe1(B2[:, 130:132], blk64f, gmx[i][:, 128:130], [('gmx', i)], [k2])
            if lat:
                pe1(B2[:, 256:384], gmx[i][:, 0:128], Mtri, [('gmx', i)], [k2])
            yield
            act(lambda: A.activation(out=Es[i], in_=B2[:, 0:128], func=AF.Exp), [k2], [('Es', i)])
            act(lambda: A.activation(out=sc4[i][:, 0:1], in_=B2[:, 128:129], func=AF.Exp), [k2], [('egc', i)])
            act(lambda: A.copy(out=sc4[i][:, 1:2], in_=B2[:, 130:131]), [k2], [('tots', i)])
            act(lambda: A.activation(out=sc4[i][:, 2:3], in_=B2[:, 128:129], func=AF.Exp, scale=-1.0, bias=sc4[i][:, 1:2]),
                [k2, ('tots', i)], [('erest', i)])
            act(lambda: A.activation(out=sc4[i][:, 3:4], in_=sc4[i][:, 1:2], func=AF.Exp), [('tots', i)], [('etot', i)])
            if lat:
                act(lambda: A.activation(out=EmT[i], in_=B2[:, 256:384], func=AF.Exp), [k2], [('EmT', i)])
                pool(lambda: P.tensor_tensor(out=EmT[i], in0=EmT[i], in1=maskQK, op=ALU.mult), [('EmT', i)], [('EmT', i)])
            pool(lambda: P.tensor_tensor(out=Es[i], in0=Es[i], in1=maskA, op=ALU.mult), [('Es', i)], [('Es', i)])
            yield
            dve(lambda: V.scalar_tensor_tensor(out=Pm[i][0], in0=KQ[hi][:, 0:128], scalar=nbcol, in1=Es[i], op0=ALU.mult, op1=ALU.mult),
                [('KQ', hi), ('Es', i)], [('P', i, 0)])
            tr(B2[:, 0:128], Pm[i][0], ident, [('P', i, 0)], [k2])
            yield
            dve(lambda: V.tensor_copy(out=PTm[i][0], in_=B2[:, 0:128]), [k2], [('PT', i, 0)])
            dve(lambda: V.tensor_tensor(out=TT[i], in0=B2[:, 0:128], in1=ident, op=ALU.add), [k2], [('TT', i)])
            for k in range(5):
                cu, nx = k % 2, 1 - k % 2
                pe1(B0[:, 0:128], PTm[i][cu], Pm[i][cu], [('PT', i, cu), ('P', i, cu)], [k0])
                if k < 4:
                    pe1(B0[:, 128:256], Pm[i][cu], PTm[i][cu], [('PT', i, cu), ('P', i, cu)], [k0])
                yield
                if k < 4:
                    act(lambda nx=nx: A.copy(out=Pm[i][nx], in_=B0[:, 0:128]), [k0], [('P', i, nx)])
                    act(lambda nx=nx: A.copy(out=PTm[i][nx], in_=B0[:, 128:256]), [k0], [('PT', i, nx)])
                else:
                    act(lambda nx=nx: A.copy(out=Pm[i][nx], in_=B0[:, 0:128]), [k0], [('P', i, nx)])
                pe1(B2[:, 0:128], Pm[i][nx], TT[i], [('P', i, nx), ('TT', i)], [k2])
                yield
                dve(lambda: V.tensor_tensor(out=TT[i], in0=B2[:, 0:128], in1=TT[i], op=ALU.add), [k2, ('TT', i)], [('TT', i)])
            dve(lambda: V.tensor_scalar(out=rv[i], in0=vtok[hi], scalar1=bcol, scalar2=None, op0=ALU.mult), [('vtok', hi)], [('rv', i)])
            dve(lambda: V.tensor_scalar(out=rw[i], in0=ktok[hi], scalar1=bcol, scalar2=None, op0=ALU.mult), [('ktok', hi)], [('rw', i)])
            dve(lambda: V.tensor_scalar(out=rw[i], in0=rw[i], scalar1=sc4[i][:, 0:1], scalar2=None, op0=ALU.mult), [('rw', i), ('egc', i)], [('rw', i)])
            dve(lambda: V.tensor_scalar(out=kdec[i], in0=ktok[hi], scalar1=sc4[i][:, 2:3], scalar2=None, op0=ALU.mult),
                [('ktok', hi), ('erest', i)], [('kdec', i)])
            if lat:
                dve(lambda: V.tensor_scalar(out=dgb[i], in0=ident, scalar1=sc4[i][:, 0:1], scalar2=None, op0=ALU.mult), [('egc', i)], [('dgb', i)])
            pe1(B0[:, 0:128], TT[i], rv[i], [('TT', i), ('rv', i)], [k0])
            pe1(B0[:, 128:256], rw[i], TT[i], [('TT', i), ('rw', i)], [k0])
            pe1(B0[:, 256:257], selrow[:, 0, :], sc4[i][:, 3:4], [('etot', i)], [k0])
            pe1(B0[:, 257:258], selrow[:, 1, :], sc4[i][:, 3:4], [('etot', i)], [k0])
            if lat:
                pe1(B0[:, 384:512], onesb, dgb[i], [('dgb', i)], [k0])
            yield
            act(lambda: A.copy(out=uu[i], in_=B0[:, 0:128]), [k0], [('uu', i)])
            act(lambda: A.copy(out=wTb[i], in_=B0[:, 128:256]), [k0], [('wTb', i)])
            act(lambda: A.copy(out=glb[i], in_=B0[:, 256:258]), [k0], [('glb', i)])
            if lat:
                act(lambda: A.copy(out=dgf[i], in_=B0[:, 384:512]), [k0], [('Es', i)])
                dve(lambda: V.tensor_tensor(out=qdT[i], in0=qT_t, in1=dgf[i], op=ALU.mult), [('Es', i)], [('qdT', i)])
                dve(lambda: V.tensor_tensor(out=qkT[i], in0=KQ[hi][:, 128:256], in1=EmT[i], op=ALU.mult), [('KQ', hi), ('EmT', i)], [('qkT', i)])
            yield
            for pi_ in ((0, 1) if d == 0 else (1, 0)):
                R = slice(pi_ * 64, pi_ * 64 + 64)
                pe1(B2[:, 0:128], wTb[i], Sb[:, c, :], [('wTb', i), ('Sb', c)], [k2])
                if lat:
                    S.op('pe', lambda: TE.matmul(B2[:, 128:256], lhsT=qdT[i], rhs=Sb[:, c, :], start=True, stop=False),
                         [('qdT', i), ('Sb', c)], [k2], pe_accum=True)
                yield
                dve(lambda R=R: V.tensor_tensor(out=vn[i][R, :], in0=uu[i][R, :], in1=B2[R, 0:128], op=ALU.subtract), [('uu', i), k2], [('vn', i)])
                if lat:
                    S.op('pe', lambda R=R: TE.matmul(B2[:, 128:256], lhsT=qkT[i][R, :], rhs=vn[i][R, :], start=False, stop=True),
                         [('qkT', i), ('vn', i)], [k2], pe_accum=True)
                pe1(B2[:, 256:384], kdec[i][R, :], vn[i][R, :], [('kdec', i), ('vn', i)], [k2])
                yield
                if lat:
                    dve(lambda R=R: V.tensor_tensor(out=osum[R, tl, h * 128:(h + 1) * 128], in0=osum[R, tl, h * 128:(h + 1) * 128],
                                                    in1=B2[R, 128:256], op=ALU.add), [k2, ('osum', tl, h)], [('osum', tl, h)])
                dve(lambda pi_=pi_: V.scalar_tensor_tensor(out=Sst[:, c, :], in0=Sst[:, c, :], scalar=glb[i][:, pi_:pi_ + 1], in1=B2[:, 256:384],
                                                           op0=ALU.mult, op1=ALU.add), [k2, ('glb', i), ('S', c)], [('S', c)])
                act(lambda: A.copy(out=Sb[:, c, :], in_=Sst[:, c, :]), [('S', c)], [('Sb', c)])
                yield

        for step in range(NSTEP):
            chains = [(h, d) for h in range(4) for d in range(2)]
            for p0 in range(0, 8, NU):
                gens = []
                for slot, (h, d) in enumerate(chains[p0:p0 + NU]):
                    ti = fwd_order[step] if d == 0 else bwd_order[step]
                    gens.append(unit_gen(h, d, ti, slot))
                alive = list(gens)
                while alive:
                    for gobj in list(alive):
                        try:
                            next(gobj)
                        except StopIteration:
                            alive.remove(gobj)
        if "S0" in dbg_t:
            dump("S0", Sst[:, int(os.environ.get('DBGC', 0)), :], [('S', c_) for c_ in range(8)])
        if "osum" in dbg_t:
            dump("osum", osum[:, 0:4, :], [('osum', tl_, h_) for tl_ in range(4) for h_ in range(4)])
        S.barrier()
        AR.release(mE)
        if stop_after <= 5:
            S.finish()
            return nc

        onw = AR.alloc([128, 128], F32)
        sq2 = [AR.alloc([128, 512], F32) for _ in range(2)]
        sse = AR.alloc([128, 16, 4], F32)
        szl = [AR.alloc([128, 512], F32) for _ in range(2)]
        ydn = [AR.alloc([128, 512], BF16) for _ in range(2)]
        S.dma('sp', onw, dr["onw"], writes=['onw'])
        for tl in range(16):
            i = tl % 2
            okeys = [('osum', tl, h_) for h_ in range(4)]
            pool(lambda tl=tl, i=i: P.tensor_tensor(out=sq2[i], in0=osum[:, tl, :], in1=osum[:, tl, :], op=ALU.mult), okeys, [('sq2', i)])
            dve(lambda tl=tl, i=i: V.tensor_reduce(out=sse[:, tl, :], in_=sq2[i].rearrange("p (h d) -> p h d", d=128), axis=AX.X, op=ALU.add),
                [('sq2', i)], [('sse', tl)])
            dve(lambda tl=tl: V.tensor_scalar(out=sse[:, tl, :], in0=sse[:, tl, :], scalar1=1.0 / 128, scalar2=EPS, op0=ALU.mult, op1=ALU.add),
                [('sse', tl)], [('sse', tl)])
            act(lambda tl=tl: A.activation(out=sse[:, tl, :], in_=sse[:, tl, :], func=AF.Sqrt), [('sse', tl)], [('sse', tl)])
            dve(lambda tl=tl: V.reciprocal(out=sse[:, tl, :], in_=sse[:, tl, :]), [('sse', tl)], [('sse', tl)])
            act(lambda tl=tl, i=i: A.activation(out=szl[i], in_=zt[:, tl, :], func=AF.Silu), [], [('szl', i)])
            for h in range(4):
                dve(lambda tl=tl, i=i, h=h: V.scalar_tensor_tensor(out=sq2[i][:, h * 128:(h + 1) * 128], in0=osum[:, tl, h * 128:(h + 1) * 128],
                                                                 scalar=sse[:, tl, h:h + 1], in1=onw, op0=ALU.mult, op1=ALU.mult),
                    okeys + [('sse', tl), 'onw', ('sq2', i)], [('sq2', i)])
            dve(lambda i=i: V.tensor_tensor(out=ydn[i], in0=sq2[i], in1=szl[i], op=ALU.mult), [('sq2', i), ('szl', i)], [('ydn', i)])
            for h in range(4):
                tr(pbt[1][:, h, :], ydn[i][:, h * 128:(h + 1) * 128], identb, [('ydn', i)], ['pbt1'])
            act(lambda tl=tl: A.copy(out=dnoT[:, :, tl * 128:(tl + 1) * 128], in_=pbt[1][:, 0:4, :]), ['pbt1'], [('dnoT', tl)])
        if "dnoT" in dbg_t:
            ddn = AR.alloc([128, 4, 512], F32)
            dve(lambda: V.tensor_copy(out=ddn, in_=dnoT[:, :, 0:512]), [('dnoT', tl_) for tl_ in range(4)], ['ddn'])
            dump("dnoT", ddn, ['ddn'])
        S.barrier()
        AR.release(mDN)
        if stop_after <= 6:
            S.finish()
            return nc

        h2 = AR.alloc_top([128, 16, 1024], BF16)
        aff = AR.alloc_top([128, 16, 16], F32)
        posm = AR.alloc_top([128, 16, 16], F32)
        gmb = AR.alloc_top([128, 16, 32], BF16)
        posTb = AR.alloc_top([16, 2048], BF16)
        mF = AR.mark()
        woutb = AR.alloc([128, 8, 1024], BF16)
        rws = AR.alloc([128, 8, 16], F32)
        rwb = AR.alloc([128, 8, 16], BF16)
        xt2 = [AR.alloc([128, 1024], F32) for _ in range(2)]
        x1t = [AR.alloc([128, 1024], F32) for _ in range(2)]
        tmp2 = AR.alloc([128, 1024], F32)
        junk2 = AR.alloc([128, 1024], F32)
        ss2 = AR.alloc([128, 16], F32)
        h2Tt = [AR.alloc([128, 8, 128], BF16) for _ in range(2)]
        smx = AR.alloc([128, 16, 4], F32)
        ee = AR.alloc([128, 16], F32)
        woutv = dr["w_out"].rearrange("(kc p) n -> p kc n", p=128)
        for q in range(4):
            S.dma('pool', woutb[:, 2 * q:2 * q + 2, :], woutv[:, 2 * q:2 * q + 2, :], writes=[('woutb', q)])
        S.dma('sp', rws, dr["router_w"].rearrange("(kc p) e -> p kc e", p=128), writes=['rws'])
        dve(lambda: V.tensor_copy(out=rwb, in_=rws), ['rws'], ['rwb'])
        for tl in range(16):
            i = tl % 2
            tsl = slice(tl * 128, (tl + 1) * 128)
            S.dma('sp', xt2[i], dr["x"][tsl, :], writes=[('xt2', i)])
            for half in range(2):
                hs = slice(half * 512, (half + 1) * 512)
                for kc in range(8):
                    lhs = attT[:, kc, tsl] if kc < 4 else dnoT[:, kc - 4, tsl]
                    mm(pb[half], lhs, woutb[:, kc, hs], kc == 0, kc == 7, [('woutb', kc // 2)], [('pby', half)])
                dve(lambda i=i, half=half, hs=hs: V.tensor_tensor(out=x1t[i][:, hs], in0=pb[half], in1=g1row[:, hs], op=ALU.mult),
                    [('pby', half)], [('x1t', i, half)])
                dve(lambda i=i, hs=hs: V.tensor_tensor(out=x1t[i][:, hs], in0=x1t[i][:, hs], in1=xt2[i][:, hs], op=ALU.add),
                    [('x1t', i, half), ('xt2', i)], [('x1t', i, half)])
            S.dma('pool', x1s[tsl, :], x1t[i], reads=[('x1t', i, 0), ('x1t', i, 1)], writes=[('x1s', tl)])
            xk = [('x1t', i, 0), ('x1t', i, 1)]
            act(lambda i=i, tl=tl: A.activation(out=junk2, in_=x1t[i], func=AF.Square, accum_out=ss2[:, tl:tl + 1]), xk, ['junk2', ('ss2', tl)])
            dve(lambda tl=tl: V.tensor_scalar(out=ss2[:, tl:tl + 1], in0=ss2[:, tl:tl + 1], scalar1=1.0 / D, scalar2=EPS, op0=ALU.mult, op1=ALU.add),
                [('ss2', tl)], [('ss2', tl)])
            act(lambda tl=tl: A.activation(out=ss2[:, tl:tl + 1], in_=ss2[:, tl:tl + 1], func=AF.Sqrt), [('ss2', tl)], [('ss2', tl)])
            dve(lambda tl=tl: V.reciprocal(out=ss2[:, tl:tl + 1], in_=ss2[:, tl:tl + 1]), [('ss2', tl)], [('ss2', tl)])
            dve(lambda i=i, tl=tl: V.scalar_tensor_tensor(out=tmp2, in0=x1t[i], scalar=ss2[:, tl:tl + 1], in1=s2row, op0=ALU.mult, op1=ALU.mult),
                xk + [('ss2', tl)], ['tmp2'])
            dve(lambda tl=tl: V.tensor_tensor(out=h2[:, tl, :], in0=tmp2, in1=b2row, op=ALU.add), ['tmp2'], [('h2', tl)])
            for kc in range(8):
                tr(pbt[i][:, kc, :], h2[:, tl, kc * 128:(kc + 1) * 128], identb, [('h2', tl)], [('pbt', i)])
            act(lambda i=i: A.copy(out=h2Tt[i], in_=pbt[i]), [('pbt', i)], [('h2Tt', i)])
            for kc in range(8):
                mm(pb[2 + i][:, 0:16], h2Tt[i][:, kc, :], rwb[:, kc, :], kc == 0, kc == 7, [('h2Tt', i), 'rwb'], [('pbr', i)])
            dve(lambda i=i, tl=tl: V.tensor_reduce(out=smx[:, tl, 0:1], in_=pb[2 + i][:, 0:16], axis=AX.X, op=ALU.max), [('pbr', i)], [('smx', tl)])
            dve(lambda tl=tl: V.tensor_scalar(out=smx[:, tl, 1:2], in0=smx[:, tl, 0:1], scalar1=-1.0, scalar2=None, op0=ALU.mult), [('smx', tl)], [('smx', tl)])
            act(lambda i=i, tl=tl: A.activation(out=ee, in_=pb[2 + i][:, 0:16], func=AF.Exp, bias=smx[:, tl, 1:2], accum_out=smx[:, tl, 2:3]),
                [('pbr', i), ('smx', tl)], ['ee', ('smx', tl)])
            dve(lambda tl=tl: V.reciprocal(out=smx[:, tl, 3:4], in_=smx[:, tl, 2:3]), [('smx', tl)], [('smx', tl)])
            dve(lambda tl=tl: V.tensor_scalar(out=aff[:, tl, :], in0=ee, scalar1=smx[:, tl, 3:4], scalar2=None, op0=ALU.mult), ['ee', ('smx', tl)], [('aff', tl)])
        if "x1" in dbg_t:
            dump("x1", x1t[0], [('x1t', 0, 0), ('x1t', 0, 1)])
        dump("aff", aff, [('aff', tl_) for tl_ in range(16)])
        S.barrier()
        AR.release(mF)
        if stop_after <= 7:
            S.finish()
            return nc

        mF2 = AR.mark()
        affT = AR.alloc([16, 2048], F32)
        work = AR.alloc([16, 2048], F32)
        maskT = AR.alloc([16, 2048], F32)
        cum = AR.alloc([16, 2048], F32)
        ones16 = AR.alloc([16, 2048], F32)
        posTf = AR.alloc([16, 2048], F32)
        m8 = AR.alloc([16, 8], F32)
        mtok = AR.alloc([128, 256], F32)
        gm = AR.alloc([128, 256], F32)
        gml = AR.alloc([128, 256], F32)
        for tl in range(16):
            q = tl // 4
            tr(pb[q][0:16, (tl % 4) * 128:(tl % 4 + 1) * 128], aff[:, tl, :], ident, [], [('pbA', q)])
        for q in range(4):
            act(lambda q=q: A.copy(out=affT[:, q * 512:(q + 1) * 512], in_=pb[q][0:16, :]), [('pbA', q)], ['affT'])
        pool(lambda: P.tensor_copy(out=work, in_=affT), ['affT'], ['work'])
        pool(lambda: P.memset(ones16, 1.0), [], ['ones16'])
        for it_ in range(CAP // 8):
            dve(lambda: V.max(out=m8, in_=work), ['work'], ['m8'])
            dve(lambda: V.match_replace(out=work, in_to_replace=m8, in_values=work, imm_value=-1.0), ['m8', 'work'], ['work'])
        dve(lambda: V.tensor_scalar(out=maskT, in0=affT, scalar1=m8[:, 7:8], scalar2=None, op0=ALU.is_ge), ['affT', 'm8'], ['maskT'])
        dve(lambda: V.tensor_tensor_scan(out=cum, data0=ones16, data1=maskT, initial=0.0, op0=ALU.mult, op1=ALU.add), ['ones16', 'maskT'], ['cum'])
        dve(lambda: V.tensor_tensor(out=posTf, in0=cum, in1=maskT, op=ALU.mult), ['cum', 'maskT'], ['posTf'])
        dve(lambda: V.tensor_scalar(out=posTf, in0=posTf, scalar1=-1.0, scalar2=None, op0=ALU.add), ['posTf'], ['posTf'])
        act(lambda: A.copy(out=posTb, in_=posTf), ['posTf'], ['posTb'])
        for tl in range(16):
            tr(pb[4][:, tl * 16:(tl + 1) * 16], posTf[:, tl * 128:(tl + 1) * 128], ident[0:16, 0:16], ['posTf'], ['pb4'])
        posm_v = posm.rearrange("p t e -> p (t e)")
        aff_v = aff.rearrange("p t e -> p (t e)")
        gmb_v = gmb.rearrange("p t (e two) -> p (t e) two", two=2)
        act(lambda: A.copy(out=posm_v, in_=pb[4][:, 0:256]), ['pb4'], ['posm'])
        dve(lambda: V.tensor_scalar(out=mtok, in0=posm_v, scalar1=0.0, scalar2=None, op0=ALU.is_ge), ['posm'], ['mtok'])
        dve(lambda: V.tensor_tensor(out=gm, in0=aff_v, in1=mtok, op=ALU.mult), ['mtok'], ['gm'])
        dve(lambda: V.tensor_copy(out=gmb_v[:, :, 0], in_=gm), ['gm'], ['gmb0'])
        dve(lambda: V.tensor_tensor(out=gml, in0=gm, in1=gmb_v[:, :, 0], op=ALU.subtract), ['gm', 'gmb0'], ['gml'])
        dve(lambda: V.tensor_copy(out=gmb_v[:, :, 1], in_=gml), ['gml'], ['gmb1'])
        if "posT" in dbg_t:
            dump("posT", posTf, ['posTf'])
        S.barrier()
        AR.release(mDN0)
        if stop_after <= 8:
            S.finish()
            return nc

        yall = AR.alloc([128, NE, 2, 1024], BF16)
        mG = AR.mark()
        wgb = AR.alloc([128, 8, 1024], BF16)
        wub = AR.alloc([128, 8, 1024], BF16)
        wdb = AR.alloc([128, 8, 1024], BF16)
        Sel = AR.alloc([128, 16, 256], BF16)
        xgT = g1row.bitcast(BF16).rearrange("p (a b) -> p a b", b=256)
        hidT = s2row.bitcast(BF16).rearrange("p (a b) -> p a b", b=256)
        sgv = b2row.rearrange("p (a b) -> p a b", b=256)
        g4 = sgv[:, 2, 0:4]
        gs = sgv[:, 2, 8:10]
        def load_expert(e):
            for (wd, dst, key) in ((dr["w_gate"][e], wgb, 'wgb'), (dr["w_up"][e], wub, 'wub'), (dr["w_down"][e], wdb, 'wdb')):
                for kc in range(8):
                    S.dma('pool', dst[:, kc, :], wd[kc * 128:(kc + 1) * 128, :], writes=[(key, kc)])

        NEXP = int(os.environ.get('NEXP', NE))
        load_expert(0)
        for e in range(NEXP):
            for tl in range(16):
                dve(lambda tl=tl, e=e: V.tensor_scalar(out=Sel[:, tl, :], in0=iota256, scalar1=posm[:, tl, e:e + 1], scalar2=None, op0=ALU.is_equal),
                    [], [('Sel', tl)])
            for kc in range(8):
                bi = kc % 2
                for tl in range(16):
                    mm(pb[bi][:, 0:256], h2[:, tl, kc * 128:(kc + 1) * 128], Sel[:, tl, :], tl == 0, tl == 15, [('Sel', tl)], [('pbg', bi)])
                act(lambda kc=kc, bi=bi: A.copy(out=xgT[:, kc, :], in_=pb[bi][:, 0:256]), [('pbg', bi)], [('xgT', kc)])
            for cc in range(2):
                for tl in range(16):
                    mm(pb[0][:, 256 + cc * 2:258 + cc * 2], Sel[:, tl, cc * 128:(cc + 1) * 128], gmb[:, tl, 2 * e:2 * e + 2], tl == 0, tl == 15,
                       [('Sel', tl), 'gmb0', 'gmb1'], [('pbg', 0)])
            act(lambda: A.copy(out=g4, in_=pb[0][:, 256:260]), [('pbg', 0)], ['g4'])
            dve(lambda: V.tensor_tensor(out=gs, in0=g4.rearrange("p (c t) -> p c t", t=2)[:, :, 0], in1=g4.rearrange("p (c t) -> p c t", t=2)[:, :, 1], op=ALU.add),
                ['g4'], ['gs'])
            for ffc in range(8):
                pg, pu = (pb[2], pb[3]) if ffc % 2 == 0 else (pb[4], pb[5])
                kg, ku = ('pbf', 2 + (ffc % 2) * 2), ('pbf', 3 + (ffc % 2) * 2)
                for kc in range(8):
                    mm(pg[:, 0:256], wgb[:, kc, ffc * 128:(ffc + 1) * 128], xgT[:, kc, :], kc == 0, kc == 7, [('wgb', kc), ('xgT', kc)], [kg])
                for kc in range(8):
                    mm(pu[:, 0:256], wub[:, kc, ffc * 128:(ffc + 1) * 128], xgT[:, kc, :], kc == 0, kc == 7, [('wub', kc), ('xgT', kc)], [ku])
                si = ffc % 2
                act(lambda pg=pg, si=si: A.activation(out=sgv[:, si, :], in_=pg[:, 0:256], func=AF.Silu), [kg], [('sg', si)])
                dve(lambda pu=pu, si=si, ffc=ffc: V.tensor_tensor(out=hidT[:, ffc, :], in0=sgv[:, si, :], in1=pu[:, 0:256], op=ALU.mult),
                    [('sg', si), ku], [('hidT', ffc)])
            for cc in range(2):
                for half in range(2):
                    bi = (cc * 2 + half) % 2
                    for ffc in range(8):
                        mm(pb[bi], hidT[:, ffc, cc * 128:(cc + 1) * 128], wdb[:, ffc, half * 512:(half + 1) * 512], ffc == 0, ffc == 7,
                           [('hidT', ffc), ('wdb', ffc)], [('pbg', bi)])
                    act(lambda e=e, cc=cc, half=half, bi=bi: A.activation(out=yall[:, e, cc, half * 512:(half + 1) * 512], in_=pb[bi], func=AF.Identity, scale=gs[:, cc:cc + 1]),
                        [('pbg', bi), 'gs'], [('yall', e)])
            if e + 1 < NEXP:
                load_expert(e + 1)
        if "yall" in dbg_t:
            dy = AR.alloc([128, 2, 1024], F32)
            dve(lambda: V.tensor_copy(out=dy, in_=yall[:, 0, :, :]), [('yall', 0)], ['dy'])
            dump("yall", dy, ['dy'])
        S.barrier()
        AR.release(mG)
        if stop_after <= 9:
            S.finish()
            return nc

        SelT = [AR.alloc([128, 2, 256], BF16) for _ in range(2)]
        xo = [AR.alloc([128, 1024], F32) for _ in range(2)]
        ot = [AR.alloc([128, 1024], F32) for _ in range(2)]
        oc = 0
        for blk in range(8):
            for e in range(NE):
                si = e % 2
                pe1(pb[4 + si][:, 0:256], oh16[:, e, :], posTb[:, blk * 256:(blk + 1) * 256], [], [('pbp', si)])
                for cc in range(2):
                    dve(lambda si=si, cc=cc: V.tensor_scalar(out=SelT[si][:, cc, :], in0=pb[4 + si][:, 0:256], scalar1=iotap[:, cc:cc + 1], scalar2=None,
                                                          op0=ALU.is_equal), [('pbp', si)], [('SelT', si, cc)])
                for cc in range(2):
                    for tt in range(2):
                        for half in range(2):
                            first = (e == 0 and cc == 0)
                            last = (e == NE - 1 and cc == 1)
                            mm(pb[tt * 2 + half], SelT[si][:, cc, tt * 128:(tt + 1) * 128], yall[:, e, cc, half * 512:(half + 1) * 512], first, last,
                               [('SelT', si, cc)], [('pbo', tt * 2 + half)])
            for tt in range(2):
                tl = blk * 2 + tt
                i = oc % 2
                oc += 1
                S.dma('sp', xo[i], x1s[tl * 128:(tl + 1) * 128, :], reads=[('x1s', tl)], writes=[('xo', i)])
                for half in range(2):
                    hs = slice(half * 512, (half + 1) * 512)
                    dve(lambda i=i, tt=tt, half=half, hs=hs: V.tensor_tensor(out=ot[i][:, hs], in0=pb[tt * 2 + half], in1=g2row[:, hs], op=ALU.mult),
                        [('pbo', tt * 2 + half)], [('ot', i, half)])
                    dve(lambda i=i, hs=hs: V.tensor_tensor(out=ot[i][:, hs], in0=ot[i][:, hs], in1=xo[i][:, hs], op=ALU.add),
                        [('ot', i, half), ('xo', i)], [('ot', i, half)])
                S.dma('act', out[tl * 128:(tl + 1) * 128, :], ot[i], reads=[('ot', i, 0), ('ot', i, 1)], writes=[('out', tl)])
        S.finish()
    return nc


_CACHE = {}


def _layout_inputs(inp, b):
    f = np.float32
    m = {}
    m["x"] = np.ascontiguousarray(inp["x"][b], f)
    m["ctx"] = np.ascontiguousarray(inp["ctx"][b], f)
    cT = np.zeros((128, 8, 2), f)
    cT[:, :, 0] = inp["c"][b].reshape(8, 128).T
    cT[:, :, 1] = inp["c_ctx"].reshape(8, 128).T
    m["cT"] = cT.reshape(128, 16)
    m["w_mod"] = np.ascontiguousarray(inp["w_mod"][0], f)
    m["bmrow"] = np.ascontiguousarray(np.tile(inp["b_mod"][0][None, :], (2, 1)), f)
    m["n2row"] = np.ascontiguousarray(np.tile(inp["norm2_w"][0][None, :], (2, 1)), f)
    n1 = inp["norm1_w"][0].reshape(8, 128).T
    n2 = inp["norm2_w"][0].reshape(8, 128).T
    m["n12T"] = np.ascontiguousarray(np.concatenate([np.repeat(n1[:, :, None], 2, axis=2).reshape(128, 16),
                                                     np.repeat(n2[:, :, None], 2, axis=2).reshape(128, 16)], axis=1), f)
    m["w_in"] = np.ascontiguousarray(inp["w_in"][0], f)
    m["qkw"] = np.ascontiguousarray(np.stack([np.tile(inp["q_norm_w"][0], 2), np.tile(inp["k_norm_w"][0], 2)], axis=1), f)
    cw = inp["conv_w"][0]
    m["convT"] = np.ascontiguousarray(cw.reshape(5, 12, 128).transpose(2, 1, 0).reshape(128, 60), f)
    m["alog"] = np.ascontiguousarray(np.tile(inp["a_log"][0].reshape(1, 8), (128, NT)), f)
    m["dtb"] = np.ascontiguousarray(np.tile(inp["dt_bias"][0].reshape(1, 8), (128, NT)), f)
    m["onw"] = np.ascontiguousarray(np.tile(inp["o_norm_w"][0].reshape(1, 128), (128, 1)), f)
    m["w_out"] = np.ascontiguousarray(inp["w_out"][0], f)
    m["router_w"] = np.ascontiguousarray(inp["router_w"][0], f)
    m["w_gate"] = np.ascontiguousarray(inp["w_gate"][0], f)
    m["w_up"] = np.ascontiguousarray(inp["w_up"][0], f)
    m["w_down"] = np.ascontiguousarray(inp["w_down"][0], f)
    return m


def kernel(**inputs):
    inp = {k: np.asarray(v) for k, v in inputs.items()}
    if "nc" not in _CACHE:
        _CACHE["nc"] = build_program()
    nc = _CACHE["nc"]
    consts = host_consts()
    in_maps = []
    shared = None
    for b in range(8):
        m = _layout_inputs(inp, b)
        if shared is None:
            shared = {k: m[k] for k in m if k not in ("x", "ctx", "cT")}
        else:
            for k in shared:
                m[k] = shared[k]
        m.update(consts)
        in_maps.append(m)
    res = run_bass_kernel_spmd(nc, in_maps, core_ids=list(range(8)))
    return np.stack([np.asarray(r["out"], np.float32) for r in res.results], axis=0)
```

```python
import os
from contextlib import ExitStack
import numpy as np
import concourse.bass as bass
import concourse.mybir as mybir
from concourse.bass_utils import run_bass_kernel_spmd

F32 = mybir.dt.float32
BF16 = mybir.dt.bfloat16
U8 = mybir.dt.uint8
AF = mybir.ActivationFunctionType
ALU = mybir.AluOpType
AX = mybir.AxisListType

NDMA = 24
D = 1024
SEQ = 2048
CTX = 256
T = SEQ + CTX
NT = T // 128
INW = 2832
EPS = 1e-6
NE = 16
CAP = 256


def _is_psum_key(k):
    n = k if isinstance(k, str) else k[0]
    return isinstance(n, str) and (n.startswith('pb') or n in ('pT', 'pm', 'acc', 'rowbank'))


class Sched:
    def __init__(self, nc, stack):
        self.nc = nc
        self.eng = {'pe': nc.tensor, 'act': nc.scalar, 'dve': nc.vector, 'pool': nc.gpsimd, 'sp': nc.sync}
        self.prog = {e: [] for e in self.eng}
        self.sem = {}
        self.cnt = {}
        for e in self.eng:
            self.sem[e] = stack.enter_context(nc.semaphore('s_' + e))
            self.cnt[e] = 0
        self.dsem = [stack.enter_context(nc.semaphore('d%d' % i)) for i in range(NDMA)]
        self.dcnt = [0] * NDMA
        self.dnext = 0
        self.seen = {e: {} for e in self.eng}
        self.lastw = {}
        self.readers = {}

    def _wait(self, e, tok):
        k, v = tok
        if self.seen[e].get(k, 0) >= v:
            return
        self.seen[e][k] = v
        self.prog[e].append(('wait', k, v))

    def _deps(self, e, reads, writes, pe_accum=False):
        best = {}

        def add(t):
            if best.get(t[0], 0) < t[1]:
                best[t[0]] = t[1]
        for r in reads:
            t = self.lastw.get(r)
            if t is not None:
                add(t)
            if _is_psum_key(r):
                for t in self.readers.get(r, ()):
                    if t[0] != e:
                        add(t)
        for w in writes:
            t = self.lastw.get(w)
            if t is not None and not (pe_accum and t[0] == 'pe'):
                add(t)
            for t in self.readers.get(w, ()):
                add(t)
        for k, v in best.items():
            self._wait(e, (k, v))

    def _commit(self, tok, reads, writes):
        for r in reads:
            lst = self.readers.setdefault(r, [])
            lst[:] = [t for t in lst if t[0] != tok[0]]
            lst.append(tok)
        for w in writes:
            self.lastw[w] = tok
            self.readers[w] = []

    def op(self, e, fn, reads=(), writes=(), pe_accum=False):
        self._deps(e, reads, writes, pe_accum)
        self.cnt[e] += 1
        self.prog[e].append(('op', fn, self.cnt[e]))
        tok = (e, self.cnt[e])
        self._commit(tok, reads, writes)
        return tok

    def dma(self, q, out, in_, reads=(), writes=(), **kw):
        self._deps(q, reads, writes)
        i = self.dnext
        self.dnext = (self.dnext + 1) % NDMA
        if self.dcnt[i] > 0:
            self._wait(q, (('d', i), self.dcnt[i]))
        self.dcnt[i] += 16
        so = self.dsem[i]
        eo = self.eng[q]
        self.prog[q].append(('dma', lambda: eo.dma_start(out=out, in_=in_, **kw).then_inc(so, 16)))
        tok = (('d', i), self.dcnt[i])
        self._commit(tok, reads, writes)
        return tok

    def barrier(self):
        for e in self.eng:
            for i in range(NDMA):
                if self.dcnt[i] > 0:
                    self._wait(e, (('d', i), self.dcnt[i]))
            for e2 in ('pe', 'act', 'dve', 'pool'):
                if self.cnt[e2] > 0:
                    self._wait(e, (e2, self.cnt[e2]))
        self.lastw = {}
        self.readers = {}

    def finish(self):
        nc = self.nc
        for i in range(NDMA):
            if self.dcnt[i] > 0:
                self._wait('sp', (('d', i), self.dcnt[i]))
        for e in ('pe', 'act', 'dve', 'pool'):
            if self.cnt[e] > 0:
                self._wait('sp', (e, self.cnt[e]))
        needed = {e: set() for e in self.eng}
        for e in self.eng:
            for it in self.prog[e]:
                if it[0] == 'wait' and isinstance(it[1], str):
                    needed[it[1]].add(it[2])
        remap = {e: {v: i + 1 for i, v in enumerate(sorted(needed[e]))} for e in self.eng}
        self.maxsem = {e: len(needed[e]) for e in self.eng}

        def emit(e):
            eo = self.eng[e]
            for it in self.prog[e]:
                if it[0] == 'wait':
                    k, v = it[1], it[2]
                    if isinstance(k, str):
                        eo.wait_ge(self.sem[k], remap[k][v])
                    else:
                        eo.wait_ge(self.dsem[k[1]], v)
                elif it[0] == 'op':
                    ins = it[1]()
                    if it[2] in remap[e]:
                        ins.then_inc(self.sem[e], 1)
                else:
                    it[1]()

        with nc.Block() as block:
            @block.sync
            def _(eng):
                emit('sp')

            @block.tensor
            def _(eng):
                emit('pe')

            @block.scalar
            def _(eng):
                emit('act')

            @block.vector
            def _(eng):
                emit('dve')

            @block.gpsimd
            def _(eng):
                emit('pool')


class Arena:
    def __init__(self, ar, nbytes):
        self.ar = ar
        self.n = nbytes
        self.off = 0

    def alloc(self, shape, dtype):
        es = 2 if dtype == BF16 else 4
        per = int(np.prod(shape[1:])) * es
        off = (self.off + 63) // 64 * 64
        assert off + per <= getattr(self, 'top', self.n), ("arena overflow", off, per, getattr(self, 'top', self.n))
        self.off = off + per
        v = self.ar[:, off:off + per].bitcast(dtype)
        if len(shape) == 3:
            v = v.rearrange("p (a b) -> p a b", b=shape[2])
        elif len(shape) == 4:
            v = v.rearrange("p (a b c) -> p a b c", b=shape[2], c=shape[3])
        if shape[0] < 128:
            v = v[0:shape[0]]
        return v

    def alloc_top(self, shape, dtype):
        es = 2 if dtype == BF16 else 4
        per = int(np.prod(shape[1:])) * es
        top = getattr(self, 'top', self.n)
        off = (top - per) // 64 * 64
        assert off >= self.off, ("arena top overflow", off, self.off)
        self.top = off
        v = self.ar[:, off:off + per].bitcast(dtype)
        if len(shape) == 3:
            v = v.rearrange("p (a b) -> p a b", b=shape[2])
        if shape[0] < 128:
            v = v[0:shape[0]]
        return v

    def release_top(self):
        self.top = self.n

    def mark(self):
        return self.off

    def release(self, m):
        self.off = m


def host_consts():
    c = {}
    c["ident"] = np.eye(128, dtype=np.float32)
    p = np.arange(128)
    t = np.arange(SEQ)
    d = p % 64
    is_row = d < 32
    fi = np.where(is_row, d % 16, (d - 32) % 16).astype(np.float32)
    freqs = (np.float32(10000.0) ** (-fi / np.float32(16.0))).astype(np.float32)
    pos = np.where(is_row[:, None], (t // 64)[None, :], (t % 64)[None, :]).astype(np.float32)
    ang = (pos * freqs[:, None]).astype(np.float32)
    c["cosT"] = np.cos(ang).astype(np.float32)
    c["sinT"] = np.sin(ang).astype(np.float32)
    rm = np.zeros((128, 128), np.float32)
    for q in range(128):
        if (q % 32) < 16:
            rm[q + 16, q] = -1.0
        else:
            rm[q - 16, q] = 1.0
    c["rotm"] = rm
    c["blk64"] = (p[:, None] // 64 == p[None, :] // 64).astype(np.float32)
    same = c["blk64"]
    r, cc = p[:, None], p[None, :]
    uinc = (r <= cc) * same
    lstr = (r > cc) * same
    linc = (r >= cc) * same
    ustr = (r < cc) * same
    msx = np.zeros((2, 128, 130), np.float32)
    msx[0, :, :128] = lstr
    msx[1, :, :128] = ustr
    msx[:, :, 128] = 1.0
    c["dnm"] = np.stack([uinc, lstr, linc, ustr]).astype(np.float32)
    c["msx"] = msx
    sel = np.zeros((2, 128, 128), np.float32)
    sel[0, 0, :] = 1.0
    sel[1, 64, :] = 1.0
    c["selrow"] = sel
    c["iota256"] = np.tile(np.arange(256, dtype=np.float32)[None, :], (128, 1))
    c["iotap"] = np.stack([p, p + 128], axis=1).astype(np.float32)
    oh = np.zeros((16, 16, 128), np.float32)
    for e in range(16):
        oh[e, e, :] = 1.0
    c["oh16"] = oh
    return c


CONST_SHAPES = {"ident": [128, 128], "cosT": [128, SEQ], "sinT": [128, SEQ], "rotm": [128, 128], "blk64": [128, 128],
                "dnm": [4, 128, 128], "msx": [2, 128, 130], "selrow": [2, 128, 128], "iota256": [128, 256],
                "iotap": [128, 2], "oh16": [16, 16, 128]}

IN_SHAPES = {"x": [SEQ, D], "ctx": [CTX, D], "cT": [128, 16], "w_mod": [D, 6 * D], "bmrow": [2, 6 * D], "n2row": [2, D], "n12T": [128, 32],
             "w_in": [D, INW], "qkw": [128, 2], "convT": [128, 60], "alog": [128, NT * 8], "dtb": [128, NT * 8],
             "onw": [128, 128], "w_out": [D, D], "router_w": [D, NE], "w_gate": [NE, D, D], "w_up": [NE, D, D],
             "w_down": [NE, D, D]}


def build_program(stop_after=99, dbg=None):
    nc = bass.Bass("TRN2", target_bir_lowering=False)
    dr = {}
    for k, s in list(IN_SHAPES.items()) + list(CONST_SHAPES.items()):
        dr[k] = nc.dram_tensor(k, s, F32, kind="ExternalInput").ap()
    out = nc.dram_tensor("out", [SEQ, D], F32, kind="ExternalOutput").ap()
    x1s = nc.dram_tensor("x1s", [SEQ, D], F32).ap()
    dbg_t = {}
    if dbg:
        for k, s in dbg.items():
            dbg_t[k] = nc.dram_tensor("dbg_" + k, s, F32, kind="ExternalOutput").ap()

    with ExitStack() as st:
        S = Sched(nc, st)
        V, A, P, TE = nc.vector, nc.scalar, nc.gpsimd, nc.tensor
        ARN = 206 * 1024
        ar = st.enter_context(nc.sbuf_tensor("arena", [128, ARN], U8))
        AR = Arena(ar, ARN)
        pb = [st.enter_context(nc.psum_tensor("pb%d" % i, [128, 512], F32))[:] for i in range(8)]
        pbt = [pb[6 + i].bitcast(BF16).rearrange("p (a b) -> p a b", b=128) for i in range(2)]

        def dve(fn, r, w):
            return S.op('dve', fn, r, w)

        def act(fn, r, w):
            return S.op('act', fn, r, w)

        def pool(fn, r, w):
            return S.op('pool', fn, r, w)

        def mm(o, l, r_, start, stop, rd, wr):
            return S.op('pe', lambda: TE.matmul(o, lhsT=l, rhs=r_, start=start, stop=stop), rd, wr, pe_accum=not start)

        def tr(o, i, idn, rd, wr):
            return S.op('pe', lambda: TE.transpose(out=o, in_=i, identity=idn), rd, wr, pe_accum=True)

        def dump(name, src_ap, rd):
            if name in dbg_t:
                S.dma('sp', dbg_t[name], src_ap, reads=rd, writes=['dbg_' + name])

        ident = AR.alloc([128, 128], F32)
        identb = AR.alloc([128, 128], BF16)
        onesf = AR.alloc([128, 128], F32)
        onesb = AR.alloc([128, 128], BF16)
        rotb = AR.alloc([128, 128], BF16)
        blk64b = AR.alloc([128, 128], BF16)
        blk64f = AR.alloc([128, 128], F32)
        dnm = AR.alloc([128, 4, 128], F32)
        msx = AR.alloc([128, 2, 130], F32)
        selrow = AR.alloc([128, 2, 128], F32)
        iota256 = AR.alloc([128, 256], F32)
        iotap = AR.alloc([128, 2], F32)
        oh16 = AR.alloc([16, 16, 128], BF16)
        mT = AR.mark()
        tmpc = AR.alloc([128, 2048], F32)
        S.dma('sp', ident, dr["ident"], writes=['ident'])
        S.dma('sp', blk64f, dr["blk64"], writes=['blk64f'])
        S.dma('sp', dnm, dr["dnm"].rearrange("a p n -> p a n"), writes=['dnm'])
        S.dma('sp', msx, dr["msx"].rearrange("a p n -> p a n"), writes=['msx'])
        S.dma('sp', selrow, dr["selrow"].rearrange("a p n -> p a n"), writes=['selrow'])
        S.dma('sp', iota256, dr["iota256"], writes=['iota256'])
        S.dma('sp', iotap, dr["iotap"], writes=['iotap'])
        S.dma('sp', tmpc[:, 0:128], dr["rotm"], writes=['tmpc'])
        dve(lambda: V.tensor_copy(out=rotb, in_=tmpc[:, 0:128]), ['tmpc'], ['rotb'])
        dve(lambda: V.tensor_copy(out=identb, in_=ident), ['ident'], ['identb'])
        dve(lambda: V.tensor_copy(out=blk64b, in_=blk64f), ['blk64f'], ['blk64b'])
        pool(lambda: P.memset(onesf, 1.0), [], ['onesf'])
        pool(lambda: P.memset(onesb, 1.0), [], ['onesb'])
        S.dma('sp', tmpc[0:16, :], dr["oh16"].rearrange("k e m -> k (e m)"), reads=['rotb'], writes=['tmpc'])
        dve(lambda: V.tensor_copy(out=oh16, in_=tmpc[0:16, :].rearrange("k (e m) -> k e m", m=128)), ['tmpc'], ['oh16'])

        S.barrier()
        AR.release(mT)
        modv = AR.alloc([128, 96], F32)
        s1 = AR.alloc([128, 16], F32)
        s2 = AR.alloc([128, 16], F32)
        n12 = AR.alloc([128, 32], F32)
        g1row = AR.alloc([128, 1024], F32)
        g2row = AR.alloc([128, 1024], F32)
        s2row = AR.alloc([128, 1024], F32)
        b2row = AR.alloc([128, 1024], F32)
        mA = AR.mark()
        if os.environ.get('ARDBG'): print('A persistent end', AR.off)
        cT = AR.alloc([128, 16], F32)
        sil = AR.alloc([128, 16], F32)
        wm = [AR.alloc([128, 8, 1024], F32) for _ in range(2)]
        bmr = AR.alloc([2, 6144], F32)
        modrow = AR.alloc([2, 6144], F32)
        n2r = AR.alloc([2, 1024], F32)
        s2r = AR.alloc([2, 1024], F32)
        sel01 = AR.alloc([2, 128], F32)
        S.dma('sp', cT, dr["cT"], writes=['cT'])
        S.dma('sp', n12, dr["n12T"], writes=['n12'])
        S.dma('sp', bmr, dr["bmrow"], writes=['bmr'])
        S.dma('sp', n2r, dr["n2row"], writes=['n2r'])
        pool(lambda: P.memset(sel01, 0.0), [], ['sel01'])
        pool(lambda: P.memset(sel01[0:1, :], 1.0), ['sel01'], ['sel01'])
        act(lambda: A.activation(out=sil, in_=cT, func=AF.Silu), ['cT'], ['sil'])
        wmv = dr["w_mod"].rearrange("(kc p) n -> p kc n", p=128)
        for j in range(6):
            buf = wm[j % 2]
            for hh in range(2):
                S.dma('sp' if hh == 0 else 'act', buf[:, 4 * hh:4 * hh + 4, :], wmv[:, 4 * hh:4 * hh + 4, j * 1024:(j + 1) * 1024],
                      writes=[('wm', j % 2, hh)])
            for half in range(2):
                bi = (2 * j + half) % 4
                bank = pb[bi]
                for kc in range(8):
                    mm(bank[0:2, :], sil[:, 2 * kc:2 * kc + 2], buf[:, kc, half * 512:(half + 1) * 512], kc == 0, kc == 7,
                       [('wm', j % 2, kc // 4), 'sil'], [('pbm', bi)])
                c0 = j * 1024 + half * 512
                dve(lambda bank=bank, c0=c0: V.tensor_tensor(out=modrow[:, c0:c0 + 512], in0=bank[0:2, :], in1=bmr[:, c0:c0 + 512], op=ALU.add),
                    [('pbm', bi), 'bmr'], [('modrow', j, half)])
        pm = pb[4]
        for idx in range(16):
            tr(pm[:, idx * 2:idx * 2 + 2], modrow[0:2, idx * 128:(idx + 1) * 128], ident[0:2, 0:2],
               [('modrow', idx // 8, (idx % 8) // 4), 'ident'], ['pm'])
        dve(lambda: V.tensor_copy(out=modv[:, 0:32], in_=pm[:, 0:32]), ['pm'], ['modv'])
        dve(lambda: V.scalar_tensor_tensor(out=s1, in0=modv[:, 16:32], scalar=1.0, in1=n12[:, 0:16], op0=ALU.add, op1=ALU.mult),
            ['modv', 'n12'], ['s1'])
        dve(lambda: V.scalar_tensor_tensor(out=s2r, in0=modrow[:, 4096:5120], scalar=1.0, in1=n2r, op0=ALU.add, op1=ALU.mult),
            [('modrow', 4, 0), ('modrow', 4, 1), 'n2r'], ['s2r'])
        rows = [(g1row, modrow, 2048, [('modrow', 2, 0), ('modrow', 2, 1)], 'g1row'), (g2row, modrow, 5120, [('modrow', 5, 0), ('modrow', 5, 1)], 'g2row'),
                (s2row, s2r, 0, ['s2r'], 's2row'), (b2row, modrow, 3072, [('modrow', 3, 0), ('modrow', 3, 1)], 'b2row')]
        ri = 0
        for (dst, src, base, skeys, key) in rows:
            for half in range(2):
                bank = pb[5 + ri % 2]
                mm(bank, sel01[0:2, :], src[0:2, base + half * 512:base + (half + 1) * 512], True, True, skeys + ['sel01'], [('rowbank', ri % 2)])
                act(lambda dst=dst, bank=bank, half=half: A.copy(out=dst[:, half * 512:(half + 1) * 512], in_=bank),
                    [('rowbank', ri % 2)], [key])
                ri += 1
        dump("modv", modv, ['modv'])
        dump("g1row", g1row, ['g1row'])
        S.barrier()
        AR.release(mA)
        mDN0 = AR.mark()
        if stop_after <= 0:
            S.finish()
            return nc

        hT = AR.alloc_top([128, 8, T], BF16)
        mB = AR.mark()
        xin = [AR.alloc([128, 1024], F32) for _ in range(3)]
        junk = AR.alloc([128, 1024], F32)
        xn = [AR.alloc([128, 1024], BF16) for _ in range(2)]
        ssq = AR.alloc([128, NT], F32)
        rstd = AR.alloc([128, NT], F32)

        def b_stats(ti):
            src = dr["ctx"][ti * 128:(ti + 1) * 128, :] if ti < 2 else dr["x"][(ti - 2) * 128:(ti - 1) * 128, :]
            xb, xnb = xin[ti % 3], xn[ti % 2]
            S.dma('sp', xb, src, writes=[('xin', ti % 3)])
            act(lambda xb=xb, ti=ti: A.activation(out=junk, in_=xb, func=AF.Square, accum_out=ssq[:, ti:ti + 1]),
                [('xin', ti % 3)], ['junk', ('ssq', ti)])
            dve(lambda ti=ti: V.tensor_scalar(out=rstd[:, ti:ti + 1], in0=ssq[:, ti:ti + 1], scalar1=1.0 / D, scalar2=EPS,
                                              op0=ALU.mult, op1=ALU.add), [('ssq', ti)], [('rstd', ti)])
            act(lambda ti=ti: A.activation(out=rstd[:, ti:ti + 1], in_=rstd[:, ti:ti + 1], func=AF.Sqrt), [('rstd', ti)], [('rstd', ti)])
            dve(lambda ti=ti: V.reciprocal(out=rstd[:, ti:ti + 1], in_=rstd[:, ti:ti + 1]), [('rstd', ti)], [('rstd', ti)])
            dve(lambda xb=xb, xnb=xnb, ti=ti: V.tensor_scalar(out=xnb, in0=xb, scalar1=rstd[:, ti:ti + 1], scalar2=None, op0=ALU.mult),
                [('xin', ti % 3), ('rstd', ti)], [('xn', ti % 2)])

        def b_transpose(ti):
            t = 1 if ti < 2 else 0
            xnb = xn[ti % 2]
            bv = pbt[ti % 2]
            for kc in range(8):
                tr(bv[:, kc, :], xnb[:, kc * 128:(kc + 1) * 128], identb, [('xn', ti % 2), 'identb'], [('pT', ti % 2)])
            for kc in range(8):
                o_ = hT[:, kc, ti * 128:(ti + 1) * 128]
                sc_ = s1[:, kc * 2 + t:kc * 2 + t + 1]
                bi_ = modv[:, kc * 2 + t:kc * 2 + t + 1]
                act(lambda o_=o_, kc=kc, bv=bv, sc_=sc_, bi_=bi_: A.activation(out=o_, in_=bv[:, kc, :], func=AF.Identity, scale=sc_, bias=bi_),
                    [('pT', ti % 2), 's1', 'modv'], [('hT', ti, kc)])

        b_stats(0)
        for ti in range(NT):
            if ti + 1 < NT:
                b_stats(ti + 1)
            b_transpose(ti)
        if "hT" in dbg_t:
            hTf = AR.alloc([128, 8, 256], F32)
            dve(lambda: V.tensor_copy(out=hTf, in_=hT[:, :, 128:384]), [('hT', a, b) for a in (1, 2) for b in range(8)], ['hTf'])
            dump("hT", hTf, ['hTf'])
        S.barrier()
        AR.release(mB)
        if stop_after <= 1:
            S.finish()
            return nc

        attT = AR.alloc([128, 4, SEQ], BF16)
        mQ = AR.mark()
        if os.environ.get('ARDBG'): print('attT end', AR.off)
        qT = AR.alloc([128, 4, SEQ], BF16)
        kTd = AR.alloc([128, 2, T], BF16)
        vext = AR.alloc([128, NT, 2, 66], BF16)
        mC1 = AR.mark()
        cosT = AR.alloc([128, SEQ], F32)
        sinT = AR.alloc([128, SEQ], F32)
        qkw = AR.alloc([128, 2], F32)
        stg = [AR.alloc([128, 8, 128], F32) for _ in range(2)]
        wpc = [AR.alloc([128, 8, 128], BF16) for _ in range(2)]
        sqb = [AR.alloc([128, 512], BF16) for _ in range(2)]
        rq = [AR.alloc([128, 512], F32) for _ in range(2)]
        qnb = [AR.alloc([128, 512], BF16) for _ in range(2)]
        t1 = [AR.alloc([128, 512], F32) for _ in range(2)]
        t2 = [AR.alloc([128, 512], F32) for _ in range(2)]
        S.dma('sp', cosT, dr["cosT"], writes=['cosT'])
        S.dma('sp', sinT, dr["sinT"], writes=['sinT'])
        S.dma('sp', qkw, dr["qkw"], writes=['qkw'])
        pool(lambda: P.memset(vext, 0.0), [], ['vext'])
        pool(lambda: P.memset(vext[:, :, :, 64:65], 1.0), ['vext'], ['vext1'])
        winv = dr["w_in"].rearrange("(kc p) n -> p kc n", p=128)
        wctr = [0]

        def load_w(col0, ncols=128, dup=False):
            ci = wctr[0] % 2
            wctr[0] += 1
            wp = wpc[ci]
            if dup:
                S.dma('pool', wp[:, :, 0:64], winv[:, :, col0:col0 + 64], writes=[('wpc', ci)])
                S.dma('pool', wp[:, :, 64:128], winv[:, :, col0:col0 + 64], writes=[('wpc', ci)])
            else:
                S.dma('pool', wp[:, :, 0:ncols], winv[:, :, col0:col0 + ncols], writes=[('wpc', ci)])
            return wp, ('wpc', ci)

        ectr = [0]

        def qk_epilogue(ps, pskey, n, wcol, rope_t0, dst, dstkey):
            i = ectr[0] % 2
            ectr[0] += 1
            act(lambda: A.activation(out=sqb[i][:, 0:n], in_=ps[:, 0:n], func=AF.Square), [pskey], [('sqb', i)])
            mm(pb[2][:, 0:n], blk64b, sqb[i][:, 0:n], True, True, [('sqb', i), 'blk64b'], ['pb2'])
            act(lambda: A.activation(out=rq[i][:, 0:n], in_=pb[2][:, 0:n], func=AF.Sqrt, scale=1.0 / 64, bias=EPS), ['pb2'], [('rq', i)])
            dve(lambda: V.reciprocal(out=rq[i][:, 0:n], in_=rq[i][:, 0:n]), [('rq', i)], [('rq', i)])
            if rope_t0 is None:
                dve(lambda: V.scalar_tensor_tensor(out=dst, in0=ps[:, 0:n], scalar=qkw[:, wcol:wcol + 1], in1=rq[i][:, 0:n],
                                                   op0=ALU.mult, op1=ALU.mult), [pskey, 'qkw', ('rq', i)], [dstkey])
                return
            dve(lambda: V.scalar_tensor_tensor(out=qnb[i][:, 0:n], in0=ps[:, 0:n], scalar=qkw[:, wcol:wcol + 1], in1=rq[i][:, 0:n],
                                               op0=ALU.mult, op1=ALU.mult), [pskey, 'qkw', ('rq', i)], [('qnb', i)])
            mm(pb[3][:, 0:n], rotb, qnb[i][:, 0:n], True, True, [('qnb', i), 'rotb'], ['pb3'])
            pool(lambda: P.tensor_tensor(out=t1[i][:, 0:n], in0=qnb[i][:, 0:n], in1=cosT[:, rope_t0:rope_t0 + n], op=ALU.mult),
                 [('qnb', i), 'cosT'], [('t1', i)])
            dve(lambda: V.tensor_tensor(out=t2[i][:, 0:n], in0=pb[3][:, 0:n], in1=sinT[:, rope_t0:rope_t0 + n], op=ALU.mult),
                ['pb3', 'sinT'], [('t2', i)])
            dve(lambda: V.tensor_tensor(out=dst, in0=t1[i][:, 0:n], in1=t2[i][:, 0:n], op=ALU.add), [('t1', i), ('t2', i)], [dstkey])

        NQB = int(os.environ.get('NQB', 4))
        pctr = [0]
        for j in range(4):
            wp, wk = load_w(128 * j)
            for b in range(NQB):
                bi = pctr[0] % 2
                pctr[0] += 1
                t0 = CTX + 512 * b
                for kc in range(8):
                    mm(pb[bi], wp[:, kc, :], hT[:, kc, t0:t0 + 512], kc == 0, kc == 7, [wk] + [('hT', ti_, kc) for ti_ in range(t0 // 128, t0 // 128 + 4)],
                       [('pbq', bi)])
                qk_epilogue(pb[bi], ('pbq', bi), 512, 0, 512 * b, qT[:, j, 512 * b:512 * (b + 1)], ('qT', j, b))
        for g in range(2):
            wp, wk = load_w(512 + 64 * g, 64, dup=True)
            for b in range(-1, NQB):
                bi = pctr[0] % 2
                pctr[0] += 1
                t0, n = (0, 256) if b < 0 else (CTX + 512 * b, 512)
                for kc in range(8):
                    mm(pb[bi][:, 0:n], wp[:, kc, :], hT[:, kc, t0:t0 + n], kc == 0, kc == 7,
                       [wk] + [('hT', ti_, kc) for ti_ in range(t0 // 128, (t0 + n) // 128)], [('pbq', bi)])
                qk_epilogue(pb[bi], ('pbq', bi), n, 1, None if b < 0 else 512 * b, kTd[:, g, t0:t0 + n], ('kTd', g, b))
        wp, wk = load_w(640)
        for ti in range(NT):
            bi = 4 + ti % 2
            for kc in range(8):
                mm(pb[bi][:, 0:128], hT[:, kc, ti * 128:(ti + 1) * 128], wp[:, kc, :], kc == 0, kc == 7, [wk, ('hT', ti, kc)], [('pbv', bi)])
            act(lambda ti=ti, bi=bi: A.copy(out=vext[:, ti, :, 0:64], in_=pb[bi][:, 0:128].rearrange("p (g d) -> p g d", d=64)),
                [('pbv', bi), 'vext'], [('vx', ti)])
        if "qT" in dbg_t:
            dq = AR.alloc([128, 4, 512], F32)
            dve(lambda: V.tensor_copy(out=dq, in_=qT[:, :, 0:512]), [('qT', j_, 0) for j_ in range(4)], ['dq'])
            dump("qT", dq, ['dq'])
        if "kT" in dbg_t:
            dk = AR.alloc([128, 2, 512], F32)
            dve(lambda: V.tensor_copy(out=dk, in_=kTd[:, :, 0:512]), [('kTd', g_, b_) for g_ in range(2) for b_ in (-1, 0)], ['dk'])
            dump("kT", dk, ['dk'])
        if "vext" in dbg_t:
            dv = AR.alloc([128, 2, 132], F32)
            dve(lambda: V.tensor_copy(out=dv, in_=vext[:, 1:3, :, :].rearrange("p a g d -> p a (g d)")), [('vx', 1), ('vx', 2), 'vext1'], ['dv'])
            dump("vext", dv, ['dv'])
        S.barrier()
        AR.release(mC1)
        if stop_after <= 2:
            S.finish()
            return nc

        mD = AR.mark()
        NSB = 3
        pbuf = [AR.alloc([128, 512], BF16) for _ in range(NSB)]
        rsb = AR.alloc([128, 512], F32)
        bcs = AR.alloc([64, 512], F32)
        NKT = int(os.environ.get('NKT', NT))
        pairs = [(j, qb) for j in range(int(os.environ.get('NJ', 4))) for qb in range(NQB)]
        its = [(p, kt, half) for p in range(len(pairs)) for kt in range(NKT) for half in range(2)]
        sbank = [pb[0], pb[1], pb[3]]

        def acc_of(p, half):
            return 4 + 2 * (p % 2) + half

        def emit_score(n):
            p, kt, half = its[n]
            j, qb = pairs[p]
            hp = slice(0, 64) if half == 0 else slice(64, 128)
            si = n % 3
            mm(sbank[si], kTd[hp, j // 2, kt * 128:(kt + 1) * 128], qT[hp, j, qb * 512:(qb + 1) * 512], True, True, [], [('pbs', si)])

        def emit_exp(n):
            si = n % 3
            act(lambda si=si: A.activation(out=pbuf[si], in_=sbank[si], func=AF.Exp, scale=0.125), [('pbs', si)], [('pbuf', si)])

        def emit_pv(n):
            p, kt, half = its[n]
            j, qb = pairs[p]
            si = n % 3
            ai = acc_of(p, half)
            mm(pb[ai][0:65, :], vext[:, kt, j // 2, 0:65], pbuf[si], kt == 0, kt == NKT - 1, [('pbuf', si)], [('acc', ai)])

        def emit_final(p, half):
            j, qb = pairs[p]
            hp = slice(0, 64) if half == 0 else slice(64, 128)
            ai = acc_of(p, half)
            acc = pb[ai]
            dve(lambda: V.reciprocal(out=rsb[64:65, :], in_=acc[64:65, :]), [('acc', ai)], ['rsb'])
            mm(pb[2][0:64, :], onesf[64:65, 0:64], rsb[64:65, :], True, True, ['rsb'], ['pb2'])
            act(lambda: A.copy(out=bcs, in_=pb[2][0:64, :]), ['pb2'], ['bcs'])
            dve(lambda: V.tensor_tensor(out=attT[hp, j, qb * 512:(qb + 1) * 512], in0=acc[0:64, :], in1=bcs, op=ALU.mult),
                [('acc', ai), 'bcs'], [('attT', j, half, qb)])

        NIT = len(its)
        for n in range(min(2, NIT)):
            emit_score(n)
        pending = []
        for m in range(NIT // 2):
            n0, n1 = 2 * m, 2 * m + 1
            if n0 + 2 < NIT:
                emit_score(n0 + 2)
            emit_exp(n0)
            if n1 + 2 < NIT:
                emit_score(n1 + 2)
            emit_pv(n0)
            emit_exp(n1)
            emit_pv(n1)
            p, kt, _ = its[n0]
            if pending and kt == min(3, NKT - 1):
                for (pp, hh) in pending:
                    emit_final(pp, hh)
                pending = []
            if kt == NKT - 1:
                for (pp, hh) in pending:
                    emit_final(pp, hh)
                pending = [(p, 0), (p, 1)]
        for (pp, hh) in pending:
            emit_final(pp, hh)
        if "attT" in dbg_t:
            da = AR.alloc([128, 4, 512], F32)
            dve(lambda: V.tensor_copy(out=da, in_=attT[:, :, 0:512]), [('attT', j_, h_, 0) for j_ in range(4) for h_ in range(2)], ['da'])
            dump("attT", da, ['da'])
        S.barrier()
        AR.release(mQ)
        if stop_after <= 3:
            S.finish()
            return nc

        zt = AR.alloc([128, 16, 512], BF16)
        dnoT = AR.alloc([128, 4, SEQ], BF16)
        mDN = AR.mark()
        dnT = AR.alloc([128, 12, T], BF16)
        gb = AR.alloc([128, NT, 16], F32)
        nbeta = AR.alloc([128, NT, 8], F32)
        mC2 = AR.mark()
        if os.environ.get('ARDBG'): print('C2 persistent end', AR.off, 'top', getattr(AR, 'top', None))
        stg = [AR.alloc([128, 8, 128], F32)] * 2
        wpc = [AR.alloc([128, 8, 128], BF16) for _ in range(2)]
        convw = AR.alloc([128, 60], F32)
        diagc = [AR.alloc([128, 5, 128], BF16) for _ in range(2)]
        rawp = [AR.alloc([128, 2312], BF16)] * 2
        cx = [AR.alloc([128, 512], F32) for _ in range(2)]
        sqd = [AR.alloc([128, 512], BF16)] * 2
        rq2 = [AR.alloc([128, 512], F32)] * 2
        gbraw = AR.alloc([128, NT, 16], F32)
        alog = AR.alloc([128, NT, 8], F32)
        dtb = AR.alloc([128, NT, 8], F32)
        S.dma('sp', convw, dr["convT"], writes=['convw'])
        S.dma('sp', alog, dr["alog"].rearrange("p (a b) -> p a b", b=8), writes=['alog'])
        S.dma('sp', dtb, dr["dtb"].rearrange("p (a b) -> p a b", b=8), writes=['dtb'])
        pool(lambda: P.memset(rawp[0], 0.0), [], [('rawp', 0)])
        cctr = [0]
        for m in range(int(os.environ.get('NDN', 12))):
            wp, wk = load_w(768 + 128 * m)
            rp, dgc = rawp[0], diagc[m % 2]
            for jt in range(5):
                dve(lambda jt=jt, m=m, dgc=dgc: V.tensor_scalar(out=dgc[:, jt, :], in0=ident, scalar1=convw[:, m * 5 + jt:m * 5 + jt + 1], scalar2=None,
                                                            op0=ALU.mult), ['ident', 'convw'], [('diagc', m % 2)])
            for b in range(-1, 4):
                t0, n = (0, 256) if b < 0 else (CTX + 512 * b, 512)
                off = 2 if b < 0 else 262 + 512 * b
                bi = pctr[0] % 2
                pctr[0] += 1
                for kc in range(8):
                    mm(pb[bi][:, 0:n], wp[:, kc, :], hT[:, kc, t0:t0 + n], kc == 0, kc == 7, [wk], [('pbq', bi)])
                act(lambda rp=rp, off=off, n=n, bi=bi: A.copy(out=rp[:, off:off + n], in_=pb[bi][:, 0:n]), [('pbq', bi)], [('rawp', 0)])
            for b in range(-1, 4):
                t0, n = (0, 256) if b < 0 else (CTX + 512 * b, 512)
                off = 2 if b < 0 else 262 + 512 * b
                ci = cctr[0] % 2
                cctr[0] += 1
                bank = pb[2 + ci]
                for jt in range(5):
                    mm(bank[:, 0:n], dgc[:, jt, :], rp[:, off + jt - 2:off + jt - 2 + n], jt == 0, jt == 4, [('diagc', m % 2), ('rawp', 0)], [('pbc', ci)])
                dst = dnT[:, m, t0:t0 + n]
                if m >= 8:
                    act(lambda dst=dst, bank=bank, n=n: A.activation(out=dst, in_=bank[:, 0:n], func=AF.Silu), [('pbc', ci)], [('dnT', m, b)])
                else:
                    act(lambda bank=bank, n=n, ci=ci: A.activation(out=cx[ci][:, 0:n], in_=bank[:, 0:n], func=AF.Silu), [('pbc', ci)], [('cx', ci)])
                    pool(lambda n=n, ci=ci: P.tensor_tensor(out=sqd[ci][:, 0:n], in0=cx[ci][:, 0:n], in1=cx[ci][:, 0:n], op=ALU.mult), [('cx', ci)], [('sqd', 0)])
                    mm(pb[4 + ci][:, 0:n], onesb, sqd[ci][:, 0:n], True, True, [('sqd', 0), 'onesb'], [('pbn', ci)])
                    act(lambda n=n, ci=ci: A.activation(out=rq2[ci][:, 0:n], in_=pb[4 + ci][:, 0:n], func=AF.Sqrt, bias=EPS), [('pbn', ci)], [('rq2', 0)])
                    dve(lambda n=n, ci=ci: V.reciprocal(out=rq2[ci][:, 0:n], in_=rq2[ci][:, 0:n]), [('rq2', 0)], [('rq2', 0)])
                    sc = 128 ** -0.5 if m < 4 else 1.0
                    dve(lambda dst=dst, n=n, ci=ci, sc=sc: V.scalar_tensor_tensor(out=dst, in0=cx[ci][:, 0:n], scalar=sc, in1=rq2[ci][:, 0:n], op0=ALU.mult, op1=ALU.mult),
                        [('cx', ci), ('rq2', 0)], [('dnT', m, b)])
        for zc in range(4):
            wp, wk = load_w(2304 + 128 * zc)
            for tl in range(16):
                ti = tl + 2
                bi = pctr[0] % 2
                pctr[0] += 1
                for kc in range(8):
                    mm(pb[bi][:, 0:128], hT[:, kc, ti * 128:(ti + 1) * 128], wp[:, kc, :], kc == 0, kc == 7, [wk], [('pbq', bi)])
                act(lambda tl=tl, zc=zc, bi=bi: A.copy(out=zt[:, tl, zc * 128:(zc + 1) * 128], in_=pb[bi][:, 0:128]), [('pbq', bi)], [('zt', tl, zc)])
        wp, wk = load_w(2816, 16)
        for ti in range(NT):
            bi = pctr[0] % 2
            pctr[0] += 1
            for kc in range(8):
                mm(pb[bi][:, 0:16], hT[:, kc, ti * 128:(ti + 1) * 128], wp[:, kc, 0:16], kc == 0, kc == 7, [wk], [('pbq', bi)])
            dve(lambda ti=ti, bi=bi: V.tensor_copy(out=gbraw[:, ti, :], in_=pb[bi][:, 0:16]), [('pbq', bi)], ['gbraw'])
        act(lambda: A.activation(out=gb[:, :, 0:8], in_=gbraw[:, :, 0:8], func=AF.Sigmoid), ['gbraw'], ['gb_b'])
        dve(lambda: V.tensor_scalar(out=nbeta, in0=gb[:, :, 0:8], scalar1=-1.0, scalar2=None, op0=ALU.mult), ['gb_b'], ['nbeta'])
        dve(lambda: V.tensor_tensor(out=gbraw[:, :, 8:16], in0=gbraw[:, :, 8:16], in1=dtb, op=ALU.add), ['gbraw', 'dtb'], ['gbraw2'])
        act(lambda: A.activation(out=gbraw[:, :, 8:16], in_=gbraw[:, :, 8:16], func=AF.Exp), ['gbraw2'], ['gbraw3'])
        act(lambda: A.activation(out=gbraw[:, :, 8:16], in_=gbraw[:, :, 8:16], func=AF.Ln, bias=1.0), ['gbraw3'], ['gbraw4'])
        act(lambda: A.activation(out=alog, in_=alog, func=AF.Exp), ['alog'], ['aexp'])
        dve(lambda: V.scalar_tensor_tensor(out=gb[:, :, 8:16], in0=gbraw[:, :, 8:16], scalar=-1.0, in1=alog, op0=ALU.mult, op1=ALU.mult),
            ['gbraw4', 'aexp'], ['gb_g'])
        if "dnT" in dbg_t:
            dd = AR.alloc([128, 12, 256], F32)
            dve(lambda: V.tensor_copy(out=dd, in_=dnT[:, :, 128:384]), [('dnT', m_, b_) for m_ in range(12) for b_ in (-1, 0)], ['dd'])
            dump("dnT", dd, ['dd'])
        dump("gb", gb, ['gb_b', 'gb_g'])
        S.barrier()
        AR.release(mC2)
        AR.release_top()
        if stop_after <= 4:
            S.finish()
            return nc

        osum = AR.alloc([128, 16, 512], F32)
        mE = AR.mark()
        NU = 4
        Sst = AR.alloc([128, 8, 128], F32)
        Sb = AR.alloc([128, 8, 128], BF16)
        ktok = [AR.alloc([128, 128], BF16) for _ in range(NU)]
        vtok = [AR.alloc([128, 128], BF16) for _ in range(NU)]
        KQ = [AR.alloc([128, 256], F32) for _ in range(NU)]
        gmx = [AR.alloc([128, 130], F32) for _ in range(NU)]
        Es = [AR.alloc([128, 128], F32) for _ in range(NU)]
        EmT = [AR.alloc([128, 128], F32) for _ in range(NU)]
        sc4 = [AR.alloc([128, 4], F32) for _ in range(NU)]
        Pm = [[AR.alloc([128, 128], F32) for _ in range(2)] for _ in range(NU)]
        PTm = [[AR.alloc([128, 128], F32) for _ in range(2)] for _ in range(NU)]
        TT = [AR.alloc([128, 128], F32) for _ in range(NU)]
        rv = [AR.alloc([128, 128], F32) for _ in range(NU)]
        rw = [AR.alloc([128, 128], F32) for _ in range(NU)]
        uu = [AR.alloc([128, 128], F32) for _ in range(NU)]
        wTb = [AR.alloc([128, 128], BF16) for _ in range(NU)]
        kdec = [AR.alloc([128, 128], BF16) for _ in range(NU)]
        dgb = [AR.alloc([128, 128], BF16) for _ in range(NU)]
        qdT = [AR.alloc([128, 128], BF16) for _ in range(NU)]
        qkT = [AR.alloc([128, 128], BF16) for _ in range(NU)]
        glb = [AR.alloc([128, 2], F32) for _ in range(NU)]
        vn = [AR.alloc([128, 128], BF16) for _ in range(NU)]
        dgf = Es
        pool(lambda: P.memset(Sst, 0.0), [], [('S', c_) for c_ in range(8)])
        pool(lambda: P.memset(Sb, 0.0), [], [('Sb', c_) for c_ in range(8)])
        pool(lambda: P.memset(osum, 0.0), [], [('osum', tl_, h_) for tl_ in range(16) for h_ in range(4)])

        def pe1(o, l, r_, rd, wr):
            return S.op('pe', lambda: TE.matmul(o, lhsT=l, rhs=r_, start=True, stop=True), rd, wr, pe_accum=True)

        NSTEP = int(os.environ.get('NSTEP', NT))
        fwd_order = list(range(NT))
        bwd_order = [1, 0] + list(range(NT - 1, 1, -1))

        def unit_gen(h, d, ti, i):
            B0, B2 = pb[2 * i], pb[2 * i + 1]
            PT = B2[:, 384:512].bitcast(BF16).rearrange("p (a b) -> p a b", b=128)
            k0, k2 = 'pbE0_%d' % i, 'pbE2_%d' % i
            kt_ = k2
            hi = i
            lat = ti >= 2
            tl = ti - 2
            tsl = slice(ti * 128, (ti + 1) * 128)
            c = d * 4 + h
            col = c
            Mtri = dnm[:, 0, :] if d == 0 else dnm[:, 2, :]
            maskA = dnm[:, 1, :] if d == 0 else dnm[:, 3, :]
            maskQK = Mtri
            gcol = gb[:, ti, 8 + col:9 + col]
            bcol = gb[:, ti, col:col + 1]
            nbcol = nbeta[:, ti, col:col + 1]
            kT_t = dnT[:, 4 + h, tsl]
            qT_t = dnT[:, h, tsl]
            vT_t = dnT[:, 8 + h, tsl]
            pk, pv = 0, 1
            tr(PT[:, pk, :], kT_t, identb, [], [kt_])
            tr(PT[:, pv, :], vT_t, identb, [], [kt_])
            pe1(B0[:, 0:128], kT_t, kT_t, [], [k0])
            if lat:
                pe1(B0[:, 128:256], kT_t, qT_t, [], [k0])
            dve(lambda: V.tensor_scalar(out=gmx[i], in0=msx[:, d, :], scalar1=gcol, scalar2=None, op0=ALU.mult), [], [('gmx', i)])
            yield
            act(lambda: A.copy(out=ktok[hi], in_=PT[:, pk, :]), [kt_], [('ktok', hi)])
            act(lambda: A.copy(out=vtok[hi], in_=PT[:, pv, :]), [kt_], [('vtok', hi)])
            act(lambda: A.copy(out=KQ[hi], in_=B0[:, 0:256]), [k0], [('KQ', hi)])
            pe1(B2[:, 0:130], Mtri, gmx[i], [('gmx', i)], [k2])
            pe1(B2[:, 130:132], blk64f, gmx[i][:, 128:130], [('gmx', i)], [k2])
            if lat:
                pe1(B2[:, 256:384], gmx[i][:, 0:128], Mtri, [('gmx', i)], [k2])
            yield
            act(lambda: A.activation(out=Es[i], in_=B2[:, 0:128], func=AF.Exp), [k2], [('Es', i)])
            act(lambda: A.activation(out=sc4[i][:, 0:1], in_=B2[:, 128:129], func=AF.Exp), [k2], [('egc', i)])
            act(lambda: A.copy(out=sc4[i][:, 1:2], in_=B2[:, 130:131]), [k2], [('tots', i)])
            act(lambda: A.activation(out=sc4[i][:, 2:3], in_=B2[:, 128:129], func=AF.Exp, scale=-1.0, bias=sc4[i][:, 1:2]),
                [k2, ('tots', i)], [('erest', i)])
            act(lambda: A.activation(out=sc4[i][:, 3:4], in_=sc4[i][:, 1:2], func=AF.Exp), [('tots', i)], [('etot', i)])
            if lat:
                act(lambda: A.activation(out=EmT[i], in_=B2[:, 256:384], func=AF.Exp), [k2], [('EmT', i)])
                pool(lambda: P.tensor_tensor(out=EmT[i], in0=EmT[i], in1=maskQK, op=ALU.mult), [('EmT', i)], [('EmT', i)])
            pool(lambda: P.tensor_tensor(out=Es[i], in0=Es[i], in1=maskA, op=ALU.mult), [('Es', i)], [('Es', i)])
            yield
            dve(lambda: V.scalar_tensor_tensor(out=Pm[i][0], in0=KQ[hi][:, 0:128], scalar=nbcol, in1=Es[i], op0=ALU.mult, op1=ALU.mult),
                [('KQ', hi), ('Es', i)], [('P', i, 0)])
            tr(B2[:, 0:128], Pm[i][0], ident, [('P', i, 0)], [k2])
            yield
            dve(lambda: V.tensor_copy(out=PTm[i][0], in_=B2[:, 0:128]), [k2], [('PT', i, 0)])
            dve(lambda: V.tensor_tensor(out=TT[i], in0=B2[:, 0:128], in1=ident, op=ALU.add), [k2], [('TT', i)])
            for k in range(5):
                cu, nx = k % 2, 1 - k % 2
                pe1(B0[:, 0:128], PTm[i][cu], Pm[i][cu], [('PT', i, cu), ('P', i, cu)], [k0])
                if k < 4:
                    pe1(B0[:, 128:256], Pm[i][cu], PTm[i][cu], [('PT', i, cu), ('P', i, cu)], [k0])
                yield
                if k < 4:
                    act(lambda nx=nx: A.copy(out=Pm[i][nx], in_=B0[:, 0:128]), [k0], [('P', i, nx)])
                    act(lambda nx=nx: A.copy(out=PTm[i][nx], in_=B0[:, 128:256]), [k0], [('PT', i, nx)])
                else:
                    act(lambda nx=nx: A.copy(out=Pm[i][nx], in_=B0[:, 0:128]), [k0], [('P', i, nx)])
                pe1(B2[:, 0:128], Pm[i][nx], TT[i], [('P', i, nx), ('TT', i)], [k2])
                yield
                dve(lambda: V.tensor_tensor(out=TT[i], in0=B2[:, 0:128], in1=TT[i], op=ALU.add), [k2, ('TT', i)], [('TT', i)])
            dve(lambda: V.tensor_scalar(out=rv[i], in0=vtok[hi], scalar1=bcol, scalar2=None, op0=ALU.mult), [('vtok', hi)], [('rv', i)])
            dve(lambda: V.tensor_scalar(out=rw[i], in0=ktok[hi], scalar1=bcol, scalar2=None, op0=ALU.mult), [('ktok', hi)], [('rw', i)])
            dve(lambda: V.tensor_scalar(out=rw[i], in0=rw[i], scalar1=sc4[i][:, 0:1], scalar2=None, op0=ALU.mult), [('rw', i), ('egc', i)], [('rw', i)])
            dve(lambda: V.tensor_scalar(out=kdec[i], in0=ktok[hi], scalar1=sc4[i][:, 2:3], scalar2=None, op0=ALU.mult),
                [('ktok', hi), ('erest', i)], [('kdec', i)])
            if lat:
                dve(lambda: V.tensor_scalar(out=dgb[i], in0=ident, scalar1=sc4[i][:, 0:1], scalar2=None, op0=ALU.mult), [('egc', i)], [('dgb', i)])
            pe1(B0[:, 0:128], TT[i], rv[i], [('TT', i), ('rv', i)], [k0])
            pe1(B0[:, 128:256], rw[i], TT[i], [('TT', i), ('rw', i)], [k0])
            pe1(B0[:, 256:257], selrow[:, 0, :], sc4[i][:, 3:4], [('etot', i)], [k0])
            pe1(B0[:, 257:258], selrow[:, 1, :], sc4[i][:, 3:4], [('etot', i)], [k0])
            if lat:
                pe1(B0[:, 384:512], onesb, dgb[i], [('dgb', i)], [k0])
            yield
            act(lambda: A.copy(out=uu[i], in_=B0[:, 0:128]), [k0], [('uu', i)])
            act(lambda: A.copy(out=wTb[i], in_=B0[:, 128:256]), [k0], [('wTb', i)])
            act(lambda: A.copy(out=glb[i], in_=B0[:, 256:258]), [k0], [('glb', i)])
            if lat:
                act(lambda: A.copy(out=dgf[i], in_=B0[:, 384:512]), [k0], [('Es', i)])
                dve(lambda: V.tensor_tensor(out=qdT[i], in0=qT_t, in1=dgf[i], op=ALU.mult), [('Es', i)], [('qdT', i)])
                dve(lambda: V.tensor_tensor(out=qkT[i], in0=KQ[hi][:, 128:256], in1=EmT[i], op=ALU.mult), [('KQ', hi), ('EmT', i)], [('qkT', i)])
            yield
            for pi_ in ((0, 1) if d == 0 else (1, 0)):
                R = slice(pi_ * 64, pi_ * 64 + 64)
                pe1(B2[:, 0:128], wTb[i], Sb[:, c, :], [('wTb', i), ('Sb', c)], [k2])
                if lat:
                    S.op('pe', lambda: TE.matmul(B2[:, 128:256], lhsT=qdT[i], rhs=Sb[:, c, :], start=True, stop=False),
                         [('qdT', i), ('Sb', c)], [k2], pe_accum=True)
                yield
                dve(lambda R=R: V.tensor_tensor(out=vn[i][R, :], in0=uu[i][R, :], in1=B2[R, 0:128], op=ALU.subtract), [('uu', i), k2], [('vn', i)])
                if lat:
                    S.op('pe', lambda R=R: TE.matmul(B2[:, 128:256], lhsT=qkT[i][R, :], rhs=vn[i][R, :], start=False, stop=True),
                         [('qkT', i), ('vn', i)], [k2], pe_accum=True)
                pe1(B2[:, 256:384], kdec[i][R, :], vn[i][R, :], [('kdec', i), ('vn', i)], [k2])
                yield
                if lat:
                    dve(lambda R=R: V.tensor_tensor(out=osum[R, tl, h * 128:(h + 1) * 128], in0=osum[R, tl, h * 128:(h + 1) * 128],
                                                    in1=B2[R, 128:256], op=ALU.add), [k2, ('osum', tl, h)], [('osum', tl, h)])
                dve(lambda pi_=pi_: V.scalar_tensor_tensor(out=Sst[:, c, :], in0=Sst[:, c, :], scalar=glb[i][:, pi_:pi_ + 1], in1=B2[:, 256:384],
                                                           op0=ALU.mult, op1=ALU.add), [k2, ('glb', i), ('S', c)], [('S', c)])
                pool(lambda: P.tensor_copy(out=Sb[:, c, :], in_=Sst[:, c, :]), [('S', c)], [('Sb', c)])
                yield

        for step in range(NSTEP):
            chains = [(h, d) for h in range(4) for d in range(2)]
            for p0 in range(0, 8, NU):
                gens = []
                for slot, (h, d) in enumerate(chains[p0:p0 + NU]):
                    ti = fwd_order[step] if d == 0 else bwd_order[step]
                    gens.append(unit_gen(h, d, ti, slot))
                alive = list(gens)
                while alive:
                    for gobj in list(alive):
                        try:
                            next(gobj)
                        except StopIteration:
                            alive.remove(gobj)
        if "S0" in dbg_t:
            dump("S0", Sst[:, int(os.environ.get('DBGC', 0)), :], [('S', c_) for c_ in range(8)])
        if "osum" in dbg_t:
            dump("osum", osum[:, 0:4, :], [('osum', tl_, h_) for tl_ in range(4) for h_ in range(4)])
        S.barrier()
        AR.release(mE)
        if stop_after <= 5:
            S.finish()
            return nc

        onw = AR.alloc([128, 128], F32)
        sq2 = [AR.alloc([128, 512], F32) for _ in range(2)]
        sse = AR.alloc([128, 16, 4], F32)
        szl = [AR.alloc([128, 512], F32) for _ in range(2)]
        ydn = [AR.alloc([128, 512], BF16) for _ in range(2)]
        S.dma('sp', onw, dr["onw"], writes=['onw'])
        for tl in range(16):
            i = tl % 2
            okeys = [('osum', tl, h_) for h_ in range(4)]
            pool(lambda tl=tl, i=i: P.tensor_tensor(out=sq2[i], in0=osum[:, tl, :], in1=osum[:, tl, :], op=ALU.mult), okeys, [('sq2', i)])
            dve(lambda tl=tl, i=i: V.tensor_reduce(out=sse[:, tl, :], in_=sq2[i].rearrange("p (h d) -> p h d", d=128), axis=AX.X, op=ALU.add),
                [('sq2', i)], [('sse', tl)])
            dve(lambda tl=tl: V.tensor_scalar(out=sse[:, tl, :], in0=sse[:, tl, :], scalar1=1.0 / 128, scalar2=EPS, op0=ALU.mult, op1=ALU.add),
                [('sse', tl)], [('sse', tl)])
            act(lambda tl=tl: A.activation(out=sse[:, tl, :], in_=sse[:, tl, :], func=AF.Sqrt), [('sse', tl)], [('sse', tl)])
            dve(lambda tl=tl: V.reciprocal(out=sse[:, tl, :], in_=sse[:, tl, :]), [('sse', tl)], [('sse', tl)])
            act(lambda tl=tl, i=i: A.activation(out=szl[i], in_=zt[:, tl, :], func=AF.Silu), [], [('szl', i)])
            for h in range(4):
                dve(lambda tl=tl, i=i, h=h: V.scalar_tensor_tensor(out=sq2[i][:, h * 128:(h + 1) * 128], in0=osum[:, tl, h * 128:(h + 1) * 128],
                                                                 scalar=sse[:, tl, h:h + 1], in1=onw, op0=ALU.mult, op1=ALU.mult),
                    okeys + [('sse', tl), 'onw', ('sq2', i)], [('sq2', i)])
            dve(lambda i=i: V.tensor_tensor(out=ydn[i], in0=sq2[i], in1=szl[i], op=ALU.mult), [('sq2', i), ('szl', i)], [('ydn', i)])
            for h in range(4):
                tr(pbt[1][:, h, :], ydn[i][:, h * 128:(h + 1) * 128], identb, [('ydn', i)], ['pbt1'])
            act(lambda tl=tl: A.copy(out=dnoT[:, :, tl * 128:(tl + 1) * 128], in_=pbt[1][:, 0:4, :]), ['pbt1'], [('dnoT', tl)])
        if "dnoT" in dbg_t:
            ddn = AR.alloc([128, 4, 512], F32)
            dve(lambda: V.tensor_copy(out=ddn, in_=dnoT[:, :, 0:512]), [('dnoT', tl_) for tl_ in range(4)], ['ddn'])
            dump("dnoT", ddn, ['ddn'])
        S.barrier()
        AR.release(mDN)
        if stop_after <= 6:
            S.finish()
            return nc

        h2 = AR.alloc_top([128, 16, 1024], BF16)
        aff = AR.alloc_top([128, 16, 16], F32)
        posm = AR.alloc_top([128, 16, 16], F32)
        gmb = AR.alloc_top([128, 16, 32], BF16)
        posTb = AR.alloc_top([16, 2048], BF16)
        mF = AR.mark()
        woutb = AR.alloc([128, 8, 1024], BF16)
        rws = AR.alloc([128, 8, 16], F32)
        rwb = AR.alloc([128, 8, 16], BF16)
        xt2 = [AR.alloc([128, 1024], F32) for _ in range(2)]
        x1t = [AR.alloc([128, 1024], F32) for _ in range(2)]
        tmp2 = AR.alloc([128, 1024], F32)
        junk2 = AR.alloc([128, 1024], F32)
        ss2 = AR.alloc([128, 16], F32)
        h2Tt = [AR.alloc([128, 8, 128], BF16) for _ in range(2)]
        smx = AR.alloc([128, 16, 4], F32)
        ee = AR.alloc([128, 16], F32)
        woutv = dr["w_out"].rearrange("(kc p) n -> p kc n", p=128)
        for q in range(4):
            S.dma('pool', woutb[:, 2 * q:2 * q + 2, :], woutv[:, 2 * q:2 * q + 2, :], writes=[('woutb', q)])
        S.dma('sp', rws, dr["router_w"].rearrange("(kc p) e -> p kc e", p=128), writes=['rws'])
        dve(lambda: V.tensor_copy(out=rwb, in_=rws), ['rws'], ['rwb'])
        for tl in range(16):
            i = tl % 2
            tsl = slice(tl * 128, (tl + 1) * 128)
            S.dma('sp', xt2[i], dr["x"][tsl, :], writes=[('xt2', i)])
            for half in range(2):
                hs = slice(half * 512, (half + 1) * 512)
                for kc in range(8):
                    lhs = attT[:, kc, tsl] if kc < 4 else dnoT[:, kc - 4, tsl]
                    mm(pb[half], lhs, woutb[:, kc, hs], kc == 0, kc == 7, [('woutb', kc // 2)], [('pby', half)])
                dve(lambda i=i, half=half, hs=hs: V.tensor_tensor(out=x1t[i][:, hs], in0=pb[half], in1=g1row[:, hs], op=ALU.mult),
                    [('pby', half)], [('x1t', i, half)])
                dve(lambda i=i, hs=hs: V.tensor_tensor(out=x1t[i][:, hs], in0=x1t[i][:, hs], in1=xt2[i][:, hs], op=ALU.add),
                    [('x1t', i, half), ('xt2', i)], [('x1t', i, half)])
            S.dma('pool', x1s[tsl, :], x1t[i], reads=[('x1t', i, 0), ('x1t', i, 1)], writes=[('x1s', tl)])
            xk = [('x1t', i, 0), ('x1t', i, 1)]
            act(lambda i=i, tl=tl: A.activation(out=junk2, in_=x1t[i], func=AF.Square, accum_out=ss2[:, tl:tl + 1]), xk, ['junk2', ('ss2', tl)])
            dve(lambda tl=tl: V.tensor_scalar(out=ss2[:, tl:tl + 1], in0=ss2[:, tl:tl + 1], scalar1=1.0 / D, scalar2=EPS, op0=ALU.mult, op1=ALU.add),
                [('ss2', tl)], [('ss2', tl)])
            act(lambda tl=tl: A.activation(out=ss2[:, tl:tl + 1], in_=ss2[:, tl:tl + 1], func=AF.Sqrt), [('ss2', tl)], [('ss2', tl)])
            dve(lambda tl=tl: V.reciprocal(out=ss2[:, tl:tl + 1], in_=ss2[:, tl:tl + 1]), [('ss2', tl)], [('ss2', tl)])
            dve(lambda i=i, tl=tl: V.scalar_tensor_tensor(out=tmp2, in0=x1t[i], scalar=ss2[:, tl:tl + 1], in1=s2row, op0=ALU.mult, op1=ALU.mult),
                xk + [('ss2', tl)], ['tmp2'])
            dve(lambda tl=tl: V.tensor_tensor(out=h2[:, tl, :], in0=tmp2, in1=b2row, op=ALU.add), ['tmp2'], [('h2', tl)])
            for kc in range(8):
                tr(pbt[i][:, kc, :], h2[:, tl, kc * 128:(kc + 1) * 128], identb, [('h2', tl)], [('pbt', i)])
            act(lambda i=i: A.copy(out=h2Tt[i], in_=pbt[i]), [('pbt', i)], [('h2Tt', i)])
            for kc in range(8):
                mm(pb[2 + i][:, 0:16], h2Tt[i][:, kc, :], rwb[:, kc, :], kc == 0, kc == 7, [('h2Tt', i), 'rwb'], [('pbr', i)])
            dve(lambda i=i, tl=tl: V.tensor_reduce(out=smx[:, tl, 0:1], in_=pb[2 + i][:, 0:16], axis=AX.X, op=ALU.max), [('pbr', i)], [('smx', tl)])
            dve(lambda tl=tl: V.tensor_scalar(out=smx[:, tl, 1:2], in0=smx[:, tl, 0:1], scalar1=-1.0, scalar2=None, op0=ALU.mult), [('smx', tl)], [('smx', tl)])
            act(lambda i=i, tl=tl: A.activation(out=ee, in_=pb[2 + i][:, 0:16], func=AF.Exp, bias=smx[:, tl, 1:2], accum_out=smx[:, tl, 2:3]),
                [('pbr', i), ('smx', tl)], ['ee', ('smx', tl)])
            dve(lambda tl=tl: V.reciprocal(out=smx[:, tl, 3:4], in_=smx[:, tl, 2:3]), [('smx', tl)], [('smx', tl)])
            dve(lambda tl=tl: V.tensor_scalar(out=aff[:, tl, :], in0=ee, scalar1=smx[:, tl, 3:4], scalar2=None, op0=ALU.mult), ['ee', ('smx', tl)], [('aff', tl)])
        if "x1" in dbg_t:
            dump("x1", x1t[0], [('x1t', 0, 0), ('x1t', 0, 1)])
        dump("aff", aff, [('aff', tl_) for tl_ in range(16)])
        S.barrier()
        AR.release(mF)
        if stop_after <= 7:
            S.finish()
            return nc

        mF2 = AR.mark()
        affT = AR.alloc([16, 2048], F32)
        work = AR.alloc([16, 2048], F32)
        maskT = AR.alloc([16, 2048], F32)
        cum = AR.alloc([16, 2048], F32)
        ones16 = AR.alloc([16, 2048], F32)
        posTf = AR.alloc([16, 2048], F32)
        m8 = AR.alloc([16, 8], F32)
        mtok = AR.alloc([128, 256], F32)
        gm = AR.alloc([128, 256], F32)
        gml = AR.alloc([128, 256], F32)
        for tl in range(16):
            q = tl // 4
            tr(pb[q][0:16, (tl % 4) * 128:(tl % 4 + 1) * 128], aff[:, tl, :], ident, [], [('pbA', q)])
        for q in range(4):
            act(lambda q=q: A.copy(out=affT[:, q * 512:(q + 1) * 512], in_=pb[q][0:16, :]), [('pbA', q)], ['affT'])
        pool(lambda: P.tensor_copy(out=work, in_=affT), ['affT'], ['work'])
        pool(lambda: P.memset(ones16, 1.0), [], ['ones16'])
        for it_ in range(CAP // 8):
            dve(lambda: V.max(out=m8, in_=work), ['work'], ['m8'])
            dve(lambda: V.match_replace(out=work, in_to_replace=m8, in_values=work, imm_value=-1.0), ['m8', 'work'], ['work'])
        dve(lambda: V.tensor_scalar(out=maskT, in0=affT, scalar1=m8[:, 7:8], scalar2=None, op0=ALU.is_ge), ['affT', 'm8'], ['maskT'])
        dve(lambda: V.tensor_tensor_scan(out=cum, data0=ones16, data1=maskT, initial=0.0, op0=ALU.mult, op1=ALU.add), ['ones16', 'maskT'], ['cum'])
        dve(lambda: V.tensor_tensor(out=posTf, in0=cum, in1=maskT, op=ALU.mult), ['cum', 'maskT'], ['posTf'])
        dve(lambda: V.tensor_scalar(out=posTf, in0=posTf, scalar1=-1.0, scalar2=None, op0=ALU.add), ['posTf'], ['posTf'])
        act(lambda: A.copy(out=posTb, in_=posTf), ['posTf'], ['posTb'])
        for tl in range(16):
            tr(pb[4][:, tl * 16:(tl + 1) * 16], posTf[:, tl * 128:(tl + 1) * 128], ident[0:16, 0:16], ['posTf'], ['pb4'])
        posm_v = posm.rearrange("p t e -> p (t e)")
        aff_v = aff.rearrange("p t e -> p (t e)")
        gmb_v = gmb.rearrange("p t (e two) -> p (t e) two", two=2)
        act(lambda: A.copy(out=posm_v, in_=pb[4][:, 0:256]), ['pb4'], ['posm'])
        dve(lambda: V.tensor_scalar(out=mtok, in0=posm_v, scalar1=0.0, scalar2=None, op0=ALU.is_ge), ['posm'], ['mtok'])
        dve(lambda: V.tensor_tensor(out=gm, in0=aff_v, in1=mtok, op=ALU.mult), ['mtok'], ['gm'])
        dve(lambda: V.tensor_copy(out=gmb_v[:, :, 0], in_=gm), ['gm'], ['gmb0'])
        dve(lambda: V.tensor_tensor(out=gml, in0=gm, in1=gmb_v[:, :, 0], op=ALU.subtract), ['gm', 'gmb0'], ['gml'])
        dve(lambda: V.tensor_copy(out=gmb_v[:, :, 1], in_=gml), ['gml'], ['gmb1'])
        if "posT" in dbg_t:
            dump("posT", posTf, ['posTf'])
        S.barrier()
        AR.release(mDN0)
        if stop_after <= 8:
            S.finish()
            return nc

        yall = AR.alloc([128, NE, 2, 1024], BF16)
        mG = AR.mark()
        wgb = AR.alloc([128, 8, 1024], BF16)
        wub = AR.alloc([128, 8, 1024], BF16)
        wdb = AR.alloc([128, 8, 1024], BF16)
        Sel = AR.alloc([128, 16, 256], BF16)
        xgT = g1row.bitcast(BF16).rearrange("p (a b) -> p a b", b=256)
        hidT = s2row.bitcast(BF16).rearrange("p (a b) -> p a b", b=256)
        sgv = b2row.rearrange("p (a b) -> p a b", b=256)
        g4 = sgv[:, 2, 0:4]
        gs = sgv[:, 2, 8:10]
        def load_expert(e):
            for (wd, dst, key) in ((dr["w_gate"][e], wgb, 'wgb'), (dr["w_up"][e], wub, 'wub'), (dr["w_down"][e], wdb, 'wdb')):
                for kc in range(8):
                    S.dma('pool', dst[:, kc, :], wd[kc * 128:(kc + 1) * 128, :], writes=[(key, kc)])

        NEXP = int(os.environ.get('NEXP', NE))
        load_expert(0)
        for e in range(NEXP):
            for tl in range(16):
                dve(lambda tl=tl, e=e: V.tensor_scalar(out=Sel[:, tl, :], in0=iota256, scalar1=posm[:, tl, e:e + 1], scalar2=None, op0=ALU.is_equal),
                    [], [('Sel', tl)])
            for kc in range(8):
                bi = kc % 2
                for tl in range(16):
                    mm(pb[bi][:, 0:256], h2[:, tl, kc * 128:(kc + 1) * 128], Sel[:, tl, :], tl == 0, tl == 15, [('Sel', tl)], [('pbg', bi)])
                act(lambda kc=kc, bi=bi: A.copy(out=xgT[:, kc, :], in_=pb[bi][:, 0:256]), [('pbg', bi)], [('xgT', kc)])
            for cc in range(2):
                for tl in range(16):
                    mm(pb[0][:, 256 + cc * 2:258 + cc * 2], Sel[:, tl, cc * 128:(cc + 1) * 128], gmb[:, tl, 2 * e:2 * e + 2], tl == 0, tl == 15,
                       [('Sel', tl), 'gmb0', 'gmb1'], [('pbg', 0)])
            act(lambda: A.copy(out=g4, in_=pb[0][:, 256:260]), [('pbg', 0)], ['g4'])
            dve(lambda: V.tensor_tensor(out=gs, in0=g4.rearrange("p (c t) -> p c t", t=2)[:, :, 0], in1=g4.rearrange("p (c t) -> p c t", t=2)[:, :, 1], op=ALU.add),
                ['g4'], ['gs'])
            for ffc in range(8):
                pg, pu = (pb[2], pb[3]) if ffc % 2 == 0 else (pb[4], pb[5])
                kg, ku = ('pbf', 2 + (ffc % 2) * 2), ('pbf', 3 + (ffc % 2) * 2)
                for kc in range(8):
                    mm(pg[:, 0:256], wgb[:, kc, ffc * 128:(ffc + 1) * 128], xgT[:, kc, :], kc == 0, kc == 7, [('wgb', kc), ('xgT', kc)], [kg])
                for kc in range(8):
                    mm(pu[:, 0:256], wub[:, kc, ffc * 128:(ffc + 1) * 128], xgT[:, kc, :], kc == 0, kc == 7, [('wub', kc), ('xgT', kc)], [ku])
                si = ffc % 2
                act(lambda pg=pg, si=si: A.activation(out=sgv[:, si, :], in_=pg[:, 0:256], func=AF.Silu), [kg], [('sg', si)])
                dve(lambda pu=pu, si=si, ffc=ffc: V.tensor_tensor(out=hidT[:, ffc, :], in0=sgv[:, si, :], in1=pu[:, 0:256], op=ALU.mult),
                    [('sg', si), ku], [('hidT', ffc)])
            for cc in range(2):
                for half in range(2):
                    bi = (cc * 2 + half) % 2
                    for ffc in range(8):
                        mm(pb[bi], hidT[:, ffc, cc * 128:(cc + 1) * 128], wdb[:, ffc, half * 512:(half + 1) * 512], ffc == 0, ffc == 7,
                           [('hidT', ffc), ('wdb', ffc)], [('pbg', bi)])
                    act(lambda e=e, cc=cc, half=half, bi=bi: A.activation(out=yall[:, e, cc, half * 512:(half + 1) * 512], in_=pb[bi], func=AF.Identity, scale=gs[:, cc:cc + 1]),
                        [('pbg', bi), 'gs'], [('yall', e)])
            if e + 1 < NEXP:
                load_expert(e + 1)
        if "yall" in dbg_t:
            dy = AR.alloc([128, 2, 1024], F32)
            dve(lambda: V.tensor_copy(out=dy, in_=yall[:, 0, :, :]), [('yall', 0)], ['dy'])
            dump("yall", dy, ['dy'])
        S.barrier()
        AR.release(mG)
        if stop_after <= 9:
            S.finish()
            return nc

        SelT = [AR.alloc([128, 2, 256], BF16) for _ in range(2)]
        xo = [AR.alloc([128, 1024], F32) for _ in range(2)]
        ot = [AR.alloc([128, 1024], F32) for _ in range(2)]
        oc = 0
        for blk in range(8):
            for e in range(NE):
                si = e % 2
                pe1(pb[4 + si][:, 0:256], oh16[:, e, :], posTb[:, blk * 256:(blk + 1) * 256], [], [('pbp', si)])
                for cc in range(2):
                    dve(lambda si=si, cc=cc: V.tensor_scalar(out=SelT[si][:, cc, :], in0=pb[4 + si][:, 0:256], scalar1=iotap[:, cc:cc + 1], scalar2=None,
                                                          op0=ALU.is_equal), [('pbp', si)], [('SelT', si, cc)])
                for cc in range(2):
                    for tt in range(2):
                        for half in range(2):
                            first = (e == 0 and cc == 0)
                            last = (e == NE - 1 and cc == 1)
                            mm(pb[tt * 2 + half], SelT[si][:, cc, tt * 128:(tt + 1) * 128], yall[:, e, cc, half * 512:(half + 1) * 512], first, last,
                               [('SelT', si, cc)], [('pbo', tt * 2 + half)])
            for tt in range(2):
                tl = blk * 2 + tt
                i = oc % 2
                oc += 1
                S.dma('sp', xo[i], x1s[tl * 128:(tl + 1) * 128, :], reads=[('x1s', tl)], writes=[('xo', i)])
                for half in range(2):
                    hs = slice(half * 512, (half + 1) * 512)
                    dve(lambda i=i, tt=tt, half=half, hs=hs: V.tensor_tensor(out=ot[i][:, hs], in0=pb[tt * 2 + half], in1=g2row[:, hs], op=ALU.mult),
                        [('pbo', tt * 2 + half)], [('ot', i, half)])
                    dve(lambda i=i, hs=hs: V.tensor_tensor(out=ot[i][:, hs], in0=ot[i][:, hs], in1=xo[i][:, hs], op=ALU.add),
                        [('ot', i, half), ('xo', i)], [('ot', i, half)])
                S.dma('act', out[tl * 128:(tl + 1) * 128, :], ot[i], reads=[('ot', i, 0), ('ot', i, 1)], writes=[('out', tl)])
        S.finish()
    return nc


_CACHE = {}


def _layout_inputs(inp, b):
    f = np.float32
    m = {}
    m["x"] = np.ascontiguousarray(inp["x"][b], f)
    m["ctx"] = np.ascontiguousarray(inp["ctx"][b], f)
    cT = np.zeros((128, 8, 2), f)
    cT[:, :, 0] = inp["c"][b].reshape(8, 128).T
    cT[:, :, 1] = inp["c_ctx"].reshape(8, 128).T
    m["cT"] = cT.reshape(128, 16)
    m["w_mod"] = np.ascontiguousarray(inp["w_mod"][0], f)
    m["bmrow"] = np.ascontiguousarray(np.tile(inp["b_mod"][0][None, :], (2, 1)), f)
    m["n2row"] = np.ascontiguousarray(np.tile(inp["norm2_w"][0][None, :], (2, 1)), f)
    n1 = inp["norm1_w"][0].reshape(8, 128).T
    n2 = inp["norm2_w"][0].reshape(8, 128).T
    m["n12T"] = np.ascontiguousarray(np.concatenate([np.repeat(n1[:, :, None], 2, axis=2).reshape(128, 16),
                                                     np.repeat(n2[:, :, None], 2, axis=2).reshape(128, 16)], axis=1), f)
    m["w_in"] = np.ascontiguousarray(inp["w_in"][0], f)
    m["qkw"] = np.ascontiguousarray(np.stack([np.tile(inp["q_norm_w"][0], 2), np.tile(inp["k_norm_w"][0], 2)], axis=1), f)
    cw = inp["conv_w"][0]
    m["convT"] = np.ascontiguousarray(cw.reshape(5, 12, 128).transpose(2, 1, 0).reshape(128, 60), f)
    m["alog"] = np.ascontiguousarray(np.tile(inp["a_log"][0].reshape(1, 8), (128, NT)), f)
    m["dtb"] = np.ascontiguousarray(np.tile(inp["dt_bias"][0].reshape(1, 8), (128, NT)), f)
    m["onw"] = np.ascontiguousarray(np.tile(inp["o_norm_w"][0].reshape(1, 128), (128, 1)), f)
    m["w_out"] = np.ascontiguousarray(inp["w_out"][0], f)
    m["router_w"] = np.ascontiguousarray(inp["router_w"][0], f)
    m["w_gate"] = np.ascontiguousarray(inp["w_gate"][0], f)
    m["w_up"] = np.ascontiguousarray(inp["w_up"][0], f)
    m["w_down"] = np.ascontiguousarray(inp["w_down"][0], f)
    return m


def kernel(**inputs):
    inp = {k: np.asarray(v) for k, v in inputs.items()}
    if "nc" not in _CACHE:
        _CACHE["nc"] = build_program()
    nc = _CACHE["nc"]
    consts = host_consts()
    in_maps = []
    shared = None
    for b in range(8):
        m = _layout_inputs(inp, b)
        if shared is None:
            shared = {k: m[k] for k in m if k not in ("x", "ctx", "cT")}
        else:
            for k in shared:
                m[k] = shared[k]
        m.update(consts)
        in_maps.append(m)
    res = run_bass_kernel_spmd(nc, in_maps, core_ids=list(range(8)))
    return np.stack([np.asarray(r["out"], np.float32) for r in res.results], axis=0)
```
